# Optimizing a Trainium2 kernel written in Bass

```python
import jax, jax.numpy as jnp
from jax import lax
import numpy as np

D_MODEL = 4096
BATCH = 4
SEQ = 4096
DEPTH = 1

D_MIX = D_MODEL
D_ATTN = D_MIX // 2
D_POOL = D_MIX - D_ATTN
HEAD_DIM = 128
N_ATTN_HEADS = D_ATTN // HEAD_DIM
POOL_WINDOWS = (2, 4, 8, 16)
N_POOL_GROUPS = len(POOL_WINDOWS)
POOL_GROUP = D_POOL // N_POOL_GROUPS
Q_BLOCK = 128
PEER_HEADS = 8
PEER_TOPK = 16
PEER_NKEYS = 128
PEER_NEXPERTS = PEER_NKEYS * PEER_NKEYS
PEER_DQ = 256
PEER_DHALF = PEER_DQ // 2
PEER_CHUNK = 64
N_MOD = 6
EPS = 1e-6

kernel_name = 'hybrid_stickbreak_pool_peer_adaln'


def rmsnorm(x, g):
    x32 = x.astype(jnp.float32)
    y = x32 * lax.rsqrt(jnp.mean(x32 * x32, axis=-1, keepdims=True) + EPS)
    return (y * g.astype(jnp.float32)).astype(x.dtype)


def stick_breaking_attention(q, k, v):
    S = q.shape[2]
    outs = []
    for i in range(S // Q_BLOCK):
        start = i * Q_BLOCK
        end = start + Q_BLOCK
        qb = q[:, :, start:end]
        kb = k[:, :, :end]
        vb = v[:, :, :end]
        z = jnp.einsum('bhtd,bhsd->bhts', qb, kb).astype(jnp.float32) * (HEAD_DIM ** -0.5)
        q_pos = start + jnp.arange(Q_BLOCK)
        k_pos = jnp.arange(end)
        causal = k_pos[None, :] < q_pos[:, None]
        log_not = jnp.where(causal, jax.nn.log_sigmoid(-z), 0.0)
        between = lax.cumsum(log_not, axis=3, reverse=True) - log_not
        a = jnp.where(causal, jnp.exp(jax.nn.log_sigmoid(z) + between), 0.0)
        outs.append(jnp.einsum('bhts,bhsd->bhtd', a.astype(vb.dtype), vb))
    return jnp.concatenate(outs, axis=2)


def multiscale_pool(p, w_pool, s_pool):
    B, S, _ = p.shape
    p32 = p.astype(jnp.float32)
    csum = jnp.cumsum(p32, axis=1)
    pos = jnp.arange(S)
    groups = []
    for gi, w in enumerate(POOL_WINDOWS):
        sl = slice(gi * POOL_GROUP, (gi + 1) * POOL_GROUP)
        cg = csum[..., sl]
        lagged = jnp.pad(cg, ((0, 0), (w, 0), (0, 0)))[:, :S]
        count = jnp.minimum(pos + 1, w).astype(jnp.float32)[None, :, None]
        groups.append((cg - lagged) / count - p32[..., sl])
    d = jnp.stack(groups, axis=2).astype(p.dtype)
    y = jnp.einsum('bsgc,gcd->bsgd', d, w_pool).reshape(B, S, D_POOL)
    return y * s_pool


def peer_ffn(h, w_query, sub_keys_1, sub_keys_2, u_experts, v_experts):
    B, S, D = h.shape
    q = jnp.einsum('bsd,dq->bsq', h, w_query).reshape(B, S, PEER_HEADS, PEER_DQ)
    q1 = q[..., :PEER_DHALF]
    q2 = q[..., PEER_DHALF:]
    s1 = jnp.einsum('bshc,nc->bshn', q1, sub_keys_1).astype(jnp.float32)
    s2 = jnp.einsum('bshc,nc->bshn', q2, sub_keys_2).astype(jnp.float32)
    v1, i1 = lax.top_k(s1, PEER_TOPK)
    v2, i2 = lax.top_k(s2, PEER_TOPK)
    cand_s = (v1[..., :, None] + v2[..., None, :]).reshape(B, S, PEER_HEADS, PEER_TOPK * PEER_TOPK)
    cand_i = (i1[..., :, None] * PEER_NKEYS + i2[..., None, :]).reshape(B, S, PEER_HEADS, PEER_TOPK * PEER_TOPK)
    top_s, sel = lax.top_k(cand_s, PEER_TOPK)
    idx = jnp.take_along_axis(cand_i, sel, axis=-1)
    g = jax.nn.softmax(top_s, axis=-1).astype(h.dtype)
    n_chunks = (B * S) // PEER_CHUNK
    hc = h.reshape(n_chunks, PEER_CHUNK, D)
    ic = idx.reshape(n_chunks, PEER_CHUNK, PEER_HEADS, PEER_TOPK)
    gc = g.reshape(n_chunks, PEER_CHUNK, PEER_HEADS, PEER_TOPK)

    def expert_chunk(args):
        hx, ix, gx = args
        u = jnp.take(u_experts, ix, axis=0)
        act = jax.nn.gelu(jnp.einsum('chkd,cd->chk', u, hx), approximate=False) * gx
        vv = jnp.take(v_experts, ix, axis=0)
        return jnp.einsum('chk,chkd->cd', act, vv)

    y = lax.map(expert_chunk, (hc, ic, gc))
    return y.reshape(B, S, D)


def setup_inputs(seed: int = 0) -> dict:
    key = jax.random.key(seed)
    ks = jax.random.split(key, 17)
    f32 = jnp.float32
    D = D_MODEL
    nrm = lambda k, shape, s: jax.random.normal(k, shape, f32) * s
    return {
        'x': nrm(ks[0], (BATCH, SEQ, D), 1.0),
        'c': nrm(ks[1], (BATCH, D), 1.0),
        'w_ada': nrm(ks[2], (DEPTH, D, N_MOD * D), 0.5 * D ** -0.5),
        'b_ada': nrm(ks[3], (DEPTH, N_MOD * D), 0.01),
        'g_norm1': 1.0 + nrm(ks[4], (DEPTH, D), 0.02),
        'w_in': nrm(ks[5], (DEPTH, D, 3 * D_ATTN + D_POOL), D ** -0.5),
        'g_attn_head': 1.0 + nrm(ks[6], (DEPTH, N_ATTN_HEADS, HEAD_DIM), 0.02),
        'w_pool': nrm(ks[7], (DEPTH, N_POOL_GROUPS, POOL_GROUP, POOL_GROUP), POOL_GROUP ** -0.5),
        's_pool': 1.0 + nrm(ks[8], (DEPTH, D_POOL), 0.02),
        'w_out': nrm(ks[9], (DEPTH, D_MIX, D), D_MIX ** -0.5),
        'g_norm2': 1.0 + nrm(ks[10], (DEPTH, D), 0.02),
        'w_query': nrm(ks[11], (DEPTH, D, PEER_HEADS * PEER_DQ), D ** -0.5),
        'sub_keys_1': nrm(ks[12], (DEPTH, PEER_NKEYS, PEER_DHALF), PEER_DHALF ** -0.5),
        'sub_keys_2': nrm(ks[13], (DEPTH, PEER_NKEYS, PEER_DHALF), PEER_DHALF ** -0.5),
        'u_experts': nrm(ks[14], (DEPTH, PEER_NEXPERTS, D), D ** -0.5),
        'v_experts': nrm(ks[15], (DEPTH, PEER_NEXPERTS, D), 0.5),
        'g_final': 1.0 + nrm(ks[16], (D,), 0.02),
    }


def reference(x, c, w_ada, b_ada, g_norm1, w_in, g_attn_head, w_pool, s_pool, w_out,
              g_norm2, w_query, sub_keys_1, sub_keys_2, u_experts, v_experts, g_final):
    B, S, D = x.shape
    silu_c = jax.nn.silu(c)
    for l in range(DEPTH):
        mod = (jnp.einsum('bd,dm->bm', silu_c, w_ada[l]) + b_ada[l]).reshape(B, N_MOD, 1, D)
        shift1, scale1, gate1 = mod[:, 0], mod[:, 1], mod[:, 2]
        shift2, scale2, gate2 = mod[:, 3], mod[:, 4], mod[:, 5]

        h = rmsnorm(x, g_norm1[l]) * (1.0 + scale1) + shift1
        proj = jnp.einsum('bsd,de->bse', h, w_in[l])
        q = proj[..., :D_ATTN].reshape(B, S, N_ATTN_HEADS, HEAD_DIM).transpose(0, 2, 1, 3)
        k = proj[..., D_ATTN:2 * D_ATTN].reshape(B, S, N_ATTN_HEADS, HEAD_DIM).transpose(0, 2, 1, 3)
        v = proj[..., 2 * D_ATTN:3 * D_ATTN].reshape(B, S, N_ATTN_HEADS, HEAD_DIM).transpose(0, 2, 1, 3)
        p = proj[..., 3 * D_ATTN:]
        o_attn = stick_breaking_attention(q, k, v).transpose(0, 2, 1, 3)
        o_attn = rmsnorm(o_attn, g_attn_head[l]).reshape(B, S, D_ATTN)
        o_pool = multiscale_pool(p, w_pool[l], s_pool[l])
        mix = jnp.einsum('bse,ed->bsd', jnp.concatenate([o_attn, o_pool], axis=-1), w_out[l])
        x = x + gate1 * mix

        h2 = rmsnorm(x, g_norm2[l]) * (1.0 + scale2) + shift2
        x = x + gate2 * peer_ffn(h2, w_query[l], sub_keys_1[l], sub_keys_2[l], u_experts[l], v_experts[l])
    return rmsnorm(x, g_final)
```

```python
import numpy as np
from contextlib import ExitStack
import concourse.bass as bass
import concourse.mybir as mybir
from concourse.bass_utils import run_bass_kernel_spmd

F32 = mybir.dt.float32
BF16 = mybir.dt.bfloat16
AF = mybir.ActivationFunctionType
ALU = mybir.AluOpType

D = 4096
NDC = 32
T = 512
NSLOT = 8
NOWN = 4
NH = 16
EPS = 1e-6
NEB = 128
GRP = 4
NGRP = NEB // GRP
POOL_W = (2, 4, 8, 16)
NVEC = 192 + 32 * 3 + 16 + 16 + 1 + 64
OFF_BADA, OFF_G1, OFF_G2, OFF_GF, OFF_GH, OFF_SP, OFF_FLAG, OFF_INVC = 0, 192, 224, 256, 288, 304, 320, 321


class _Buf:
    def __init__(self, name):
        self.name = name
        self.last_write = None
        self.reads = {}
        self.dsem = None


_BUFS = {}


def Buf(name):
    if name not in _BUFS:
        _BUFS[name] = _Buf(name)
    return _BUFS[name]


class Sched:
    def __init__(self, nc, stack):
        self.nc = nc
        self.stack = stack
        self.eng = {"pe": nc.tensor, "act": nc.scalar, "dve": nc.vector, "pool": nc.gpsimd, "sp": nc.sync}
        self.sem = {}
        self.cnt = {}
        for e in ("pe", "act", "dve", "pool"):
            self.sem[e] = stack.enter_context(nc.semaphore("s_" + e))
            self.cnt[e] = 0
        self.waited = {e: {} for e in self.eng}
        self.dcnt = {}
        self.dsems = []

    def _wait(self, engine, ev):
        if ev[0] == "c":
            _, e, v = ev
            if e == "pe" and engine == "pe":
                return
            key = ("c", e)
            sem = self.sem[e]
        else:
            b = ev[1]
            key = ("d", b.name)
            sem = b.dsem
            v = self.dcnt[b.name]
        if self.waited[engine].get(key, 0) >= v:
            return
        self.waited[engine][key] = v
        self.eng[engine].wait_ge(sem, v)

    def _deps(self, engine, reads, writes):
        for b in reads:
            if b.last_write is not None:
                self._wait(engine, b.last_write)
        for b in writes:
            if b.last_write is not None:
                self._wait(engine, b.last_write)
            for ev in list(b.reads.values()):
                self._wait(engine, ev)

    def _commit(self, ev, reads, writes):
        key = (ev[0], ev[1]) if ev[0] == "c" else ("d", ev[1].name)
        for b in reads:
            if b not in writes:
                b.reads[key] = ev
        for b in writes:
            b.last_write = ev
            b.reads = {}

    def op(self, engine, fn, reads=(), writes=()):
        reads = list(reads)
        writes = list(writes)
        self._deps(engine, reads, writes)
        ins = fn(self.eng[engine])
        ins.then_inc(self.sem[engine], 1)
        self.cnt[engine] += 1
        self._commit(("c", engine, self.cnt[engine]), reads, writes)
        return ins

    def dma(self, queue, out, in_, owner, reads=(), writes=(), **kw):
        reads = list(reads)
        writes = list(writes)
        if owner.dsem is None:
            owner.dsem = self.stack.enter_context(self.nc.semaphore("d_" + owner.name))
            self.dcnt[owner.name] = 0
            self.dsems.append(owner)
        self._deps(queue, reads, writes)
        ins = self.eng[queue].dma_start(out=out, in_=in_, **kw)
        ins.then_inc(owner.dsem, 16)
        self.dcnt[owner.name] += 16
        self._commit(("d", owner), reads, writes)
        return ins

    def barrier(self):
        for q in ("pe", "act", "dve", "pool", "sp"):
            for e in ("pe", "act", "dve", "pool"):
                if self.cnt[e] > 0 and e != q:
                    self._wait(q, ("c", e, self.cnt[e]))
            for b in self.dsems:
                self._wait(q, ("d", b))

    def finish(self):
        for e in ("pe", "act", "dve", "pool"):
            if self.cnt[e] > 0:
                self._wait("sp", ("c", e, self.cnt[e]))
        for b in self.dsems:
            self._wait("sp", ("d", b))


def build_program(stop_after=99, debug=False):
    nc = bass.Bass("TRN2", target_bir_lowering=False)
    _BUFS.clear()
    uniq = [0]

    def din(name, shape):
        return nc.dram_tensor(name, shape, F32, kind="ExternalInput").ap()

    def dscr(name, shape, dt=BF16):
        kind = "ExternalOutput" if debug else "Internal"
        return nc.dram_tensor(name, shape, dt, kind=kind).ap()

    xT = din("xT", [NSLOT, 128, NDC, T])
    cT = din("cT", [128, NDC])
    w_ada = din("w_ada", [D, 6 * D])
    vecs = din("vecs", [128, NVEC])
    consts = din("consts", [128, 5, 128])
    w_in = din("w_in", [D, 2 * D])
    w_pool = din("w_pool", [4, 512, 512])
    w_out = din("w_out", [D, D])
    w_query = din("w_query", [D, 2048])
    skT = din("skT", [128, 2, 128])
    uT = din("uT", [NEB, 128, NDC * 128])
    vE = din("vE", [NEB * 128, D])
    outT = nc.dram_tensor("outT", [NOWN, 128, NDC, T], F32, kind="ExternalOutput").ap()

    hT_s = dscr("hT_s", [NSLOT, 128, NDC, T])
    QT_s = dscr("QT_s", [NH, 128, NOWN * T])
    KT_s = dscr("KT_s", [NH, 128, NSLOT * T])
    V_s = dscr("V_s", [NSLOT * 4, 128, 2048])
    oT_s = dscr("oT_s", [NOWN, 128, NDC, T])
    if debug:
        dbg_mod = nc.dram_tensor("dbg_mod", [128, 192], F32, kind="ExternalOutput").ap()
        dbg_x1 = nc.dram_tensor("dbg_x1", [NOWN, 128, NDC, T], F32, kind="ExternalOutput").ap()
        dbg_h2 = nc.dram_tensor("dbg_h2", [NOWN, 128, NDC, T], BF16, kind="ExternalOutput").ap()
        dbg_s12 = nc.dram_tensor("dbg_s12", [NOWN, 128, 4, 8, 256], F32, kind="ExternalOutput").ap()
        dbg_td = nc.dram_tensor("dbg_td", [NOWN, 128, 4, 8, 2], F32, kind="ExternalOutput").ap()
    BhT_s, BQT_s, BKT_s, BV_s, BoT_s, Bout = [Buf(n) for n in ("hT_s", "QT_s", "KT_s", "V_s", "oT_s", "outd")]
    Bdbg = Buf("dbg")

    with ExitStack() as gs:
        S = Sched(nc, gs)

        def sb(st, name, shape, dt):
            uniq[0] += 1
            return st.enter_context(nc.sbuf_tensor(f"{name}_{uniq[0]}", shape, dt))

        ps = [gs.enter_context(nc.psum_tensor(f"ps{i}", [128, 512], F32)) for i in range(8)]
        P = [Buf(f"ps{i}") for i in range(8)]

        cst = sb(gs, "cst", [128, 5, 128], BF16)
        Bcst = Buf("cst")
        S.dma("pool", cst[:], consts, Bcst, writes=[Bcst])
        ones_bf, negones_bf, ident_bf, negU_bf, tri_bf = [cst[:, i, :] for i in range(5)]
        vec = sb(gs, "vec", [128, NVEC], F32)
        Bvec = Buf("vec")
        S.dma("sp", vec[:], vecs, Bvec, writes=[Bvec])
        modv = sb(gs, "modv", [128, 192], F32)
        A12 = sb(gs, "A12", [128, 64], F32)
        Bmod = Buf("modv")
        BA12 = Buf("A12")
        flag = vec[:, OFF_FLAG:OFF_FLAG + 1]

        with ExitStack() as ph:
            cT_sb = sb(ph, "cT_sb", [128, NDC], F32)
            sc = sb(ph, "sc", [128, NDC], BF16)
            wa = [sb(ph, f"wa{i}", [128, NDC, 512], BF16) for i in range(2)]
            Bc, Bsc = Buf("cT_sb"), Buf("sc")
            Bwa = [Buf("wa0"), Buf("wa1")]
            S.dma("sp", cT_sb[:], cT, Bc, writes=[Bc])
            S.op("act", lambda e: e.activation(out=sc[:], in_=cT_sb[:], func=AF.Silu), reads=[Bc], writes=[Bsc])
            for ct in range(48):
                k = ct % 2
                S.dma("pool", wa[k][:], w_ada[:, ct * 512:(ct + 1) * 512].rearrange("(dc p) n -> p dc n", p=128),
                      Bwa[k], writes=[Bwa[k]], max_dma_last_dim=8192)
                for mm in range(4):
                    j = ct * 4 + mm
                    for dc in range(NDC):
                        S.op("pe", lambda e: e.matmul(ps[0][:, j:j + 1], lhsT=wa[k][:, dc, mm * 128:(mm + 1) * 128],
                                                      rhs=sc[:, dc:dc + 1], start=(dc == 0), stop=(dc == NDC - 1)),
                             reads=[Bwa[k], Bsc], writes=[P[0]])
            S.op("dve", lambda e: e.tensor_tensor(out=modv[:], in0=ps[0][:, 0:192], in1=vec[:, OFF_BADA:OFF_BADA + 192],
                                                  op=ALU.add), reads=[P[0], Bvec], writes=[Bmod])
            S.op("dve", lambda e: e.scalar_tensor_tensor(out=A12[:, 0:32], in0=modv[:, 32:64], scalar=1.0, op0=ALU.add,
                                                         in1=vec[:, OFF_G1:OFF_G1 + 32], op1=ALU.mult),
                 reads=[Bmod, Bvec], writes=[BA12])
            S.op("dve", lambda e: e.scalar_tensor_tensor(out=A12[:, 32:64], in0=modv[:, 128:160], scalar=1.0, op0=ALU.add,
                                                         in1=vec[:, OFF_G2:OFF_G2 + 32], op1=ALU.mult),
                 reads=[Bmod, Bvec], writes=[BA12])
            if debug:
                S.dma("sp", dbg_mod, modv[:], Bmod, reads=[Bmod], writes=[Bdbg])
            S.barrier()
        A1 = A12[:, 0:32]
        A2 = A12[:, 32:64]
        B1 = modv[:, 0:32]
        gate1 = modv[:, 64:96]
        B2 = modv[:, 96:128]
        gate2 = modv[:, 160:192]
        Bmv = [Bmod, BA12, Bvec]

        def emit_rstd(st_bufs, Xt, BXs, rstd, Brstd, bank, n_part_inv):
            sq, Bsq = st_bufs
            for dc in range(NDC):
                k = dc % 2
                S.op("act", lambda e: e.activation(out=sq[k][:], in_=Xt[:, dc, :], func=AF.Square),
                     reads=[BXs[dc // 8]], writes=[Bsq[k]])
                S.op("pe", lambda e: e.matmul(ps[bank][:], lhsT=ones_bf, rhs=sq[k][:], start=(dc == 0), stop=(dc == NDC - 1)),
                     reads=[Bsq[k], Bcst], writes=[P[bank]])
            S.op("act", lambda e: e.activation(out=rstd[:], in_=ps[bank][:], func=AF.Sqrt, scale=n_part_inv, bias=EPS),
                 reads=[P[bank]], writes=[Brstd])
            S.op("dve", lambda e: e.reciprocal(out=rstd[:], in_=rstd[:]), reads=[Brstd], writes=[Brstd])

        def emit_norm_mod(Xt, BXs, rstd, Brstd, Acol, Bcol, tmp, Btmp, hT, BhT):
            for dc in range(NDC):
                k = dc % 2
                S.op("dve", lambda e: e.scalar_tensor_tensor(out=tmp[k][:], in0=Xt[:, dc, :], scalar=Acol[:, dc:dc + 1],
                                                             op0=ALU.mult, in1=rstd[:], op1=ALU.mult),
                     reads=[BXs[dc // 8], Brstd] + Bmv, writes=[Btmp[k]])
                S.op("act", lambda e: e.activation(out=hT[:, dc, :], in_=tmp[k][:], func=AF.Identity,
                                                   bias=Bcol[:, dc:dc + 1]),
                     reads=[Btmp[k]] + Bmv, writes=[BhT])

        if stop_after >= 1:
            with ExitStack() as ph:
                X = sb(ph, "X1", [128, NDC, T], F32)
                BX = [Buf(f"X1_{i}") for i in range(4)]
                sq = [sb(ph, f"sq{i}", [128, T], BF16) for i in range(2)]
                Bsq = [Buf("sq0"), Buf("sq1")]
                rstd = sb(ph, "rstd", [128, T], F32)
                Brstd = Buf("rstd")
                tmp = [sb(ph, f"tmp{i}", [128, T], F32) for i in range(2)]
                Btmp = [Buf("tmp0"), Buf("tmp1")]
                hT = [sb(ph, f"hT{i}", [128, NDC, T], BF16) for i in range(2)]
                BhT = [Buf("hT0"), Buf("hT1")]
                for s in range(NSLOT):
                    for qd in range(4):
                        S.dma("sp", X[:, qd * 8:(qd + 1) * 8, :], xT[s, :, qd * 8:(qd + 1) * 8, :], BX[qd], writes=[BX[qd]])
                    emit_rstd((sq, Bsq), X, BX, rstd, Brstd, s % 2, 1.0 / D)
                    emit_norm_mod(X, BX, rstd, Brstd, A1, B1, tmp, Btmp, hT[s % 2], BhT[s % 2])
                    S.dma("act", hT_s[s], hT[s % 2][:], BhT[s % 2], reads=[BhT[s % 2]], writes=[BhT_s])
                S.barrier()

        if stop_after >= 2:
            with ExitStack() as ph:
                Wg = [sb(ph, f"Wg{i}", [128, NDC, 512], BF16) for i in range(2)]
                BWg = [Buf("Wg0"), Buf("Wg1")]
                hTt = [sb(ph, f"hTt{i}", [128, NDC, T], BF16) for i in range(2)]
                BhTt = [Buf("hTt0"), Buf("hTt1")]
                stg = [sb(ph, f"stg{i}", [128, 512], BF16) for i in range(4)]
                Bstg = [Buf(f"stg{i}") for i in range(4)]
                Pb = [sb(ph, f"Pb{i}", [128, 528], F32) for i in range(4)]
                BPb = [Buf(f"Pb{i}") for i in range(4)]
                T1 = sb(ph, "T1", [128, 528], F32)
                T2 = sb(ph, "T2", [128, 528], F32)
                BT1, BT2 = Buf("T1"), Buf("T2")
                dT = [sb(ph, f"dT{i}", [128, 512], BF16) for i in range(4)]
                BdT = [Buf(f"dT{i}") for i in range(4)]
                d16 = sb(ph, "d16", [128, 16], F32)
                Bd16 = Buf("d16")
                hh = sb(ph, "hh", [128, NDC, 16], BF16)
                Bhh = Buf("hh")
                wp = sb(ph, "wp", [128, 4, 512], BF16)
                Bwp = Buf("wp")
                nload = [0]
                nps = [0]
                nst = [0]

                def load_h(slot):
                    k = nload[0] % 2
                    nload[0] += 1
                    S.dma("sp", hTt[k][:], hT_s[slot], BhTt[k], reads=[BhT_s], writes=[BhTt[k]])
                    return k

                for cg in range(16):
                    kw = cg % 2
                    S.dma("pool", Wg[kw][:], w_in[:, cg * 512:(cg + 1) * 512].rearrange("(dc p) n -> p dc n", p=128),
                          BWg[kw], writes=[BWg[kw]], max_dma_last_dim=8192)
                    kind = cg // 4
                    sub = cg % 4
                    if kind in (0, 1):
                        slots = [1, 3, 5, 7] if kind == 0 else list(range(NSLOT))
                        for si, slot in enumerate(slots):
                            kh = load_h(slot)
                            for hx in range(4):
                                head = sub * 4 + hx
                                r = nps[0] % 4
                                nps[0] += 1
                                for dc in range(NDC):
                                    S.op("pe", lambda e: e.matmul(ps[r][:], lhsT=Wg[kw][:, dc, hx * 128:(hx + 1) * 128],
                                                                  rhs=hTt[kh][:, dc, :], start=(dc == 0), stop=(dc == NDC - 1)),
                                         reads=[BWg[kw], BhTt[kh]], writes=[P[r]])
                                q = nst[0] % 4
                                nst[0] += 1
                                sc_ = (128.0 ** -0.5) if kind == 0 else 1.0
                                S.op("act", lambda e: e.activation(out=stg[q][:], in_=ps[r][:], func=AF.Copy, scale=sc_),
                                     reads=[P[r]], writes=[Bstg[q]])
                                if kind == 0:
                                    S.dma("act", QT_s[head, :, si * T:(si + 1) * T], stg[q][:], Bstg[q], reads=[Bstg[q]], writes=[BQT_s])
                                else:
                                    S.dma("act", KT_s[head, :, slot * T:(slot + 1) * T], stg[q][:], Bstg[q], reads=[Bstg[q]], writes=[BKT_s])
                    elif kind == 2:
                        for slot in range(NSLOT):
                            kh = load_h(slot)
                            for tb in range(4):
                                r = nps[0] % 4
                                nps[0] += 1
                                for dc in range(NDC):
                                    S.op("pe", lambda e: e.matmul(ps[r][:], lhsT=hTt[kh][:, dc, tb * 128:(tb + 1) * 128],
                                                                  rhs=Wg[kw][:, dc, :], start=(dc == 0), stop=(dc == NDC - 1)),
                                         reads=[BWg[kw], BhTt[kh]], writes=[P[r]])
                                q = nst[0] % 4
                                nst[0] += 1
                                if slot == 0:
                                    S.op("act", lambda e: e.activation(out=stg[q][:], in_=ps[r][:], func=AF.Identity, scale=flag),
                                         reads=[P[r], Bvec], writes=[Bstg[q]])
                                else:
                                    S.op("act", lambda e: e.activation(out=stg[q][:], in_=ps[r][:], func=AF.Copy),
                                         reads=[P[r]], writes=[Bstg[q]])
                                S.dma("act", V_s[slot * 4 + tb, :, sub * 512:(sub + 1) * 512], stg[q][:], Bstg[q],
                                      reads=[Bstg[q]], writes=[BV_s])
                    else:
                        g = sub
                        wdw = POOL_W[g]
                        S.dma("pool", wp[:], w_pool[g].rearrange("(cc p) n -> p cc n", p=128), Bwp, writes=[Bwp])
                        for j in range(NOWN):
                            S.dma("sp", hh[:], hT_s[2 * j, :, :, T - 16:T], Bhh, reads=[BhT_s], writes=[Bhh])
                            kh = load_h(2 * j + 1)
                            for cc in range(4):
                                r = nps[0] % 4
                                nps[0] += 1
                                for dc in range(NDC):
                                    S.op("pe", lambda e: e.matmul(ps[r][:, 0:16], lhsT=Wg[kw][:, dc, cc * 128:(cc + 1) * 128],
                                                                  rhs=hh[:, dc, :], start=(dc == 0), stop=(dc == NDC - 1)),
                                         reads=[BWg[kw], Bhh], writes=[P[r]])
                                if j == 0:
                                    S.op("act", lambda e: e.activation(out=Pb[cc][:, 0:16], in_=ps[r][:, 0:16], func=AF.Identity, scale=flag),
                                         reads=[P[r], Bvec], writes=[BPb[cc]])
                                else:
                                    S.op("act", lambda e: e.activation(out=Pb[cc][:, 0:16], in_=ps[r][:, 0:16], func=AF.Copy),
                                         reads=[P[r]], writes=[BPb[cc]])
                                r = nps[0] % 4
                                nps[0] += 1
                                for dc in range(NDC):
                                    S.op("pe", lambda e: e.matmul(ps[r][:], lhsT=Wg[kw][:, dc, cc * 128:(cc + 1) * 128],
                                                                  rhs=hTt[kh][:, dc, :], start=(dc == 0), stop=(dc == NDC - 1)),
                                         reads=[BWg[kw], BhTt[kh]], writes=[P[r]])
                                S.op("act", lambda e: e.activation(out=Pb[cc][:, 16:528], in_=ps[r][:], func=AF.Copy),
                                     reads=[P[r]], writes=[BPb[cc]])
                                S.op("dve", lambda e: e.tensor_tensor(out=T1[:, 1:528], in0=Pb[cc][:, 1:528], in1=Pb[cc][:, 0:527], op=ALU.add),
                                     reads=[BPb[cc]], writes=[BT1])
                                ws, Bws = T1, BT1
                                if wdw >= 4:
                                    S.op("dve", lambda e: e.tensor_tensor(out=T2[:, 3:528], in0=T1[:, 3:528], in1=T1[:, 1:526], op=ALU.add),
                                         reads=[BT1], writes=[BT2])
                                    ws, Bws = T2, BT2
                                if wdw >= 8:
                                    S.op("dve", lambda e: e.tensor_tensor(out=T1[:, 7:528], in0=T2[:, 7:528], in1=T2[:, 3:524], op=ALU.add),
                                         reads=[BT2], writes=[BT1])
                                    ws, Bws = T1, BT1
                                if wdw >= 16:
                                    S.op("dve", lambda e: e.tensor_tensor(out=T2[:, 15:528], in0=T1[:, 15:528], in1=T1[:, 7:520], op=ALU.add),
                                         reads=[BT1], writes=[BT2])
                                    ws, Bws = T2, BT2
                                S.op("dve", lambda e: e.scalar_tensor_tensor(out=dT[cc][:], in0=ws[:, 16:528], scalar=1.0 / wdw, op0=ALU.mult,
                                                                             in1=Pb[cc][:, 16:528], op1=ALU.subtract),
                                     reads=[Bws, BPb[cc]], writes=[BdT[cc]])
                                if j == 0:
                                    S.op("dve", lambda e: e.tensor_tensor(out=d16[:], in0=ws[:, 16:32],
                                                                          in1=vec[:, OFF_INVC + g * 16:OFF_INVC + (g + 1) * 16], op=ALU.mult),
                                         reads=[Bws, Bvec], writes=[Bd16])
                                    S.op("dve", lambda e: e.tensor_tensor(out=dT[cc][:, 0:16], in0=d16[:], in1=Pb[cc][:, 16:32], op=ALU.subtract),
                                         reads=[Bd16, BPb[cc]], writes=[BdT[cc]])
                            for dd in range(4):
                                r = nps[0] % 4
                                nps[0] += 1
                                for cc in range(4):
                                    S.op("pe", lambda e: e.matmul(ps[r][:], lhsT=wp[:, cc, dd * 128:(dd + 1) * 128], rhs=dT[cc][:],
                                                                  start=(cc == 0), stop=(cc == 3)),
                                         reads=[Bwp, BdT[cc]], writes=[P[r]])
                                q = nst[0] % 4
                                nst[0] += 1
                                col = OFF_SP + g * 4 + dd
                                S.op("act", lambda e: e.activation(out=stg[q][:], in_=ps[r][:], func=AF.Identity, scale=vec[:, col:col + 1]),
                                     reads=[P[r], Bvec], writes=[Bstg[q]])
                                S.dma("act", oT_s[j, :, 16 + g * 4 + dd, :], stg[q][:], Bstg[q], reads=[Bstg[q]], writes=[BoT_s])
                S.barrier()

        if stop_after >= 3:
            with ExitStack() as ph:
                KTh = [sb(ph, f"KTh{i}", [128, NSLOT * T], BF16) for i in range(2)]
                Vh = [sb(ph, f"Vh{i}", [128, NSLOT * 4, 128], BF16) for i in range(2)]
                QTh = [sb(ph, f"QTh{i}", [128, NOWN * T], BF16) for i in range(2)]
                BKTh = [Buf("KTh0"), Buf("KTh1")]
                BVh = [Buf("Vh0"), Buf("Vh1")]
                BQTh = [Buf("QTh0"), Buf("QTh1")]
                E = [sb(ph, f"E{i}", [128, T], F32) for i in range(2)]
                BE = [Buf("E0"), Buf("E1")]
                Lp = [sb(ph, f"Lp{i}", [128, T], BF16) for i in range(3)]
                BLp = [Buf(f"Lp{i}") for i in range(3)]
                Ls = sb(ph, "Ls", [128, T], BF16)
                BLs = Buf("Ls")
                Aa = [sb(ph, f"Aa{i}", [128, T], BF16) for i in range(2)]
                BAa = [Buf("Aa0"), Buf("Aa1")]
                sqa = sb(ph, "sqa", [128, T], BF16)
                Bsqa = Buf("sqa")
                rsa = sb(ph, "rsa", [128, T], F32)
                Brsa = Buf("rsa")
                ost = [sb(ph, f"ost{i}", [128, T], BF16) for i in range(2)]
                Bost = [Buf("ost0"), Buf("ost1")]

                units = []
                for h in range(NH):
                    for j in range(NOWN):
                        qs = 2 * j + 1
                        kbs = list(range(qs * 4 + 3, -1, -1))
                        for ui, kb in enumerate(kbs):
                            c0 = (kb - qs * 4) * 128 if kb >= qs * 4 else 0
                            units.append(dict(h=h, j=j, kb=kb, c0=c0, first=(ui == 0), last=(ui == len(kbs) - 1),
                                              diag=(kb >= qs * 4)))

                def load_head(h):
                    k = h % 2
                    S.dma("sp", KTh[k][:], KT_s[h], BKTh[k], reads=[BKT_s], writes=[BKTh[k]])
                    S.dma("sp", QTh[k][:], QT_s[h], BQTh[k], reads=[BQT_s], writes=[BQTh[k]])
                    S.dma("sp", Vh[k][:], V_s[:, :, h * 128:(h + 1) * 128].rearrange("tb p d -> p tb d"), BVh[k],
                          reads=[BV_s], writes=[BVh[k]])

                def stage1(i, u):
                    hk = u["h"] % 2
                    zb = i % 3
                    c0 = u["c0"]
                    qcol = u["j"] * T
                    S.op("pe", lambda e: e.matmul(ps[zb][:, c0:T], lhsT=KTh[hk][:, u["kb"] * 128:(u["kb"] + 1) * 128],
                                                  rhs=QTh[hk][:, qcol + c0:qcol + T], start=True, stop=True),
                         reads=[BKTh[hk], BQTh[hk]], writes=[P[zb]])
                    S.op("act", lambda e: e.activation(out=E[i % 2][:, c0:T], in_=ps[zb][:, c0:T], func=AF.Exp),
                         reads=[P[zb]], writes=[BE[i % 2]])
                    S.op("act", lambda e: e.activation(out=Lp[i % 3][:, c0:T], in_=E[i % 2][:, c0:T], func=AF.Ln, bias=1.0),
                         reads=[BE[i % 2]], writes=[BLp[i % 3]])
                    if u["diag"]:
                        S.op("dve", lambda e: e.tensor_tensor(out=Lp[i % 3][:, c0:c0 + 128], in0=Lp[i % 3][:, c0:c0 + 128],
                                                              in1=tri_bf, op=ALU.mult),
                             reads=[BLp[i % 3], Bcst], writes=[BLp[i % 3]])

                def stage2(i, u):
                    zb = i % 3
                    c0 = u["c0"]
                    S.op("pe", lambda e: e.matmul(ps[zb][:, c0:T], lhsT=negU_bf, rhs=Lp[i % 3][:, c0:T], start=False, stop=u["first"],
                                                  skip_group_check=True),
                         reads=[BLp[i % 3], Bcst], writes=[P[zb]])
                    if u["first"]:
                        S.op("pool", lambda e: e.memset(Ls[:], 0.0), writes=[BLs])
                    else:
                        S.op("pe", lambda e: e.matmul(ps[zb][:, c0:T], lhsT=negones_bf, rhs=Ls[:, c0:T], start=False, stop=True,
                                                      skip_group_check=True),
                             reads=[BLs, Bcst], writes=[P[zb]])
                    if not u["last"]:
                        S.op("pool", lambda e: e.tensor_tensor(out=Ls[:, c0:T], in0=Ls[:, c0:T], in1=Lp[i % 3][:, c0:T], op=ALU.add),
                             reads=[BLs, BLp[i % 3]], writes=[BLs])

                def stage3(i, u):
                    hk = u["h"] % 2
                    zb = i % 3
                    c0 = u["c0"]
                    ob = 3 + (u["h"] * NOWN + u["j"]) % 2
                    S.op("act", lambda e: e.activation(out=Aa[i % 2][:, c0:T], in_=ps[zb][:, c0:T], func=AF.Exp),
                         reads=[P[zb]], writes=[BAa[i % 2]])
                    if u["diag"]:
                        S.op("dve", lambda e: e.tensor_tensor(out=Aa[i % 2][:, c0:c0 + 128], in0=Aa[i % 2][:, c0:c0 + 128],
                                                              in1=tri_bf, op=ALU.mult),
                             reads=[BAa[i % 2], Bcst], writes=[BAa[i % 2]])
                    S.op("pe", lambda e: e.matmul(ps[ob][:, c0:T], lhsT=Vh[hk][:, u["kb"], :], rhs=Aa[i % 2][:, c0:T],
                                                  start=u["first"], stop=u["last"], skip_group_check=True),
                         reads=[BVh[hk], BAa[i % 2]], writes=[P[ob]])
                    if u["last"]:
                        h, j = u["h"], u["j"]
                        k = (h * NOWN + j) % 2
                        S.op("act", lambda e: e.activation(out=sqa[:], in_=ps[ob][:], func=AF.Square), reads=[P[ob]], writes=[Bsqa])
                        S.op("pe", lambda e: e.matmul(ps[5][:], lhsT=ones_bf, rhs=sqa[:], start=True, stop=True),
                             reads=[Bsqa, Bcst], writes=[P[5]])
                        S.op("act", lambda e: e.activation(out=rsa[:], in_=ps[5][:], func=AF.Sqrt, scale=1.0 / 128, bias=EPS),
                             reads=[P[5]], writes=[Brsa])
                        S.op("dve", lambda e: e.reciprocal(out=rsa[:], in_=rsa[:]), reads=[Brsa], writes=[Brsa])
                        S.op("dve", lambda e: e.scalar_tensor_tensor(out=ost[k][:], in0=ps[ob][:], scalar=vec[:, OFF_GH + h:OFF_GH + h + 1],
                                                                     op0=ALU.mult, in1=rsa[:], op1=ALU.mult),
                             reads=[P[ob], Brsa, Bvec], writes=[Bost[k]])
                        S.dma("sp", oT_s[j, :, h, :], ost[k][:], Bost[k], reads=[Bost[k]], writes=[BoT_s])

                load_head(0)
                n = len(units)
                for i in range(n + 2):
                    if i < n:
                        u = units[i]
                        if u["first"] and u["j"] == 0 and u["h"] + 1 < NH:
                            load_head(u["h"] + 1)
                        stage1(i, u)
                    if 0 <= i - 1 < n:
                        stage2(i - 1, units[i - 1])
                    if 0 <= i - 2 < n:
                        stage3(i - 2, units[i - 2])
                S.barrier()

        if stop_after >= 4:
            with ExitStack() as ph:
                X = sb(ph, "X2", [128, NDC, T], F32)
                BX = [Buf(f"X2_{i}") for i in range(4)]
                h2T = sb(ph, "h2T", [128, NDC, T], BF16)
                Bh2T = Buf("h2T")
                S12 = sb(ph, "S12", [128, 4, 8, 256], F32)
                BS12 = Buf("S12")
                TD = sb(ph, "TD", [128, 4, 8, 2], F32)
                BTD = Buf("TD")
                sq = [sb(ph, f"sqb{i}", [128, T], BF16) for i in range(2)]
                Bsq = [Buf("sqb0"), Buf("sqb1")]
                rstd = sb(ph, "rstd2", [128, T], F32)
                Brstd = Buf("rstd2")
                skb = sb(ph, "skb", [128, 2, 128], BF16)
                Bskb = Buf("skb")
                S.dma("pool", skb[:], skT, Bskb, writes=[Bskb])
                for j in range(NOWN):
                    with ExitStack() as p4:
                        oTt = sb(p4, "oTt", [128, NDC, T], BF16)
                        BoTt = Buf("oTt")
                        Wo = [sb(p4, f"Wo{i}", [128, NDC, 256], BF16) for i in range(2)]
                        BWo = [Buf("Wo0"), Buf("Wo1")]
                        for qd in range(4):
                            S.dma("sp", X[:, qd * 8:(qd + 1) * 8, :], xT[2 * j + 1, :, qd * 8:(qd + 1) * 8, :], BX[qd], writes=[BX[qd]])
                        S.dma("sp", oTt[:], oT_s[j], BoTt, reads=[BoT_s], writes=[BoTt])
                        for cg in range(16):
                            k = cg % 2
                            S.dma("pool", Wo[k][:], w_out[:, cg * 256:(cg + 1) * 256].rearrange("(ec p) n -> p ec n", p=128),
                                  BWo[k], writes=[BWo[k]])
                            for dd in range(2):
                                dch = cg * 2 + dd
                                r = dch % 4
                                for ec in range(NDC):
                                    S.op("pe", lambda e: e.matmul(ps[r][:], lhsT=Wo[k][:, ec, dd * 128:(dd + 1) * 128], rhs=oTt[:, ec, :],
                                                                  start=(ec == 0), stop=(ec == NDC - 1)),
                                         reads=[BWo[k], BoTt], writes=[P[r]])
                                S.op("dve", lambda e: e.scalar_tensor_tensor(out=X[:, dch, :], in0=ps[r][:], scalar=gate1[:, dch:dch + 1],
                                                                             op0=ALU.mult, in1=X[:, dch, :], op1=ALU.add),
                                     reads=[P[r], BX[dch // 8]] + Bmv, writes=[BX[dch // 8]])
                        if debug:
                            S.dma("sp", dbg_x1[j], X[:], BX[0], reads=BX, writes=[Bdbg])
                        S.barrier()
                    if stop_after < 5:
                        continue
                    with ExitStack() as p5:
                        tmp = [sb(p5, f"tmpb{i}", [128, T], F32) for i in range(2)]
                        Btmp = [Buf("tmpb0"), Buf("tmpb1")]
                        Wq = [sb(p5, f"Wq{i}", [128, NDC, 256], BF16) for i in range(2)]
                        BWq = [Buf("Wq0"), Buf("Wq1")]
                        qT = [sb(p5, f"qT{i}", [128, 2, T], BF16) for i in range(2)]
                        BqT = [Buf("qT0"), Buf("qT1")]
                        v16 = sb(p5, "v16", [128, 2, 16], F32)
                        Bv16 = Buf("v16")
                        tmpk = sb(p5, "tmpk", [128, 128], F32)
                        Btmpk = Buf("tmpk")
                        cand = sb(p5, "cand", [128, 256], F32)
                        cand2 = sb(p5, "cand2", [128, 256], F32)
                        Bcand, Bcand2 = Buf("cand"), Buf("cand2")
                        c16 = sb(p5, "c16", [128, 16], F32)
                        Bc16 = Buf("c16")
                        e16 = sb(p5, "e16", [128, 16], F32)
                        Be16 = Buf("e16")
                        sm = sb(p5, "sm", [128, 4], F32)
                        Bsm = Buf("sm")
                        emit_rstd((sq, Bsq), X, BX, rstd, Brstd, 4, 1.0 / D)
                        emit_norm_mod(X, BX, rstd, Brstd, A2, B2, tmp, Btmp, h2T, Bh2T)
                        if debug:
                            S.dma("sp", dbg_h2[j], h2T[:], Bh2T, reads=[Bh2T], writes=[Bdbg])
                        for hq in range(8):
                            k = hq % 2
                            S.dma("pool", Wq[k][:], w_query[:, hq * 256:(hq + 1) * 256].rearrange("(dc p) n -> p dc n", p=128),
                                  BWq[k], writes=[BWq[k]])
                            for cc in range(2):
                                r = cc
                                for dc in range(NDC):
                                    S.op("pe", lambda e: e.matmul(ps[r][:], lhsT=Wq[k][:, dc, cc * 128:(cc + 1) * 128], rhs=h2T[:, dc, :],
                                                                  start=(dc == 0), stop=(dc == NDC - 1)),
                                         reads=[BWq[k], Bh2T], writes=[P[r]])
                                S.op("act", lambda e: e.activation(out=qT[k][:, cc, :], in_=ps[r][:], func=AF.Copy),
                                     reads=[P[r]], writes=[BqT[k]])
                            for tb in range(4):
                                r = 2 + tb % 2
                                for w_ in range(2):
                                    S.op("pe", lambda e: e.matmul(ps[r][:, w_ * 128:(w_ + 1) * 128], lhsT=qT[k][:, w_, tb * 128:(tb + 1) * 128],
                                                                  rhs=skb[:, w_, :], start=True, stop=True, skip_group_check=True),
                                         reads=[BqT[k], Bskb], writes=[P[r]])
                                S.op("act", lambda e: e.activation(out=S12[:, tb, hq, :], in_=ps[r][:, 0:256], func=AF.Copy),
                                     reads=[P[r]], writes=[BS12])
                                for w_ in range(2):
                                    src = S12[:, tb, hq, w_ * 128:(w_ + 1) * 128]
                                    S.op("dve", lambda e: e.max(out=v16[:, w_, 0:8], in_=src), reads=[BS12], writes=[Bv16])
                                    S.op("dve", lambda e: e.match_replace(out=tmpk[:], in_to_replace=v16[:, w_, 0:8], in_values=src,
                                                                          imm_value=-1e30), reads=[BS12, Bv16], writes=[Btmpk])
                                    S.op("dve", lambda e: e.max(out=v16[:, w_, 8:16], in_=tmpk[:]), reads=[Btmpk], writes=[Bv16])
                                S.op("pool", lambda e: e.tensor_tensor(out=cand[:].rearrange("p (a b) -> p a b", a=16),
                                                                       in0=v16[:, 0, :].unsqueeze(2).broadcast_to([128, 16, 16]),
                                                                       in1=v16[:, 1, :].unsqueeze(1).broadcast_to([128, 16, 16]), op=ALU.add),
                                     reads=[Bv16], writes=[Bcand])
                                S.op("dve", lambda e: e.max(out=c16[:, 0:8], in_=cand[:]), reads=[Bcand], writes=[Bc16])
                                S.op("dve", lambda e: e.match_replace(out=cand2[:], in_to_replace=c16[:, 0:8], in_values=cand[:],
                                                                      imm_value=-1e30), reads=[Bcand, Bc16], writes=[Bcand2])
                                S.op("dve", lambda e: e.max(out=c16[:, 8:16], in_=cand2[:]), reads=[Bcand2], writes=[Bc16])
                                S.op("dve", lambda e: e.tensor_scalar(out=sm[:, 0:1], in0=c16[:, 0:1], scalar1=-1.0, scalar2=None, op0=ALU.mult),
                                     reads=[Bc16], writes=[Bsm])
                                S.op("act", lambda e: e.activation(out=e16[:], in_=c16[:], func=AF.Exp, bias=sm[:, 0:1], accum_out=sm[:, 1:2]),
                                     reads=[Bc16, Bsm], writes=[Be16, Bsm])
                                S.op("act", lambda e: e.activation(out=sm[:, 2:3], in_=sm[:, 1:2], func=AF.Ln), reads=[Bsm], writes=[Bsm])
                                S.op("dve", lambda e: e.tensor_tensor(out=TD[:, tb, hq, 1:2], in0=sm[:, 0:1], in1=sm[:, 2:3], op=ALU.subtract),
                                     reads=[Bsm], writes=[BTD])
                                S.op("dve", lambda e: e.tensor_copy(out=TD[:, tb, hq, 0:1], in_=c16[:, 15:16]), reads=[Bc16], writes=[BTD])
                        if debug:
                            S.dma("sp", dbg_s12[j], S12[:], BS12, reads=[BS12], writes=[Bdbg])
                            S.dma("sp", dbg_td[j], TD[:], BTD, reads=[BTD], writes=[Bdbg])
                        S.barrier()
                    if stop_after < 6:
                        continue
                    with ExitStack() as p6:
                        ub = [sb(p6, f"ub{i}", [128, NDC, 128], BF16) for i in range(2)]
                        Bub = [Buf("ub0"), Buf("ub1")]
                        vb = [sb(p6, f"vb{i}", [128, GRP, 2048], BF16) for i in range(2)]
                        Bvb = [Buf("vb0"), Buf("vb1")]
                        actT = [sb(p6, f"actT{i}", [128, T], BF16) for i in range(GRP)]
                        BactT = [Buf(f"actT{i}") for i in range(GRP)]
                        Cb = [sb(p6, f"Cb{i}", [128, GRP * 128], F32) for i in range(2)]
                        BCb = [Buf("Cb0"), Buf("Cb1")]
                        Gx = [sb(p6, f"Gx{i}", [128, GRP * 128], BF16) for i in range(2)]
                        BGx = [Buf("Gx0"), Buf("Gx1")]
                        Gm = [sb(p6, f"Gm{i}", [128, GRP * 128], BF16) for i in range(2)]
                        BGm = [Buf("Gm0"), Buf("Gm1")]
                        gl = [sb(p6, f"gl{i}", [128, T], BF16) for i in range(2)]
                        Bgl = [Buf("gl0"), Buf("gl1")]
                        WTB = [0, 1, 2, 3]
                        SB_ = [4, 5]
                        YB = [6, 7]
                        cnt = dict(w=0, u=0, s=0, y=0)

                        def wbuild_pair(g, pi):
                            tb, hq = pi // 8, pi % 8
                            k = cnt["w"] % 2
                            cnt["w"] += 1
                            i0 = g * GRP
                            S.op("pool", lambda e: e.tensor_tensor(
                                out=Cb[k][:].rearrange("p (a b) -> p a b", a=GRP),
                                in0=S12[:, tb, hq, i0:i0 + GRP].unsqueeze(2).broadcast_to([128, GRP, 128]),
                                in1=S12[:, tb, hq, 128:256].unsqueeze(1).broadcast_to([128, GRP, 128]), op=ALU.add),
                                reads=[BS12], writes=[BCb[k]])
                            S.op("act", lambda e: e.activation(out=Gx[k][:], in_=Cb[k][:], func=AF.Exp, bias=TD[:, tb, hq, 1:2]),
                                 reads=[BCb[k], BTD], writes=[BGx[k]])
                            S.op("dve", lambda e: e.scalar_tensor_tensor(out=Gm[k][:], in0=Cb[k][:], scalar=TD[:, tb, hq, 0:1], op0=ALU.is_ge,
                                                                         in1=Gx[k][:], op1=ALU.mult),
                                 reads=[BCb[k], BGx[k], BTD], writes=[BGm[k]])
                            for a in range(GRP):
                                S.op("pe", lambda e: e.matmul(ps[WTB[a]][:, tb * 128:(tb + 1) * 128], lhsT=Gm[k][:, a * 128:(a + 1) * 128],
                                                              rhs=ident_bf, start=(hq == 0), stop=(hq == 7), skip_group_check=True),
                                     reads=[BGm[k], Bcst], writes=[P[WTB[a]]])

                        def load_u(eb):
                            k = cnt["u"] % 2
                            cnt["u"] += 1
                            S.dma("pool", ub[k][:].rearrange("p a b -> p (a b)"), uT[eb], Bub[k], writes=[Bub[k]], max_dma_last_dim=8192)
                            return k

                        def load_v(g, hf):
                            S.dma("pool", vb[hf][:], vE[g * GRP * 128:(g + 1) * GRP * 128, hf * 2048:(hf + 1) * 2048].rearrange("(a p) d -> p a d", p=128),
                                  Bvb[hf], writes=[Bvb[hf]], max_dma_last_dim=8192)

                        for pi in range(32):
                            wbuild_pair(0, pi)
                        for g in range(NGRP):
                            for a in range(GRP):
                                ku = load_u(g * GRP + a)
                                sbk = SB_[cnt["s"] % 2]
                                kg = cnt["s"] % 2
                                cnt["s"] += 1
                                for dc in range(NDC):
                                    S.op("pe", lambda e: e.matmul(ps[sbk][:], lhsT=ub[ku][:, dc, :], rhs=h2T[:, dc, :],
                                                                  start=(dc == 0), stop=(dc == NDC - 1)),
                                         reads=[Bub[ku], Bh2T], writes=[P[sbk]])
                                S.op("act", lambda e: e.activation(out=gl[kg][:], in_=ps[sbk][:], func=AF.Gelu),
                                     reads=[P[sbk]], writes=[Bgl[kg]])
                                S.op("dve", lambda e: e.tensor_tensor(out=actT[a][:], in0=gl[kg][:], in1=ps[WTB[a]][:], op=ALU.mult),
                                     reads=[Bgl[kg], P[WTB[a]]], writes=[BactT[a]])
                            load_v(g, 0)
                            load_v(g, 1)
                            for dch in range(NDC):
                                hf = dch // 16
                                yb = YB[cnt["y"] % 2]
                                cnt["y"] += 1
                                for a in range(GRP):
                                    S.op("pe", lambda e: e.matmul(ps[yb][:], lhsT=vb[hf][:, a, (dch % 16) * 128:(dch % 16 + 1) * 128], rhs=actT[a][:],
                                                                  start=(a == 0), stop=(a == GRP - 1)),
                                         reads=[Bvb[hf], BactT[a]], writes=[P[yb]])
                                S.op("dve", lambda e: e.scalar_tensor_tensor(out=X[:, dch, :], in0=ps[yb][:], scalar=gate2[:, dch:dch + 1],
                                                                             op0=ALU.mult, in1=X[:, dch, :], op1=ALU.add),
                                     reads=[P[yb], BX[dch // 8]] + Bmv, writes=[BX[dch // 8]])
                                if g + 1 < NGRP:
                                    wbuild_pair(g + 1, dch)
                        emit_rstd((sq, Bsq), X, BX, rstd, Brstd, 4, 1.0 / D)
                        ostf = [sb(p6, f"ostf{i}", [128, T], F32) for i in range(2)]
                        Bostf = [Buf("ostf0"), Buf("ostf1")]
                        for dc in range(NDC):
                            k = dc % 2
                            S.op("dve", lambda e: e.scalar_tensor_tensor(out=ostf[k][:], in0=X[:, dc, :], scalar=vec[:, OFF_GF + dc:OFF_GF + dc + 1],
                                                                         op0=ALU.mult, in1=rstd[:], op1=ALU.mult),
                                 reads=[BX[dc // 8], Brstd, Bvec], writes=[Bostf[k]])
                            S.dma("sp", outT[j, :, dc, :], ostf[k][:], Bostf[k], reads=[Bostf[k]], writes=[Bout])
                        S.barrier()
        S.finish()
    return nc


def _consts():
    j = np.arange(128)
    ones = np.ones((128, 128), np.float32)
    ident = np.eye(128, dtype=np.float32)
    negU = -(j[:, None] >= j[None, :]).astype(np.float32)
    tri = (j[:, None] < j[None, :]).astype(np.float32)
    return np.ascontiguousarray(np.stack([ones, -ones, ident, negU, tri], axis=1))


def _pcol(v):
    return np.ascontiguousarray(np.asarray(v, np.float32).reshape(-1, 128).T)


def prepare_inputs(x, c, w_ada, b_ada, g_norm1, w_in, g_attn_head, w_pool, s_pool, w_out, g_norm2, w_query,
                   sub_keys_1, sub_keys_2, u_experts, v_experts, g_final, cores=range(8)):
    x = np.asarray(x, np.float32)
    shared = dict(
        w_ada=np.ascontiguousarray(np.asarray(w_ada, np.float32)[0]),
        consts=_consts(),
        w_in=np.ascontiguousarray(np.asarray(w_in, np.float32)[0]),
        w_pool=np.ascontiguousarray(np.asarray(w_pool, np.float32)[0]),
        w_out=np.ascontiguousarray(np.asarray(w_out, np.float32)[0]),
        w_query=np.ascontiguousarray(np.asarray(w_query, np.float32)[0]),
        skT=np.ascontiguousarray(np.stack([np.asarray(sub_keys_1, np.float32)[0].T, np.asarray(sub_keys_2, np.float32)[0].T], axis=1)),
        vE=np.ascontiguousarray(np.asarray(v_experts, np.float32)[0]),
    )
    u = np.asarray(u_experts, np.float32)[0]
    shared["uT"] = np.ascontiguousarray(u.reshape(NEB, 128, NDC, 128).transpose(0, 3, 2, 1)).reshape(NEB, 128, NDC * 128)
    in_maps = []
    for core in cores:
        b, par = core // 2, core % 2
        xb = x[b]
        xt = xb.reshape(NSLOT, T, NDC, 128).transpose(0, 3, 2, 1)
        loc = np.zeros((NSLOT, 128, NDC, T), np.float32)
        if par == 1:
            loc[:] = xt
        else:
            loc[1:] = xt[:NSLOT - 1]
        vecs = np.zeros((128, NVEC), np.float32)
        vecs[:, OFF_BADA:OFF_BADA + 192] = _pcol(np.asarray(b_ada)[0])
        vecs[:, OFF_G1:OFF_G1 + 32] = _pcol(np.asarray(g_norm1)[0])
        vecs[:, OFF_G2:OFF_G2 + 32] = _pcol(np.asarray(g_norm2)[0])
        vecs[:, OFF_GF:OFF_GF + 32] = _pcol(np.asarray(g_final))
        vecs[:, OFF_GH:OFF_GH + 16] = np.asarray(g_attn_head, np.float32)[0].T
        vecs[:, OFF_SP:OFF_SP + 16] = _pcol(np.asarray(s_pool)[0])
        vecs[:, OFF_FLAG] = float(par)
        for g, w in enumerate(POOL_W):
            cntv = np.minimum(np.arange(16) + 1, w) if par == 0 else np.full(16, w)
            vecs[:, OFF_INVC + g * 16:OFF_INVC + (g + 1) * 16] = (1.0 / cntv.astype(np.float32))[None, :]
        m = dict(shared)
        m["xT"] = loc
        m["cT"] = _pcol(np.asarray(c, np.float32)[b])
        m["vecs"] = vecs
        in_maps.append(m)
    return in_maps


def assemble_output(results, cores=range(8)):
    out = np.zeros((4, 4096, D), np.float32)
    for core, r in zip(cores, results):
        b, par = core // 2, core % 2
        o = np.asarray(r["outT"])
        for j in range(NOWN):
            tile = 2 * j + par
            out[b, tile * T:(tile + 1) * T, :] = o[j].transpose(2, 1, 0).reshape(T, D)
    return out


_NC_CACHE = {}


def kernel(**inputs):
    if "nc" not in _NC_CACHE:
        _NC_CACHE["nc"] = build_program()
    nc = _NC_CACHE["nc"]
    in_maps = prepare_inputs(**inputs)
    res = run_bass_kernel_spmd(nc, in_maps, core_ids=list(range(8)))
    return assemble_output(res.results)
```

```python
import numpy as np
from contextlib import ExitStack
import concourse.bass as bass
import concourse.mybir as mybir
from concourse.bass_utils import run_bass_kernel_spmd

F32 = mybir.dt.float32
BF16 = mybir.dt.bfloat16
AF = mybir.ActivationFunctionType
ALU = mybir.AluOpType

D = 4096
NDC = 32
T = 512
NSLOT = 8
NOWN = 4
NH = 16
EPS = 1e-6
NEB = 128
GRP = 4
NGRP = NEB // GRP
POOL_W = (2, 4, 8, 16)
NVEC = 192 + 32 * 3 + 16 + 16 + 1 + 64
OFF_BADA, OFF_G1, OFF_G2, OFF_GF, OFF_GH, OFF_SP, OFF_FLAG, OFF_INVC = 0, 192, 224, 256, 288, 304, 320, 321


class _Buf:
    def __init__(self, name):
        self.name = name
        self.last_write = None
        self.reads = {}
        self.dsem = None


_BUFS = {}


def Buf(name):
    if name not in _BUFS:
        _BUFS[name] = _Buf(name)
    return _BUFS[name]


class Sched:
    def __init__(self, nc, stack):
        self.nc = nc
        self.stack = stack
        self.eng = {"pe": nc.tensor, "act": nc.scalar, "dve": nc.vector, "pool": nc.gpsimd, "sp": nc.sync}
        self.sem = {}
        self.cnt = {}
        for e in ("pe", "act", "dve", "pool"):
            self.sem[e] = stack.enter_context(nc.semaphore("s_" + e))
            self.cnt[e] = 0
        self.waited = {e: {} for e in self.eng}
        self.dcnt = {}
        self.dsems = []

    def _wait(self, engine, ev):
        if ev[0] == "c":
            _, e, v = ev
            if e == "pe" and engine == "pe":
                return
            key = ("c", e)
            sem = self.sem[e]
        else:
            b = ev[1]
            key = ("d", b.name)
            sem = b.dsem
            v = self.dcnt[b.name]
        if self.waited[engine].get(key, 0) >= v:
            return
        self.waited[engine][key] = v
        self.eng[engine].wait_ge(sem, v)

    def _deps(self, engine, reads, writes):
        for b in reads:
            if b.last_write is not None:
                self._wait(engine, b.last_write)
        for b in writes:
            if b.last_write is not None:
                self._wait(engine, b.last_write)
            for ev in list(b.reads.values()):
                self._wait(engine, ev)

    def _commit(self, ev, reads, writes):
        key = (ev[0], ev[1]) if ev[0] == "c" else ("d", ev[1].name)
        for b in reads:
            if b not in writes:
                b.reads[key] = ev
        for b in writes:
            b.last_write = ev
            b.reads = {}

    def op(self, engine, fn, reads=(), writes=()):
        reads = list(reads)
        writes = list(writes)
        self._deps(engine, reads, writes)
        ins = fn(self.eng[engine])
        ins.then_inc(self.sem[engine], 1)
        self.cnt[engine] += 1
        self._commit(("c", engine, self.cnt[engine]), reads, writes)
        return ins

    def dma(self, queue, out, in_, owner, reads=(), writes=(), **kw):
        reads = list(reads)
        writes = list(writes)
        if owner.dsem is None:
            owner.dsem = self.stack.enter_context(self.nc.semaphore("d_" + owner.name))
            self.dcnt[owner.name] = 0
            self.dsems.append(owner)
        self._deps(queue, reads, writes)
        ins = self.eng[queue].dma_start(out=out, in_=in_, **kw)
        ins.then_inc(owner.dsem, 16)
        self.dcnt[owner.name] += 16
        self._commit(("d", owner), reads, writes)
        return ins

    def barrier(self):
        for q in ("pe", "act", "dve", "pool", "sp"):
            for e in ("pe", "act", "dve", "pool"):
                if self.cnt[e] > 0 and e != q:
                    self._wait(q, ("c", e, self.cnt[e]))
            for b in self.dsems:
                self._wait(q, ("d", b))

    def finish(self):
        for e in ("pe", "act", "dve", "pool"):
            if self.cnt[e] > 0:
                self._wait("sp", ("c", e, self.cnt[e]))
        for b in self.dsems:
            self._wait("sp", ("d", b))


def build_program(stop_after=99, debug=False):
    nc = bass.Bass("TRN2", target_bir_lowering=False)
    _BUFS.clear()
    uniq = [0]

    def din(name, shape):
        return nc.dram_tensor(name, shape, F32, kind="ExternalInput").ap()

    def dscr(name, shape, dt=BF16):
        kind = "ExternalOutput" if debug else "Internal"
        return nc.dram_tensor(name, shape, dt, kind=kind).ap()

    xT = din("xT", [NSLOT, 128, NDC, T])
    cT = din("cT", [128, NDC])
    w_ada = din("w_ada", [D, 6 * D])
    vecs = din("vecs", [128, NVEC])
    consts = din("consts", [128, 5, 128])
    w_in = din("w_in", [D, 2 * D])
    w_pool = din("w_pool", [4, 512, 512])
    w_out = din("w_out", [D, D])
    w_query = din("w_query", [D, 2048])
    skT = din("skT", [128, 2, 128])
    uT = din("uT", [NEB, 128, NDC * 128])
    vE = din("vE", [NEB * 128, D])
    outT = nc.dram_tensor("outT", [NOWN, 128, NDC, T], F32, kind="ExternalOutput").ap()

    hT_s = dscr("hT_s", [NSLOT, 128, NDC, T])
    QT_s = dscr("QT_s", [NH, 128, NOWN * T])
    KT_s = dscr("KT_s", [NH, 128, NSLOT * T])
    V_s = dscr("V_s", [NSLOT * 4, 128, 2048])
    oT_s = dscr("oT_s", [NOWN, 128, NDC, T])
    if debug:
        dbg_mod = nc.dram_tensor("dbg_mod", [128, 192], F32, kind="ExternalOutput").ap()
        dbg_x1 = nc.dram_tensor("dbg_x1", [NOWN, 128, NDC, T], F32, kind="ExternalOutput").ap()
        dbg_h2 = nc.dram_tensor("dbg_h2", [NOWN, 128, NDC, T], BF16, kind="ExternalOutput").ap()
        dbg_s12 = nc.dram_tensor("dbg_s12", [NOWN, 128, 4, 8, 256], F32, kind="ExternalOutput").ap()
        dbg_td = nc.dram_tensor("dbg_td", [NOWN, 128, 4, 8, 2], F32, kind="ExternalOutput").ap()
    BhT_s, BQT_s, BKT_s, BV_s, BoT_s, Bout = [Buf(n) for n in ("hT_s", "QT_s", "KT_s", "V_s", "oT_s", "outd")]
    Bdbg = Buf("dbg")

    with ExitStack() as gs:
        S = Sched(nc, gs)

        def sb(st, name, shape, dt):
            uniq[0] += 1
            return st.enter_context(nc.sbuf_tensor(f"{name}_{uniq[0]}", shape, dt))

        ps = [gs.enter_context(nc.psum_tensor(f"ps{i}", [128, 512], F32)) for i in range(8)]
        P = [Buf(f"ps{i}") for i in range(8)]

        cst = sb(gs, "cst", [128, 5, 128], BF16)
        Bcst = Buf("cst")
        S.dma("pool", cst[:], consts, Bcst, writes=[Bcst])
        ones_bf, negones_bf, ident_bf, negU_bf, tri_bf = [cst[:, i, :] for i in range(5)]
        vec = sb(gs, "vec", [128, NVEC], F32)
        Bvec = Buf("vec")
        S.dma("sp", vec[:], vecs, Bvec, writes=[Bvec])
        modv = sb(gs, "modv", [128, 192], F32)
        A12 = sb(gs, "A12", [128, 64], F32)
        Bmod = Buf("modv")
        BA12 = Buf("A12")
        flag = vec[:, OFF_FLAG:OFF_FLAG + 1]

        with ExitStack() as ph:
            cT_sb = sb(ph, "cT_sb", [128, NDC], F32)
            sc = sb(ph, "sc", [128, NDC], BF16)
            wa = [sb(ph, f"wa{i}", [128, NDC, 512], BF16) for i in range(2)]
            Bc, Bsc = Buf("cT_sb"), Buf("sc")
            Bwa = [Buf("wa0"), Buf("wa1")]
            S.dma("sp", cT_sb[:], cT, Bc, writes=[Bc])
            S.op("act", lambda e: e.activation(out=sc[:], in_=cT_sb[:], func=AF.Silu), reads=[Bc], writes=[Bsc])
            for ct in range(48):
                k = ct % 2
                S.dma("pool", wa[k][:], w_ada[:, ct * 512:(ct + 1) * 512].rearrange("(dc p) n -> p dc n", p=128),
                      Bwa[k], writes=[Bwa[k]], max_dma_last_dim=8192)
                for mm in range(4):
                    j = ct * 4 + mm
                    for dc in range(NDC):
                        S.op("pe", lambda e: e.matmul(ps[0][:, j:j + 1], lhsT=wa[k][:, dc, mm * 128:(mm + 1) * 128],
                                                      rhs=sc[:, dc:dc + 1], start=(dc == 0), stop=(dc == NDC - 1)),
                             reads=[Bwa[k], Bsc], writes=[P[0]])
            S.op("dve", lambda e: e.tensor_tensor(out=modv[:], in0=ps[0][:, 0:192], in1=vec[:, OFF_BADA:OFF_BADA + 192],
                                                  op=ALU.add), reads=[P[0], Bvec], writes=[Bmod])
            S.op("dve", lambda e: e.scalar_tensor_tensor(out=A12[:, 0:32], in0=modv[:, 32:64], scalar=1.0, op0=ALU.add,
                                                         in1=vec[:, OFF_G1:OFF_G1 + 32], op1=ALU.mult),
                 reads=[Bmod, Bvec], writes=[BA12])
            S.op("dve", lambda e: e.scalar_tensor_tensor(out=A12[:, 32:64], in0=modv[:, 128:160], scalar=1.0, op0=ALU.add,
                                                         in1=vec[:, OFF_G2:OFF_G2 + 32], op1=ALU.mult),
                 reads=[Bmod, Bvec], writes=[BA12])
            if debug:
                S.dma("sp", dbg_mod, modv[:], Bmod, reads=[Bmod], writes=[Bdbg])
            S.barrier()
        A1 = A12[:, 0:32]
        A2 = A12[:, 32:64]
        B1 = modv[:, 0:32]
        gate1 = modv[:, 64:96]
        B2 = modv[:, 96:128]
        gate2 = modv[:, 160:192]
        Bmv = [Bmod, BA12, Bvec]

        def emit_rstd(st_bufs, Xt, BXs, rstd, Brstd, bank, n_part_inv):
            sq, Bsq = st_bufs
            for dc in range(NDC):
                k = dc % 2
                S.op("act", lambda e: e.activation(out=sq[k][:], in_=Xt[:, dc, :], func=AF.Square),
                     reads=[BXs[dc // 8]], writes=[Bsq[k]])
                S.op("pe", lambda e: e.matmul(ps[bank][:], lhsT=ones_bf, rhs=sq[k][:], start=(dc == 0), stop=(dc == NDC - 1)),
                     reads=[Bsq[k], Bcst], writes=[P[bank]])
            S.op("act", lambda e: e.activation(out=rstd[:], in_=ps[bank][:], func=AF.Sqrt, scale=n_part_inv, bias=EPS),
                 reads=[P[bank]], writes=[Brstd])
            S.op("dve", lambda e: e.reciprocal(out=rstd[:], in_=rstd[:]), reads=[Brstd], writes=[Brstd])

        def emit_norm_mod(Xt, BXs, rstd, Brstd, Acol, Bcol, tmp, Btmp, hT, BhT):
            for dc in range(NDC):
                k = dc % 2
                S.op("dve", lambda e: e.scalar_tensor_tensor(out=tmp[k][:], in0=Xt[:, dc, :], scalar=Acol[:, dc:dc + 1],
                                                             op0=ALU.mult, in1=rstd[:], op1=ALU.mult),
                     reads=[BXs[dc // 8], Brstd] + Bmv, writes=[Btmp[k]])
                S.op("act", lambda e: e.activation(out=hT[:, dc, :], in_=tmp[k][:], func=AF.Identity,
                                                   bias=Bcol[:, dc:dc + 1]),
                     reads=[Btmp[k]] + Bmv, writes=[BhT])

        if stop_after >= 1:
            with ExitStack() as ph:
                X = sb(ph, "X1", [128, NDC, T], F32)
                BX = [Buf(f"X1_{i}") for i in range(4)]
                sq = [sb(ph, f"sq{i}", [128, T], BF16) for i in range(2)]
                Bsq = [Buf("sq0"), Buf("sq1")]
                rstd = sb(ph, "rstd", [128, T], F32)
                Brstd = Buf("rstd")
                tmp = [sb(ph, f"tmp{i}", [128, T], F32) for i in range(2)]
                Btmp = [Buf("tmp0"), Buf("tmp1")]
                hT = [sb(ph, f"hT{i}", [128, NDC, T], BF16) for i in range(2)]
                BhT = [Buf("hT0"), Buf("hT1")]
                for s in range(NSLOT):
                    for qd in range(4):
                        S.dma("sp", X[:, qd * 8:(qd + 1) * 8, :], xT[s, :, qd * 8:(qd + 1) * 8, :], BX[qd], writes=[BX[qd]])
                    emit_rstd((sq, Bsq), X, BX, rstd, Brstd, s % 2, 1.0 / D)
                    emit_norm_mod(X, BX, rstd, Brstd, A1, B1, tmp, Btmp, hT[s % 2], BhT[s % 2])
                    S.dma("act", hT_s[s], hT[s % 2][:], BhT[s % 2], reads=[BhT[s % 2]], writes=[BhT_s])
                S.barrier()

        if stop_after >= 2:
            with ExitStack() as ph:
                Wg = [sb(ph, f"Wg{i}", [128, NDC, 512], BF16) for i in range(2)]
                BWg = [Buf("Wg0"), Buf("Wg1")]
                hTt = [sb(ph, f"hTt{i}", [128, NDC, T], BF16) for i in range(2)]
                BhTt = [Buf("hTt0"), Buf("hTt1")]
                stg = [sb(ph, f"stg{i}", [128, 512], BF16) for i in range(4)]
                Bstg = [Buf(f"stg{i}") for i in range(4)]
                Pb = [sb(ph, f"Pb{i}", [128, 528], F32) for i in range(4)]
                BPb = [Buf(f"Pb{i}") for i in range(4)]
                T1 = sb(ph, "T1", [128, 528], F32)
                T2 = sb(ph, "T2", [128, 528], F32)
                BT1, BT2 = Buf("T1"), Buf("T2")
                dT = [sb(ph, f"dT{i}", [128, 512], BF16) for i in range(4)]
                BdT = [Buf(f"dT{i}") for i in range(4)]
                d16 = sb(ph, "d16", [128, 16], F32)
                Bd16 = Buf("d16")
                hh = sb(ph, "hh", [128, NDC, 16], BF16)
                Bhh = Buf("hh")
                wp = sb(ph, "wp", [128, 4, 512], BF16)
                Bwp = Buf("wp")
                nload = [0]
                nps = [0]
                nst = [0]

                def load_h(slot):
                    k = nload[0] % 2
                    nload[0] += 1
                    S.dma("sp", hTt[k][:], hT_s[slot], BhTt[k], reads=[BhT_s], writes=[BhTt[k]])
                    return k

                for cg in range(16):
                    kw = cg % 2
                    S.dma("pool", Wg[kw][:], w_in[:, cg * 512:(cg + 1) * 512].rearrange("(dc p) n -> p dc n", p=128),
                          BWg[kw], writes=[BWg[kw]], max_dma_last_dim=8192)
                    kind = cg // 4
                    sub = cg % 4
                    if kind in (0, 1):
                        slots = [1, 3, 5, 7] if kind == 0 else list(range(NSLOT))
                        for si, slot in enumerate(slots):
                            kh = load_h(slot)
                            for hx in range(4):
                                head = sub * 4 + hx
                                r = nps[0] % 4
                                nps[0] += 1
                                for dc in range(NDC):
                                    S.op("pe", lambda e: e.matmul(ps[r][:], lhsT=Wg[kw][:, dc, hx * 128:(hx + 1) * 128],
                                                                  rhs=hTt[kh][:, dc, :], start=(dc == 0), stop=(dc == NDC - 1)),
                                         reads=[BWg[kw], BhTt[kh]], writes=[P[r]])
                                q = nst[0] % 4
                                nst[0] += 1
                                sc_ = (128.0 ** -0.5) if kind == 0 else 1.0
                                S.op("act", lambda e: e.activation(out=stg[q][:], in_=ps[r][:], func=AF.Copy, scale=sc_),
                                     reads=[P[r]], writes=[Bstg[q]])
                                if kind == 0:
                                    S.dma("act", QT_s[head, :, si * T:(si + 1) * T], stg[q][:], Bstg[q], reads=[Bstg[q]], writes=[BQT_s])
                                else:
                                    S.dma("act", KT_s[head, :, slot * T:(slot + 1) * T], stg[q][:], Bstg[q], reads=[Bstg[q]], writes=[BKT_s])
                    elif kind == 2:
                        for slot in range(NSLOT):
                            kh = load_h(slot)
                            for tb in range(4):
                                r = nps[0] % 4
                                nps[0] += 1
                                for dc in range(NDC):
                                    S.op("pe", lambda e: e.matmul(ps[r][:], lhsT=hTt[kh][:, dc, tb * 128:(tb + 1) * 128],
                                                                  rhs=Wg[kw][:, dc, :], start=(dc == 0), stop=(dc == NDC - 1)),
                                         reads=[BWg[kw], BhTt[kh]], writes=[P[r]])
                                q = nst[0] % 4
                                nst[0] += 1
                                if slot == 0:
                                    S.op("act", lambda e: e.activation(out=stg[q][:], in_=ps[r][:], func=AF.Identity, scale=flag),
                                         reads=[P[r], Bvec], writes=[Bstg[q]])
                                else:
                                    S.op("act", lambda e: e.activation(out=stg[q][:], in_=ps[r][:], func=AF.Copy),
                                         reads=[P[r]], writes=[Bstg[q]])
                                S.dma("act", V_s[slot * 4 + tb, :, sub * 512:(sub + 1) * 512], stg[q][:], Bstg[q],
                                      reads=[Bstg[q]], writes=[BV_s])
                    else:
                        g = sub
                        wdw = POOL_W[g]
                        S.dma("pool", wp[:], w_pool[g].rearrange("(cc p) n -> p cc n", p=128), Bwp, writes=[Bwp])
                        for j in range(NOWN):
                            S.dma("sp", hh[:], hT_s[2 * j, :, :, T - 16:T], Bhh, reads=[BhT_s], writes=[Bhh])
                            kh = load_h(2 * j + 1)
                            for cc in range(4):
                                r = nps[0] % 4
                                nps[0] += 1
                                for dc in range(NDC):
                                    S.op("pe", lambda e: e.matmul(ps[r][:, 0:16], lhsT=Wg[kw][:, dc, cc * 128:(cc + 1) * 128],
                                                                  rhs=hh[:, dc, :], start=(dc == 0), stop=(dc == NDC - 1)),
                                         reads=[BWg[kw], Bhh], writes=[P[r]])
                                if j == 0:
                                    S.op("act", lambda e: e.activation(out=Pb[cc][:, 0:16], in_=ps[r][:, 0:16], func=AF.Identity, scale=flag),
                                         reads=[P[r], Bvec], writes=[BPb[cc]])
                                else:
                                    S.op("act", lambda e: e.activation(out=Pb[cc][:, 0:16], in_=ps[r][:, 0:16], func=AF.Copy),
                                         reads=[P[r]], writes=[BPb[cc]])
                                r = nps[0] % 4
                                nps[0] += 1
                                for dc in range(NDC):
                                    S.op("pe", lambda e: e.matmul(ps[r][:], lhsT=Wg[kw][:, dc, cc * 128:(cc + 1) * 128],
                                                                  rhs=hTt[kh][:, dc, :], start=(dc == 0), stop=(dc == NDC - 1)),
                                         reads=[BWg[kw], BhTt[kh]], writes=[P[r]])
                                S.op("act", lambda e: e.activation(out=Pb[cc][:, 16:528], in_=ps[r][:], func=AF.Copy),
                                     reads=[P[r]], writes=[BPb[cc]])
                                S.op("dve", lambda e: e.tensor_tensor(out=T1[:, 1:528], in0=Pb[cc][:, 1:528], in1=Pb[cc][:, 0:527], op=ALU.add),
                                     reads=[BPb[cc]], writes=[BT1])
                                ws, Bws = T1, BT1
                                if wdw >= 4:
                                    S.op("dve", lambda e: e.tensor_tensor(out=T2[:, 3:528], in0=T1[:, 3:528], in1=T1[:, 1:526], op=ALU.add),
                                         reads=[BT1], writes=[BT2])
                                    ws, Bws = T2, BT2
                                if wdw >= 8:
                                    S.op("dve", lambda e: e.tensor_tensor(out=T1[:, 7:528], in0=T2[:, 7:528], in1=T2[:, 3:524], op=ALU.add),
                                         reads=[BT2], writes=[BT1])
                                    ws, Bws = T1, BT1
                                if wdw >= 16:
                                    S.op("dve", lambda e: e.tensor_tensor(out=T2[:, 15:528], in0=T1[:, 15:528], in1=T1[:, 7:520], op=ALU.add),
                                         reads=[BT1], writes=[BT2])
                                    ws, Bws = T2, BT2
                                S.op("dve", lambda e: e.scalar_tensor_tensor(out=dT[cc][:], in0=ws[:, 16:528], scalar=1.0 / wdw, op0=ALU.mult,
                                                                             in1=Pb[cc][:, 16:528], op1=ALU.subtract),
                                     reads=[Bws, BPb[cc]], writes=[BdT[cc]])
                                if j == 0:
                                    S.op("dve", lambda e: e.tensor_tensor(out=d16[:], in0=ws[:, 16:32],
                                                                          in1=vec[:, OFF_INVC + g * 16:OFF_INVC + (g + 1) * 16], op=ALU.mult),
                                         reads=[Bws, Bvec], writes=[Bd16])
                                    S.op("dve", lambda e: e.tensor_tensor(out=dT[cc][:, 0:16], in0=d16[:], in1=Pb[cc][:, 16:32], op=ALU.subtract),
                                         reads=[Bd16, BPb[cc]], writes=[BdT[cc]])
                            for dd in range(4):
                                r = nps[0] % 4
                                nps[0] += 1
                                for cc in range(4):
                                    S.op("pe", lambda e: e.matmul(ps[r][:], lhsT=wp[:, cc, dd * 128:(dd + 1) * 128], rhs=dT[cc][:],
                                                                  start=(cc == 0), stop=(cc == 3)),
                                         reads=[Bwp, BdT[cc]], writes=[P[r]])
                                q = nst[0] % 4
                                nst[0] += 1
                                col = OFF_SP + g * 4 + dd
                                S.op("act", lambda e: e.activation(out=stg[q][:], in_=ps[r][:], func=AF.Identity, scale=vec[:, col:col + 1]),
                                     reads=[P[r], Bvec], writes=[Bstg[q]])
                                S.dma("act", oT_s[j, :, 16 + g * 4 + dd, :], stg[q][:], Bstg[q], reads=[Bstg[q]], writes=[BoT_s])
                S.barrier()

        if stop_after >= 3:
            with ExitStack() as ph:
                KTh = [sb(ph, f"KTh{i}", [128, NSLOT * T], BF16) for i in range(2)]
                Vh = [sb(ph, f"Vh{i}", [128, NSLOT * 4, 128], BF16) for i in range(2)]
                QTh = [sb(ph, f"QTh{i}", [128, NOWN * T], BF16) for i in range(2)]
                BKTh = [Buf("KTh0"), Buf("KTh1")]
                BVh = [Buf("Vh0"), Buf("Vh1")]
                BQTh = [Buf("QTh0"), Buf("QTh1")]
                E = [sb(ph, f"E{i}", [128, T], F32) for i in range(2)]
                BE = [Buf("E0"), Buf("E1")]
                Lp = [sb(ph, f"Lp{i}", [128, T], BF16) for i in range(3)]
                BLp = [Buf(f"Lp{i}") for i in range(3)]
                Ls = sb(ph, "Ls", [128, T], BF16)
                BLs = Buf("Ls")
                Aa = [sb(ph, f"Aa{i}", [128, T], BF16) for i in range(2)]
                BAa = [Buf("Aa0"), Buf("Aa1")]
                sqa = sb(ph, "sqa", [128, T], BF16)
                Bsqa = Buf("sqa")
                rsa = sb(ph, "rsa", [128, T], F32)
                Brsa = Buf("rsa")
                ost = [sb(ph, f"ost{i}", [128, T], BF16) for i in range(2)]
                Bost = [Buf("ost0"), Buf("ost1")]

                units = []
                for h in range(NH):
                    for j in range(NOWN):
                        qs = 2 * j + 1
                        kbs = list(range(qs * 4 + 3, -1, -1))
                        for ui, kb in enumerate(kbs):
                            c0 = (kb - qs * 4) * 128 if kb >= qs * 4 else 0
                            units.append(dict(h=h, j=j, kb=kb, c0=c0, first=(ui == 0), last=(ui == len(kbs) - 1),
                                              diag=(kb >= qs * 4)))

                def load_head(h):
                    k = h % 2
                    S.dma("sp", KTh[k][:], KT_s[h], BKTh[k], reads=[BKT_s], writes=[BKTh[k]])
                    S.dma("sp", QTh[k][:], QT_s[h], BQTh[k], reads=[BQT_s], writes=[BQTh[k]])
                    S.dma("sp", Vh[k][:], V_s[:, :, h * 128:(h + 1) * 128].rearrange("tb p d -> p tb d"), BVh[k],
                          reads=[BV_s], writes=[BVh[k]])

                def stage0(i, u):
                    hk = u["h"] % 2
                    zb = i % 4
                    c0 = u["c0"]
                    qcol = u["j"] * T
                    S.op("pe", lambda e: e.matmul(ps[zb][:, c0:T], lhsT=KTh[hk][:, u["kb"] * 128:(u["kb"] + 1) * 128],
                                                  rhs=QTh[hk][:, qcol + c0:qcol + T], start=True, stop=True),
                         reads=[BKTh[hk], BQTh[hk]], writes=[P[zb]])

                def stage1(i, u):
                    zb = i % 4
                    c0 = u["c0"]
                    S.op("act", lambda e: e.activation(out=E[i % 2][:, c0:T], in_=ps[zb][:, c0:T], func=AF.Exp),
                         reads=[P[zb]], writes=[BE[i % 2]])
                    S.op("act", lambda e: e.activation(out=Lp[i % 3][:, c0:T], in_=E[i % 2][:, c0:T], func=AF.Ln, bias=1.0),
                         reads=[BE[i % 2]], writes=[BLp[i % 3]])
                    if u["diag"]:
                        S.op("dve", lambda e: e.tensor_tensor(out=Lp[i % 3][:, c0:c0 + 128], in0=Lp[i % 3][:, c0:c0 + 128],
                                                              in1=tri_bf, op=ALU.mult),
                             reads=[BLp[i % 3], Bcst], writes=[BLp[i % 3]])

                def stage2(i, u):
                    zb = i % 4
                    c0 = u["c0"]
                    S.op("pe", lambda e: e.matmul(ps[zb][:, c0:T], lhsT=negU_bf, rhs=Lp[i % 3][:, c0:T], start=False, stop=u["first"],
                                                  skip_group_check=True),
                         reads=[BLp[i % 3], Bcst], writes=[P[zb]])
                    if u["first"]:
                        S.op("pool", lambda e: e.memset(Ls[:], 0.0), writes=[BLs])
                    else:
                        S.op("pe", lambda e: e.matmul(ps[zb][:, c0:T], lhsT=negones_bf, rhs=Ls[:, c0:T], start=False, stop=True,
                                                      skip_group_check=True),
                             reads=[BLs, Bcst], writes=[P[zb]])
                    if not u["last"]:
                        S.op("pool", lambda e: e.tensor_tensor(out=Ls[:, c0:T], in0=Ls[:, c0:T], in1=Lp[i % 3][:, c0:T], op=ALU.add),
                             reads=[BLs, BLp[i % 3]], writes=[BLs])

                def stage3(i, u):
                    hk = u["h"] % 2
                    zb = i % 4
                    c0 = u["c0"]
                    ob = 4 + (u["h"] * NOWN + u["j"]) % 2
                    S.op("act", lambda e: e.activation(out=Aa[i % 2][:, c0:T], in_=ps[zb][:, c0:T], func=AF.Exp),
                         reads=[P[zb]], writes=[BAa[i % 2]])
                    if u["diag"]:
                        S.op("dve", lambda e: e.tensor_tensor(out=Aa[i % 2][:, c0:c0 + 128], in0=Aa[i % 2][:, c0:c0 + 128],
                                                              in1=tri_bf, op=ALU.mult),
                             reads=[BAa[i % 2], Bcst], writes=[BAa[i % 2]])
                    S.op("pe", lambda e: e.matmul(ps[ob][:, c0:T], lhsT=Vh[hk][:, u["kb"], :], rhs=Aa[i % 2][:, c0:T],
                                                  start=u["first"], stop=u["last"], skip_group_check=True),
                         reads=[BVh[hk], BAa[i % 2]], writes=[P[ob]])
                    if u["last"]:
                        h, j = u["h"], u["j"]
                        k = (h * NOWN + j) % 2
                        S.op("act", lambda e: e.activation(out=sqa[:], in_=ps[ob][:], func=AF.Square), reads=[P[ob]], writes=[Bsqa])
                        S.op("pe", lambda e: e.matmul(ps[6][:], lhsT=ones_bf, rhs=sqa[:], start=True, stop=True),
                             reads=[Bsqa, Bcst], writes=[P[6]])
                        S.op("act", lambda e: e.activation(out=rsa[:], in_=ps[6][:], func=AF.Sqrt, scale=1.0 / 128, bias=EPS),
                             reads=[P[6]], writes=[Brsa])
                        S.op("dve", lambda e: e.reciprocal(out=rsa[:], in_=rsa[:]), reads=[Brsa], writes=[Brsa])
                        S.op("dve", lambda e: e.scalar_tensor_tensor(out=ost[k][:], in0=ps[ob][:], scalar=vec[:, OFF_GH + h:OFF_GH + h + 1],
                                                                     op0=ALU.mult, in1=rsa[:], op1=ALU.mult),
                             reads=[P[ob], Brsa, Bvec], writes=[Bost[k]])
                        S.dma("sp", oT_s[j, :, h, :], ost[k][:], Bost[k], reads=[Bost[k]], writes=[BoT_s])

                load_head(0)
                n = len(units)
                for i in range(n + 3):
                    if i < n:
                        u = units[i]
                        if u["first"] and u["j"] == 0 and u["h"] + 1 < NH:
                            load_head(u["h"] + 1)
                        stage0(i, u)
                    if 0 <= i - 1 < n:
                        stage1(i - 1, units[i - 1])
                    if 0 <= i - 2 < n:
                        stage2(i - 2, units[i - 2])
                    if 0 <= i - 3 < n:
                        stage3(i - 3, units[i - 3])
                S.barrier()

        if stop_after >= 4:
            with ExitStack() as ph:
                X = sb(ph, "X2", [128, NDC, T], F32)
                BX = [Buf(f"X2_{i}") for i in range(4)]
                h2T = sb(ph, "h2T", [128, NDC, T], BF16)
                Bh2T = Buf("h2T")
                S12 = sb(ph, "S12", [128, 4, 8, 256], F32)
                BS12 = Buf("S12")
                TD = sb(ph, "TD", [128, 4, 8, 2], F32)
                BTD = Buf("TD")
                sq = [sb(ph, f"sqb{i}", [128, T], BF16) for i in range(2)]
                Bsq = [Buf("sqb0"), Buf("sqb1")]
                rstd = sb(ph, "rstd2", [128, T], F32)
                Brstd = Buf("rstd2")
                skb = sb(ph, "skb", [128, 2, 128], BF16)
                Bskb = Buf("skb")
                S.dma("pool", skb[:], skT, Bskb, writes=[Bskb])
                for j in range(NOWN):
                    with ExitStack() as p4:
                        oTt = sb(p4, "oTt", [128, NDC, T], BF16)
                        BoTt = Buf("oTt")
                        Wo = [sb(p4, f"Wo{i}", [128, NDC, 256], BF16) for i in range(2)]
                        BWo = [Buf("Wo0"), Buf("Wo1")]
                        for qd in range(4):
                            S.dma("sp", X[:, qd * 8:(qd + 1) * 8, :], xT[2 * j + 1, :, qd * 8:(qd + 1) * 8, :], BX[qd], writes=[BX[qd]])
                        S.dma("sp", oTt[:], oT_s[j], BoTt, reads=[BoT_s], writes=[BoTt])
                        for cg in range(16):
                            k = cg % 2
                            S.dma("pool", Wo[k][:], w_out[:, cg * 256:(cg + 1) * 256].rearrange("(ec p) n -> p ec n", p=128),
                                  BWo[k], writes=[BWo[k]])
                            for dd in range(2):
                                dch = cg * 2 + dd
                                r = dch % 4
                                for ec in range(NDC):
                                    S.op("pe", lambda e: e.matmul(ps[r][:], lhsT=Wo[k][:, ec, dd * 128:(dd + 1) * 128], rhs=oTt[:, ec, :],
                                                                  start=(ec == 0), stop=(ec == NDC - 1)),
                                         reads=[BWo[k], BoTt], writes=[P[r]])
                                S.op("dve", lambda e: e.scalar_tensor_tensor(out=X[:, dch, :], in0=ps[r][:], scalar=gate1[:, dch:dch + 1],
                                                                             op0=ALU.mult, in1=X[:, dch, :], op1=ALU.add),
                                     reads=[P[r], BX[dch // 8]] + Bmv, writes=[BX[dch // 8]])
                        if debug:
                            S.dma("sp", dbg_x1[j], X[:], BX[0], reads=BX, writes=[Bdbg])
                        S.barrier()
                    if stop_after < 5:
                        continue
                    with ExitStack() as p5:
                        tmp = [sb(p5, f"tmpb{i}", [128, T], F32) for i in range(2)]
                        Btmp = [Buf("tmpb0"), Buf("tmpb1")]
                        Wq = [sb(p5, f"Wq{i}", [128, NDC, 256], BF16) for i in range(2)]
                        BWq = [Buf("Wq0"), Buf("Wq1")]
                        qT = [sb(p5, f"qT{i}", [128, 2, T], BF16) for i in range(2)]
                        BqT = [Buf("qT0"), Buf("qT1")]
                        v16 = sb(p5, "v16", [128, 2, 16], F32)
                        Bv16 = Buf("v16")
                        tmpk = sb(p5, "tmpk", [128, 128], F32)
                        Btmpk = Buf("tmpk")
                        cand = sb(p5, "cand", [128, 256], F32)
                        cand2 = sb(p5, "cand2", [128, 256], F32)
                        Bcand, Bcand2 = Buf("cand"), Buf("cand2")
                        c16 = sb(p5, "c16", [128, 16], F32)
                        Bc16 = Buf("c16")
                        e16 = sb(p5, "e16", [128, 16], F32)
                        Be16 = Buf("e16")
                        sm = sb(p5, "sm", [128, 4], F32)
                        Bsm = Buf("sm")
                        emit_rstd((sq, Bsq), X, BX, rstd, Brstd, 4, 1.0 / D)
                        emit_norm_mod(X, BX, rstd, Brstd, A2, B2, tmp, Btmp, h2T, Bh2T)
                        if debug:
                            S.dma("sp", dbg_h2[j], h2T[:], Bh2T, reads=[Bh2T], writes=[Bdbg])
                        for hq in range(8):
                            k = hq % 2
                            S.dma("pool", Wq[k][:], w_query[:, hq * 256:(hq + 1) * 256].rearrange("(dc p) n -> p dc n", p=128),
                                  BWq[k], writes=[BWq[k]])
                            for cc in range(2):
                                r = cc
                                for dc in range(NDC):
                                    S.op("pe", lambda e: e.matmul(ps[r][:], lhsT=Wq[k][:, dc, cc * 128:(cc + 1) * 128], rhs=h2T[:, dc, :],
                                                                  start=(dc == 0), stop=(dc == NDC - 1)),
                                         reads=[BWq[k], Bh2T], writes=[P[r]])
                                S.op("act", lambda e: e.activation(out=qT[k][:, cc, :], in_=ps[r][:], func=AF.Copy),
                                     reads=[P[r]], writes=[BqT[k]])
                            for tb in range(4):
                                r = 2 + tb % 2
                                for w_ in range(2):
                                    S.op("pe", lambda e: e.matmul(ps[r][:, w_ * 128:(w_ + 1) * 128], lhsT=qT[k][:, w_, tb * 128:(tb + 1) * 128],
                                                                  rhs=skb[:, w_, :], start=True, stop=True, skip_group_check=True),
                                         reads=[BqT[k], Bskb], writes=[P[r]])
                                S.op("act", lambda e: e.activation(out=S12[:, tb, hq, :], in_=ps[r][:, 0:256], func=AF.Copy),
                                     reads=[P[r]], writes=[BS12])
                                for w_ in range(2):
                                    src = S12[:, tb, hq, w_ * 128:(w_ + 1) * 128]
                                    S.op("dve", lambda e: e.max(out=v16[:, w_, 0:8], in_=src), reads=[BS12], writes=[Bv16])
                                    S.op("dve", lambda e: e.match_replace(out=tmpk[:], in_to_replace=v16[:, w_, 0:8], in_values=src,
                                                                          imm_value=-1e30), reads=[BS12, Bv16], writes=[Btmpk])
                                    S.op("dve", lambda e: e.max(out=v16[:, w_, 8:16], in_=tmpk[:]), reads=[Btmpk], writes=[Bv16])
                                S.op("pool", lambda e: e.tensor_tensor(out=cand[:].rearrange("p (a b) -> p a b", a=16),
                                                                       in0=v16[:, 0, :].unsqueeze(2).broadcast_to([128, 16, 16]),
                                                                       in1=v16[:, 1, :].unsqueeze(1).broadcast_to([128, 16, 16]), op=ALU.add),
                                     reads=[Bv16], writes=[Bcand])
                                S.op("dve", lambda e: e.max(out=c16[:, 0:8], in_=cand[:]), reads=[Bcand], writes=[Bc16])
                                S.op("dve", lambda e: e.match_replace(out=cand2[:], in_to_replace=c16[:, 0:8], in_values=cand[:],
                                                                      imm_value=-1e30), reads=[Bcand, Bc16], writes=[Bcand2])
                                S.op("dve", lambda e: e.max(out=c16[:, 8:16], in_=cand2[:]), reads=[Bcand2], writes=[Bc16])
                                S.op("dve", lambda e: e.tensor_scalar(out=sm[:, 0:1], in0=c16[:, 0:1], scalar1=-1.0, scalar2=None, op0=ALU.mult),
                                     reads=[Bc16], writes=[Bsm])
                                S.op("act", lambda e: e.activation(out=e16[:], in_=c16[:], func=AF.Exp, bias=sm[:, 0:1], accum_out=sm[:, 1:2]),
                                     reads=[Bc16, Bsm], writes=[Be16, Bsm])
                                S.op("act", lambda e: e.activation(out=sm[:, 2:3], in_=sm[:, 1:2], func=AF.Ln), reads=[Bsm], writes=[Bsm])
                                S.op("dve", lambda e: e.tensor_tensor(out=TD[:, tb, hq, 1:2], in0=sm[:, 0:1], in1=sm[:, 2:3], op=ALU.subtract),
                                     reads=[Bsm], writes=[BTD])
                                S.op("dve", lambda e: e.tensor_copy(out=TD[:, tb, hq, 0:1], in_=c16[:, 15:16]), reads=[Bc16], writes=[BTD])
                        if debug:
                            S.dma("sp", dbg_s12[j], S12[:], BS12, reads=[BS12], writes=[Bdbg])
                            S.dma("sp", dbg_td[j], TD[:], BTD, reads=[BTD], writes=[Bdbg])
                        S.barrier()
                    if stop_after < 6:
                        continue
                    with ExitStack() as p6:
                        ub = [sb(p6, f"ub{i}", [128, NDC, 128], BF16) for i in range(2)]
                        Bub = [Buf("ub0"), Buf("ub1")]
                        vb = [sb(p6, f"vb{i}", [128, GRP, 2048], BF16) for i in range(2)]
                        Bvb = [Buf("vb0"), Buf("vb1")]
                        actT = [sb(p6, f"actT{i}", [128, T], BF16) for i in range(GRP)]
                        BactT = [Buf(f"actT{i}") for i in range(GRP)]
                        NCB, NGM, LAG = 3, 6, 4
                        Cb = [sb(p6, f"Cb{i}", [128, GRP * 128], F32) for i in range(NCB)]
                        BCb = [Buf(f"Cb{i}") for i in range(NCB)]
                        Gx = [sb(p6, f"Gx{i}", [128, GRP * 128], BF16) for i in range(NCB)]
                        BGx = [Buf(f"Gx{i}") for i in range(NCB)]
                        Gm = [sb(p6, f"Gm{i}", [128, GRP * 128], BF16) for i in range(NGM)]
                        BGm = [Buf(f"Gm{i}") for i in range(NGM)]
                        gl = [sb(p6, f"gl{i}", [128, T], BF16) for i in range(2)]
                        Bgl = [Buf("gl0"), Buf("gl1")]
                        WTB = [0, 1, 2, 3]
                        SB_ = [4, 5]
                        YB = [6, 7]
                        cnt = dict(w=0, s=0, y=0)
                        pending = []

                        def wbuild_elem(g, pi):
                            tb, hq = pi // 8, pi % 8
                            k = cnt["w"] % NCB
                            km = cnt["w"] % NGM
                            cnt["w"] += 1
                            i0 = g * GRP
                            S.op("pool", lambda e: e.tensor_tensor(
                                out=Cb[k][:].rearrange("p (a b) -> p a b", a=GRP),
                                in0=S12[:, tb, hq, i0:i0 + GRP].unsqueeze(2).broadcast_to([128, GRP, 128]),
                                in1=S12[:, tb, hq, 128:256].unsqueeze(1).broadcast_to([128, GRP, 128]), op=ALU.add),
                                reads=[BS12], writes=[BCb[k]])
                            S.op("act", lambda e: e.activation(out=Gx[k][:], in_=Cb[k][:], func=AF.Exp, bias=TD[:, tb, hq, 1:2]),
                                 reads=[BCb[k], BTD], writes=[BGx[k]])
                            S.op("dve", lambda e: e.scalar_tensor_tensor(out=Gm[km][:], in0=Cb[k][:], scalar=TD[:, tb, hq, 0:1], op0=ALU.is_ge,
                                                                         in1=Gx[k][:], op1=ALU.mult),
                                 reads=[BCb[k], BGx[k], BTD], writes=[BGm[km]])
                            pending.append((km, tb, hq))

                        def emit_tr():
                            km, tb, hq = pending.pop(0)
                            for a in range(GRP):
                                S.op("pe", lambda e: e.matmul(ps[WTB[a]][:, tb * 128:(tb + 1) * 128], lhsT=Gm[km][:, a * 128:(a + 1) * 128],
                                                              rhs=ident_bf, start=(hq == 0), stop=(hq == 7), skip_group_check=True),
                                     reads=[BGm[km], Bcst], writes=[P[WTB[a]]])

                        def load_u(eb):
                            k = eb % 2
                            S.dma("pool", ub[k][:].rearrange("p a b -> p (a b)"), uT[eb], Bub[k], writes=[Bub[k]], max_dma_last_dim=8192)

                        def load_v(g, hf):
                            S.dma("pool", vb[hf][:], vE[g * GRP * 128:(g + 1) * GRP * 128, hf * 2048:(hf + 1) * 2048].rearrange("(a p) d -> p a d", p=128),
                                  Bvb[hf], writes=[Bvb[hf]], max_dma_last_dim=8192)

                        load_u(0)
                        load_u(1)
                        load_v(0, 0)
                        load_v(0, 1)
                        for pi in range(32):
                            wbuild_elem(0, pi)
                            emit_tr()
                        for g in range(NGRP):
                            for a in range(GRP):
                                eb = g * GRP + a
                                ku = eb % 2
                                sbk = SB_[cnt["s"] % 2]
                                kg = cnt["s"] % 2
                                cnt["s"] += 1
                                for dc in range(NDC):
                                    S.op("pe", lambda e: e.matmul(ps[sbk][:], lhsT=ub[ku][:, dc, :], rhs=h2T[:, dc, :],
                                                                  start=(dc == 0), stop=(dc == NDC - 1)),
                                         reads=[Bub[ku], Bh2T], writes=[P[sbk]])
                                if a + 2 < GRP:
                                    load_u(eb + 2)
                                if a == 0:
                                    while pending:
                                        emit_tr()
                                S.op("act", lambda e: e.activation(out=gl[kg][:], in_=ps[sbk][:], func=AF.Gelu),
                                     reads=[P[sbk]], writes=[Bgl[kg]])
                                S.op("dve", lambda e: e.tensor_tensor(out=actT[a][:], in0=gl[kg][:], in1=ps[WTB[a]][:], op=ALU.mult),
                                     reads=[Bgl[kg], P[WTB[a]]], writes=[BactT[a]])
                            for dch in range(NDC):
                                hf = dch // 16
                                yb = YB[cnt["y"] % 2]
                                cnt["y"] += 1
                                for a in range(GRP):
                                    S.op("pe", lambda e: e.matmul(ps[yb][:], lhsT=vb[hf][:, a, (dch % 16) * 128:(dch % 16 + 1) * 128], rhs=actT[a][:],
                                                                  start=(a == 0), stop=(a == GRP - 1)),
                                         reads=[Bvb[hf], BactT[a]], writes=[P[yb]])
                                S.op("dve", lambda e: e.scalar_tensor_tensor(out=X[:, dch, :], in0=ps[yb][:], scalar=gate2[:, dch:dch + 1],
                                                                             op0=ALU.mult, in1=X[:, dch, :], op1=ALU.add),
                                     reads=[P[yb], BX[dch // 8]] + Bmv, writes=[BX[dch // 8]])
                                if g + 1 < NGRP:
                                    wbuild_elem(g + 1, dch)
                                    if len(pending) > LAG:
                                        emit_tr()
                                    if dch == 8:
                                        load_u((g + 1) * GRP)
                                    if dch == 12:
                                        load_u((g + 1) * GRP + 1)
                                    if dch == 15:
                                        load_v(g + 1, 0)
                            if g + 1 < NGRP:
                                load_v(g + 1, 1)
                        emit_rstd((sq, Bsq), X, BX, rstd, Brstd, 4, 1.0 / D)
                        ostf = [Cb[0], Cb[1]]
                        Bostf = [BCb[0], BCb[1]]
                        for dc in range(NDC):
                            k = dc % 2
                            S.op("dve", lambda e: e.scalar_tensor_tensor(out=ostf[k][:], in0=X[:, dc, :], scalar=vec[:, OFF_GF + dc:OFF_GF + dc + 1],
                                                                         op0=ALU.mult, in1=rstd[:], op1=ALU.mult),
                                 reads=[BX[dc // 8], Brstd, Bvec], writes=[Bostf[k]])
                            S.dma("sp", outT[j, :, dc, :], ostf[k][:], Bostf[k], reads=[Bostf[k]], writes=[Bout])
                        S.barrier()
        S.finish()
    return nc


def _consts():
    j = np.arange(128)
    ones = np.ones((128, 128), np.float32)
    ident = np.eye(128, dtype=np.float32)
    negU = -(j[:, None] >= j[None, :]).astype(np.float32)
    tri = (j[:, None] < j[None, :]).astype(np.float32)
    return np.ascontiguousarray(np.stack([ones, -ones, ident, negU, tri], axis=1))


def _pcol(v):
    return np.ascontiguousarray(np.asarray(v, np.float32).reshape(-1, 128).T)


def prepare_inputs(x, c, w_ada, b_ada, g_norm1, w_in, g_attn_head, w_pool, s_pool, w_out, g_norm2, w_query,
                   sub_keys_1, sub_keys_2, u_experts, v_experts, g_final, cores=range(8)):
    x = np.asarray(x, np.float32)
    shared = dict(
        w_ada=np.ascontiguousarray(np.asarray(w_ada, np.float32)[0]),
        consts=_consts(),
        w_in=np.ascontiguousarray(np.asarray(w_in, np.float32)[0]),
        w_pool=np.ascontiguousarray(np.asarray(w_pool, np.float32)[0]),
        w_out=np.ascontiguousarray(np.asarray(w_out, np.float32)[0]),
        w_query=np.ascontiguousarray(np.asarray(w_query, np.float32)[0]),
        skT=np.ascontiguousarray(np.stack([np.asarray(sub_keys_1, np.float32)[0].T, np.asarray(sub_keys_2, np.float32)[0].T], axis=1)),
        vE=np.ascontiguousarray(np.asarray(v_experts, np.float32)[0]),
    )
    u = np.asarray(u_experts, np.float32)[0]
    shared["uT"] = np.ascontiguousarray(u.reshape(NEB, 128, NDC, 128).transpose(0, 3, 2, 1)).reshape(NEB, 128, NDC * 128)
    in_maps = []
    for core in cores:
        b, par = core // 2, core % 2
        xb = x[b]
        xt = xb.reshape(NSLOT, T, NDC, 128).transpose(0, 3, 2, 1)
        loc = np.zeros((NSLOT, 128, NDC, T), np.float32)
        if par == 1:
            loc[:] = xt
        else:
            loc[1:] = xt[:NSLOT - 1]
        vecs = np.zeros((128, NVEC), np.float32)
        vecs[:, OFF_BADA:OFF_BADA + 192] = _pcol(np.asarray(b_ada)[0])
        vecs[:, OFF_G1:OFF_G1 + 32] = _pcol(np.asarray(g_norm1)[0])
        vecs[:, OFF_G2:OFF_G2 + 32] = _pcol(np.asarray(g_norm2)[0])
        vecs[:, OFF_GF:OFF_GF + 32] = _pcol(np.asarray(g_final))
        vecs[:, OFF_GH:OFF_GH + 16] = np.asarray(g_attn_head, np.float32)[0].T
        vecs[:, OFF_SP:OFF_SP + 16] = _pcol(np.asarray(s_pool)[0])
        vecs[:, OFF_FLAG] = float(par)
        for g, w in enumerate(POOL_W):
            cntv = np.minimum(np.arange(16) + 1, w) if par == 0 else np.full(16, w)
            vecs[:, OFF_INVC + g * 16:OFF_INVC + (g + 1) * 16] = (1.0 / cntv.astype(np.float32))[None, :]
        m = dict(shared)
        m["xT"] = loc
        m["cT"] = _pcol(np.asarray(c, np.float32)[b])
        m["vecs"] = vecs
        in_maps.append(m)
    return in_maps


def assemble_output(results, cores=range(8)):
    out = np.zeros((4, 4096, D), np.float32)
    for core, r in zip(cores, results):
        b, par = core // 2, core % 2
        o = np.asarray(r["outT"])
        for j in range(NOWN):
            tile = 2 * j + par
            out[b, tile * T:(tile + 1) * T, :] = o[j].transpose(2, 1, 0).reshape(T, D)
    return out


_NC_CACHE = {}


def kernel(**inputs):
    if "nc" not in _NC_CACHE:
        _NC_CACHE["nc"] = build_program()
    nc = _NC_CACHE["nc"]
    in_maps = prepare_inputs(**inputs)
    res = run_bass_kernel_spmd(nc, in_maps, core_ids=list(range(8)))
    return assemble_output(res.results)
```

```python
import numpy as np
from contextlib import ExitStack
import concourse.bass as bass
import concourse.mybir as mybir
from concourse.bass_utils import run_bass_kernel_spmd

F32 = mybir.dt.float32
BF16 = mybir.dt.bfloat16
AF = mybir.ActivationFunctionType
ALU = mybir.AluOpType

D = 4096
NDC = 32
T = 512
NSLOT = 8
NOWN = 4
NH = 16
EPS = 1e-6
NEB = 128
GRP = 4
NGRP = NEB // GRP
POOL_W = (2, 4, 8, 16)
NVEC = 192 + 32 * 3 + 16 + 16 + 1 + 64
OFF_BADA, OFF_G1, OFF_G2, OFF_GF, OFF_GH, OFF_SP, OFF_FLAG, OFF_INVC = 0, 192, 224, 256, 288, 304, 320, 321


class _Buf:
    def __init__(self, name):
        self.name = name
        self.last_write = None
        self.reads = {}
        self.dsem = None


_BUFS = {}


def Buf(name):
    if name not in _BUFS:
        _BUFS[name] = _Buf(name)
    return _BUFS[name]


class Sched:
    def __init__(self, nc, stack):
        self.nc = nc
        self.stack = stack
        self.eng = {"pe": nc.tensor, "act": nc.scalar, "dve": nc.vector, "pool": nc.gpsimd, "sp": nc.sync}
        self.sem = {}
        self.cnt = {}
        for e in ("pe", "act", "dve", "pool"):
            self.sem[e] = stack.enter_context(nc.semaphore("s_" + e))
            self.cnt[e] = 0
        self.waited = {e: {} for e in self.eng}
        self.dcnt = {}
        self.dsems = []

    def _wait(self, engine, ev):
        if ev[0] == "c":
            _, e, v = ev
            if e == "pe" and engine == "pe":
                return
            key = ("c", e)
            sem = self.sem[e]
        else:
            b = ev[1]
            key = ("d", b.name)
            sem = b.dsem
            v = self.dcnt[b.name]
        if self.waited[engine].get(key, 0) >= v:
            return
        self.waited[engine][key] = v
        self.eng[engine].wait_ge(sem, v)

    def _deps(self, engine, reads, writes):
        for b in reads:
            if b.last_write is not None:
                self._wait(engine, b.last_write)
        for b in writes:
            if b.last_write is not None:
                self._wait(engine, b.last_write)
            for ev in list(b.reads.values()):
                self._wait(engine, ev)

    def _commit(self, ev, reads, writes):
        key = (ev[0], ev[1]) if ev[0] == "c" else ("d", ev[1].name)
        for b in reads:
            if b not in writes:
                b.reads[key] = ev
        for b in writes:
            b.last_write = ev
            b.reads = {}

    def op(self, engine, fn, reads=(), writes=()):
        reads = list(reads)
        writes = list(writes)
        self._deps(engine, reads, writes)
        ins = fn(self.eng[engine])
        ins.then_inc(self.sem[engine], 1)
        self.cnt[engine] += 1
        self._commit(("c", engine, self.cnt[engine]), reads, writes)
        return ins

    def dma(self, queue, out, in_, owner, reads=(), writes=(), **kw):
        reads = list(reads)
        writes = list(writes)
        if owner.dsem is None:
            owner.dsem = self.stack.enter_context(self.nc.semaphore("d_" + owner.name))
            self.dcnt[owner.name] = 0
            self.dsems.append(owner)
        self._deps(queue, reads, writes)
        ins = self.eng[queue].dma_start(out=out, in_=in_, **kw)
        ins.then_inc(owner.dsem, 16)
        self.dcnt[owner.name] += 16
        self._commit(("d", owner), reads, writes)
        return ins

    def barrier(self):
        for q in ("pe", "act", "dve", "pool", "sp"):
            for e in ("pe", "act", "dve", "pool"):
                if self.cnt[e] > 0 and e != q:
                    self._wait(q, ("c", e, self.cnt[e]))
            for b in self.dsems:
                self._wait(q, ("d", b))

    def finish(self):
        for e in ("pe", "act", "dve", "pool"):
            if self.cnt[e] > 0:
                self._wait("sp", ("c", e, self.cnt[e]))
        for b in self.dsems:
            self._wait("sp", ("d", b))


def build_program(stop_after=99, debug=False):
    nc = bass.Bass("TRN2", target_bir_lowering=False)
    _BUFS.clear()
    uniq = [0]

    def din(name, shape):
        return nc.dram_tensor(name, shape, F32, kind="ExternalInput").ap()

    def dscr(name, shape, dt=BF16):
        kind = "ExternalOutput" if debug else "Internal"
        return nc.dram_tensor(name, shape, dt, kind=kind).ap()

    xT = din("xT", [NSLOT, 128, NDC, T])
    cT = din("cT", [128, NDC])
    w_ada = din("w_ada", [D, 6 * D])
    vecs = din("vecs", [128, NVEC])
    consts = din("consts", [128, 5, 128])
    w_in = din("w_in", [D, 2 * D])
    w_pool = din("w_pool", [4, 512, 512])
    w_out = din("w_out", [D, D])
    w_query = din("w_query", [D, 2048])
    skT = din("skT", [128, 2, 128])
    uT = din("uT", [NEB, 128, NDC * 128])
    vE = din("vE", [NEB * 128, D])
    outT = nc.dram_tensor("outT", [NOWN, 128, NDC, T], F32, kind="ExternalOutput").ap()

    hT_s = dscr("hT_s", [NSLOT, 128, NDC, T])
    QT_s = dscr("QT_s", [NH, 128, NOWN * T])
    KT_s = dscr("KT_s", [NH, 128, NSLOT * T])
    V_s = dscr("V_s", [NSLOT * 4, 128, 2048])
    oT_s = dscr("oT_s", [NOWN, 128, NDC, T])
    if debug:
        dbg_mod = nc.dram_tensor("dbg_mod", [128, 192], F32, kind="ExternalOutput").ap()
        dbg_x1 = nc.dram_tensor("dbg_x1", [NOWN, 128, NDC, T], F32, kind="ExternalOutput").ap()
        dbg_h2 = nc.dram_tensor("dbg_h2", [NOWN, 128, NDC, T], BF16, kind="ExternalOutput").ap()
        dbg_s12 = nc.dram_tensor("dbg_s12", [NOWN, 128, 4, 8, 256], F32, kind="ExternalOutput").ap()
        dbg_td = nc.dram_tensor("dbg_td", [NOWN, 128, 4, 8, 2], F32, kind="ExternalOutput").ap()
    BhT_s, BQT_s, BKT_s, BV_s, BoT_s, Bout = [Buf(n) for n in ("hT_s", "QT_s", "KT_s", "V_s", "oT_s", "outd")]
    Bdbg = Buf("dbg")

    with ExitStack() as gs:
        S = Sched(nc, gs)

        def sb(st, name, shape, dt):
            uniq[0] += 1
            return st.enter_context(nc.sbuf_tensor(f"{name}_{uniq[0]}", shape, dt))

        ps = [gs.enter_context(nc.psum_tensor(f"ps{i}", [128, 512], F32)) for i in range(8)]
        P = [Buf(f"ps{i}") for i in range(8)]

        cst = sb(gs, "cst", [128, 5, 128], BF16)
        Bcst = Buf("cst")
        S.dma("pool", cst[:], consts, Bcst, writes=[Bcst])
        ones_bf, negones_bf, ident_bf, negU_bf, tri_bf = [cst[:, i, :] for i in range(5)]
        vec = sb(gs, "vec", [128, NVEC], F32)
        Bvec = Buf("vec")
        S.dma("sp", vec[:], vecs, Bvec, writes=[Bvec])
        modv = sb(gs, "modv", [128, 192], F32)
        A12 = sb(gs, "A12", [128, 64], F32)
        Bmod = Buf("modv")
        BA12 = Buf("A12")
        flag = vec[:, OFF_FLAG:OFF_FLAG + 1]

        with ExitStack() as ph:
            cT_sb = sb(ph, "cT_sb", [128, NDC], F32)
            sc = sb(ph, "sc", [128, NDC], BF16)
            wa = [sb(ph, f"wa{i}", [128, NDC, 512], BF16) for i in range(2)]
            Bc, Bsc = Buf("cT_sb"), Buf("sc")
            Bwa = [Buf("wa0"), Buf("wa1")]
            S.dma("sp", cT_sb[:], cT, Bc, writes=[Bc])
            S.op("act", lambda e: e.activation(out=sc[:], in_=cT_sb[:], func=AF.Silu), reads=[Bc], writes=[Bsc])
            for ct in range(48):
                k = ct % 2
                S.dma("pool", wa[k][:], w_ada[:, ct * 512:(ct + 1) * 512].rearrange("(dc p) n -> p dc n", p=128),
                      Bwa[k], writes=[Bwa[k]], max_dma_last_dim=8192)
                for mm in range(4):
                    j = ct * 4 + mm
                    for dc in range(NDC):
                        S.op("pe", lambda e: e.matmul(ps[0][:, j:j + 1], lhsT=wa[k][:, dc, mm * 128:(mm + 1) * 128],
                                                      rhs=sc[:, dc:dc + 1], start=(dc == 0), stop=(dc == NDC - 1)),
                             reads=[Bwa[k], Bsc], writes=[P[0]])
            S.op("dve", lambda e: e.tensor_tensor(out=modv[:], in0=ps[0][:, 0:192], in1=vec[:, OFF_BADA:OFF_BADA + 192],
                                                  op=ALU.add), reads=[P[0], Bvec], writes=[Bmod])
            S.op("dve", lambda e: e.scalar_tensor_tensor(out=A12[:, 0:32], in0=modv[:, 32:64], scalar=1.0, op0=ALU.add,
                                                         in1=vec[:, OFF_G1:OFF_G1 + 32], op1=ALU.mult),
                 reads=[Bmod, Bvec], writes=[BA12])
            S.op("dve", lambda e: e.scalar_tensor_tensor(out=A12[:, 32:64], in0=modv[:, 128:160], scalar=1.0, op0=ALU.add,
                                                         in1=vec[:, OFF_G2:OFF_G2 + 32], op1=ALU.mult),
                 reads=[Bmod, Bvec], writes=[BA12])
            if debug:
                S.dma("sp", dbg_mod, modv[:], Bmod, reads=[Bmod], writes=[Bdbg])
            S.barrier()
        A1 = A12[:, 0:32]
        A2 = A12[:, 32:64]
        B1 = modv[:, 0:32]
        gate1 = modv[:, 64:96]
        B2 = modv[:, 96:128]
        gate2 = modv[:, 160:192]
        Bmv = [Bmod, BA12, Bvec]

        def emit_rstd(st_bufs, Xt, BXs, rstd, Brstd, bank, n_part_inv):
            sq, Bsq = st_bufs
            for dc in range(NDC):
                k = dc % 2
                S.op("act", lambda e: e.activation(out=sq[k][:], in_=Xt[:, dc, :], func=AF.Square),
                     reads=[BXs[dc // 8]], writes=[Bsq[k]])
                S.op("pe", lambda e: e.matmul(ps[bank][:], lhsT=ones_bf, rhs=sq[k][:], start=(dc == 0), stop=(dc == NDC - 1)),
                     reads=[Bsq[k], Bcst], writes=[P[bank]])
            S.op("act", lambda e: e.activation(out=rstd[:], in_=ps[bank][:], func=AF.Sqrt, scale=n_part_inv, bias=EPS),
                 reads=[P[bank]], writes=[Brstd])
            S.op("dve", lambda e: e.reciprocal(out=rstd[:], in_=rstd[:]), reads=[Brstd], writes=[Brstd])

        def emit_norm_mod(Xt, BXs, rstd, Brstd, Acol, Bcol, tmp, Btmp, hT, BhT):
            for dc in range(NDC):
                k = dc % 2
                S.op("dve", lambda e: e.scalar_tensor_tensor(out=tmp[k][:], in0=Xt[:, dc, :], scalar=Acol[:, dc:dc + 1],
                                                             op0=ALU.mult, in1=rstd[:], op1=ALU.mult),
                     reads=[BXs[dc // 8], Brstd] + Bmv, writes=[Btmp[k]])
                S.op("act", lambda e: e.activation(out=hT[:, dc, :], in_=tmp[k][:], func=AF.Identity,
                                                   bias=Bcol[:, dc:dc + 1]),
                     reads=[Btmp[k]] + Bmv, writes=[BhT])

        if stop_after >= 1:
            with ExitStack() as ph:
                X = sb(ph, "X1", [128, NDC, T], F32)
                BX = [Buf(f"X1_{i}") for i in range(4)]
                sq = [sb(ph, f"sq{i}", [128, T], BF16) for i in range(2)]
                Bsq = [Buf("sq0"), Buf("sq1")]
                rstd = sb(ph, "rstd", [128, T], F32)
                Brstd = Buf("rstd")
                tmp = [sb(ph, f"tmp{i}", [128, T], F32) for i in range(2)]
                Btmp = [Buf("tmp0"), Buf("tmp1")]
                hT = [sb(ph, f"hT{i}", [128, NDC, T], BF16) for i in range(2)]
                BhT = [Buf("hT0"), Buf("hT1")]
                for s in range(NSLOT):
                    for qd in range(4):
                        S.dma("sp", X[:, qd * 8:(qd + 1) * 8, :], xT[s, :, qd * 8:(qd + 1) * 8, :], BX[qd], writes=[BX[qd]])
                    emit_rstd((sq, Bsq), X, BX, rstd, Brstd, s % 2, 1.0 / D)
                    emit_norm_mod(X, BX, rstd, Brstd, A1, B1, tmp, Btmp, hT[s % 2], BhT[s % 2])
                    S.dma("act", hT_s[s], hT[s % 2][:], BhT[s % 2], reads=[BhT[s % 2]], writes=[BhT_s])
                S.barrier()

        if stop_after >= 2:
            with ExitStack() as ph:
                Wg = [sb(ph, f"Wg{i}", [128, NDC, 512], BF16) for i in range(2)]
                BWg = [Buf("Wg0"), Buf("Wg1")]
                hTt = [sb(ph, f"hTt{i}", [128, NDC, T], BF16) for i in range(2)]
                BhTt = [Buf("hTt0"), Buf("hTt1")]
                stg = [sb(ph, f"stg{i}", [128, 512], BF16) for i in range(4)]
                Bstg = [Buf(f"stg{i}") for i in range(4)]
                Pb = [sb(ph, f"Pb{i}", [128, 528], F32) for i in range(4)]
                BPb = [Buf(f"Pb{i}") for i in range(4)]
                T1 = sb(ph, "T1", [128, 528], F32)
                T2 = sb(ph, "T2", [128, 528], F32)
                BT1, BT2 = Buf("T1"), Buf("T2")
                dT = [sb(ph, f"dT{i}", [128, 512], BF16) for i in range(4)]
                BdT = [Buf(f"dT{i}") for i in range(4)]
                d16 = sb(ph, "d16", [128, 16], F32)
                Bd16 = Buf("d16")
                hh = sb(ph, "hh", [128, NDC, 16], BF16)
                Bhh = Buf("hh")
                wp = sb(ph, "wp", [128, 4, 512], BF16)
                Bwp = Buf("wp")
                nload = [0]
                nps = [0]
                nst = [0]

                def load_h(slot):
                    k = nload[0] % 2
                    nload[0] += 1
                    S.dma("sp", hTt[k][:], hT_s[slot], BhTt[k], reads=[BhT_s], writes=[BhTt[k]])
                    return k

                for cg in range(16):
                    kw = cg % 2
                    S.dma("pool", Wg[kw][:], w_in[:, cg * 512:(cg + 1) * 512].rearrange("(dc p) n -> p dc n", p=128),
                          BWg[kw], writes=[BWg[kw]], max_dma_last_dim=8192)
                    kind = cg // 4
                    sub = cg % 4
                    if kind in (0, 1):
                        slots = [1, 3, 5, 7] if kind == 0 else list(range(NSLOT))
                        for si, slot in enumerate(slots):
                            kh = load_h(slot)
                            for hx in range(4):
                                head = sub * 4 + hx
                                r = nps[0] % 4
                                nps[0] += 1
                                for dc in range(NDC):
                                    S.op("pe", lambda e: e.matmul(ps[r][:], lhsT=Wg[kw][:, dc, hx * 128:(hx + 1) * 128],
                                                                  rhs=hTt[kh][:, dc, :], start=(dc == 0), stop=(dc == NDC - 1)),
                                         reads=[BWg[kw], BhTt[kh]], writes=[P[r]])
                                q = nst[0] % 4
                                nst[0] += 1
                                sc_ = (128.0 ** -0.5) if kind == 0 else 1.0
                                S.op("act", lambda e: e.activation(out=stg[q][:], in_=ps[r][:], func=AF.Copy, scale=sc_),
                                     reads=[P[r]], writes=[Bstg[q]])
                                if kind == 0:
                                    S.dma("act", QT_s[head, :, si * T:(si + 1) * T], stg[q][:], Bstg[q], reads=[Bstg[q]], writes=[BQT_s])
                                else:
                                    S.dma("act", KT_s[head, :, slot * T:(slot + 1) * T], stg[q][:], Bstg[q], reads=[Bstg[q]], writes=[BKT_s])
                    elif kind == 2:
                        for slot in range(NSLOT):
                            kh = load_h(slot)
                            for tb in range(4):
                                r = nps[0] % 4
                                nps[0] += 1
                                for dc in range(NDC):
                                    S.op("pe", lambda e: e.matmul(ps[r][:], lhsT=hTt[kh][:, dc, tb * 128:(tb + 1) * 128],
                                                                  rhs=Wg[kw][:, dc, :], start=(dc == 0), stop=(dc == NDC - 1)),
                                         reads=[BWg[kw], BhTt[kh]], writes=[P[r]])
                                q = nst[0] % 4
                                nst[0] += 1
                                if slot == 0:
                                    S.op("act", lambda e: e.activation(out=stg[q][:], in_=ps[r][:], func=AF.Identity, scale=flag),
                                         reads=[P[r], Bvec], writes=[Bstg[q]])
                                else:
                                    S.op("act", lambda e: e.activation(out=stg[q][:], in_=ps[r][:], func=AF.Copy),
                                         reads=[P[r]], writes=[Bstg[q]])
                                S.dma("act", V_s[slot * 4 + tb, :, sub * 512:(sub + 1) * 512], stg[q][:], Bstg[q],
                                      reads=[Bstg[q]], writes=[BV_s])
                    else:
                        g = sub
                        wdw = POOL_W[g]
                        S.dma("pool", wp[:], w_pool[g].rearrange("(cc p) n -> p cc n", p=128), Bwp, writes=[Bwp])
                        for j in range(NOWN):
                            S.dma("sp", hh[:], hT_s[2 * j, :, :, T - 16:T], Bhh, reads=[BhT_s], writes=[Bhh])
                            kh = load_h(2 * j + 1)
                            for cc in range(4):
                                r = nps[0] % 4
                                nps[0] += 1
                                for dc in range(NDC):
                                    S.op("pe", lambda e: e.matmul(ps[r][:, 0:16], lhsT=Wg[kw][:, dc, cc * 128:(cc + 1) * 128],
                                                                  rhs=hh[:, dc, :], start=(dc == 0), stop=(dc == NDC - 1)),
                                         reads=[BWg[kw], Bhh], writes=[P[r]])
                                if j == 0:
                                    S.op("act", lambda e: e.activation(out=Pb[cc][:, 0:16], in_=ps[r][:, 0:16], func=AF.Identity, scale=flag),
                                         reads=[P[r], Bvec], writes=[BPb[cc]])
                                else:
                                    S.op("act", lambda e: e.activation(out=Pb[cc][:, 0:16], in_=ps[r][:, 0:16], func=AF.Copy),
                                         reads=[P[r]], writes=[BPb[cc]])
                                r = nps[0] % 4
                                nps[0] += 1
                                for dc in range(NDC):
                                    S.op("pe", lambda e: e.matmul(ps[r][:], lhsT=Wg[kw][:, dc, cc * 128:(cc + 1) * 128],
                                                                  rhs=hTt[kh][:, dc, :], start=(dc == 0), stop=(dc == NDC - 1)),
                                         reads=[BWg[kw], BhTt[kh]], writes=[P[r]])
                                S.op("act", lambda e: e.activation(out=Pb[cc][:, 16:528], in_=ps[r][:], func=AF.Copy),
                                     reads=[P[r]], writes=[BPb[cc]])
                                S.op("dve", lambda e: e.tensor_tensor(out=T1[:, 1:528], in0=Pb[cc][:, 1:528], in1=Pb[cc][:, 0:527], op=ALU.add),
                                     reads=[BPb[cc]], writes=[BT1])
                                ws, Bws = T1, BT1
                                if wdw >= 4:
                                    S.op("dve", lambda e: e.tensor_tensor(out=T2[:, 3:528], in0=T1[:, 3:528], in1=T1[:, 1:526], op=ALU.add),
                                         reads=[BT1], writes=[BT2])
                                    ws, Bws = T2, BT2
                                if wdw >= 8:
                                    S.op("dve", lambda e: e.tensor_tensor(out=T1[:, 7:528], in0=T2[:, 7:528], in1=T2[:, 3:524], op=ALU.add),
                                         reads=[BT2], writes=[BT1])
                                    ws, Bws = T1, BT1
                                if wdw >= 16:
                                    S.op("dve", lambda e: e.tensor_tensor(out=T2[:, 15:528], in0=T1[:, 15:528], in1=T1[:, 7:520], op=ALU.add),
                                         reads=[BT1], writes=[BT2])
                                    ws, Bws = T2, BT2
                                S.op("dve", lambda e: e.scalar_tensor_tensor(out=dT[cc][:], in0=ws[:, 16:528], scalar=1.0 / wdw, op0=ALU.mult,
                                                                             in1=Pb[cc][:, 16:528], op1=ALU.subtract),
                                     reads=[Bws, BPb[cc]], writes=[BdT[cc]])
                                if j == 0:
                                    S.op("dve", lambda e: e.tensor_tensor(out=d16[:], in0=ws[:, 16:32],
                                                                          in1=vec[:, OFF_INVC + g * 16:OFF_INVC + (g + 1) * 16], op=ALU.mult),
                                         reads=[Bws, Bvec], writes=[Bd16])
                                    S.op("dve", lambda e: e.tensor_tensor(out=dT[cc][:, 0:16], in0=d16[:], in1=Pb[cc][:, 16:32], op=ALU.subtract),
                                         reads=[Bd16, BPb[cc]], writes=[BdT[cc]])
                            for dd in range(4):
                                r = nps[0] % 4
                                nps[0] += 1
                                for cc in range(4):
                                    S.op("pe", lambda e: e.matmul(ps[r][:], lhsT=wp[:, cc, dd * 128:(dd + 1) * 128], rhs=dT[cc][:],
                                                                  start=(cc == 0), stop=(cc == 3)),
                                         reads=[Bwp, BdT[cc]], writes=[P[r]])
                                q = nst[0] % 4
                                nst[0] += 1
                                col = OFF_SP + g * 4 + dd
                                S.op("act", lambda e: e.activation(out=stg[q][:], in_=ps[r][:], func=AF.Identity, scale=vec[:, col:col + 1]),
                                     reads=[P[r], Bvec], writes=[Bstg[q]])
                                S.dma("act", oT_s[j, :, 16 + g * 4 + dd, :], stg[q][:], Bstg[q], reads=[Bstg[q]], writes=[BoT_s])
                S.barrier()

        if stop_after >= 3:
            with ExitStack() as ph:
                KTh = [sb(ph, f"KTh{i}", [128, NSLOT * T], BF16) for i in range(2)]
                Vh = [sb(ph, f"Vh{i}", [128, NSLOT * 4, 128], BF16) for i in range(2)]
                QTh = [sb(ph, f"QTh{i}", [128, NOWN * T], BF16) for i in range(2)]
                BKTh = [Buf("KTh0"), Buf("KTh1")]
                BVh = [Buf("Vh0"), Buf("Vh1")]
                BQTh = [Buf("QTh0"), Buf("QTh1")]
                E = [sb(ph, f"E{i}", [128, T], F32) for i in range(2)]
                BE = [Buf("E0"), Buf("E1")]
                Lp = [sb(ph, f"Lp{i}", [128, T], BF16) for i in range(3)]
                BLp = [Buf(f"Lp{i}") for i in range(3)]
                Ls = sb(ph, "Ls", [128, T], BF16)
                BLs = Buf("Ls")
                Aa = [sb(ph, f"Aa{i}", [128, T], BF16) for i in range(2)]
                BAa = [Buf("Aa0"), Buf("Aa1")]
                sqa = sb(ph, "sqa", [128, T], BF16)
                Bsqa = Buf("sqa")
                rsa = sb(ph, "rsa", [128, T], F32)
                Brsa = Buf("rsa")
                ost = [sb(ph, f"ost{i}", [128, T], BF16) for i in range(2)]
                Bost = [Buf("ost0"), Buf("ost1")]

                units = []
                for h in range(NH):
                    for j in range(NOWN):
                        qs = 2 * j + 1
                        kbs = list(range(qs * 4 + 3, -1, -1))
                        for ui, kb in enumerate(kbs):
                            c0 = (kb - qs * 4) * 128 if kb >= qs * 4 else 0
                            units.append(dict(h=h, j=j, kb=kb, c0=c0, first=(ui == 0), last=(ui == len(kbs) - 1),
                                              diag=(kb >= qs * 4)))

                def load_head(h):
                    k = h % 2
                    S.dma("sp", KTh[k][:], KT_s[h], BKTh[k], reads=[BKT_s], writes=[BKTh[k]])
                    S.dma("sp", QTh[k][:], QT_s[h], BQTh[k], reads=[BQT_s], writes=[BQTh[k]])
                    S.dma("sp", Vh[k][:], V_s[:, :, h * 128:(h + 1) * 128].rearrange("tb p d -> p tb d"), BVh[k],
                          reads=[BV_s], writes=[BVh[k]])

                def stage0(i, u):
                    hk = u["h"] % 2
                    zb = i % 4
                    c0 = u["c0"]
                    qcol = u["j"] * T
                    S.op("pe", lambda e: e.matmul(ps[zb][:, c0:T], lhsT=KTh[hk][:, u["kb"] * 128:(u["kb"] + 1) * 128],
                                                  rhs=QTh[hk][:, qcol + c0:qcol + T], start=True, stop=True),
                         reads=[BKTh[hk], BQTh[hk]], writes=[P[zb]])

                def stage1(i, u):
                    zb = i % 4
                    c0 = u["c0"]
                    S.op("act", lambda e: e.activation(out=E[i % 2][:, c0:T], in_=ps[zb][:, c0:T], func=AF.Exp),
                         reads=[P[zb]], writes=[BE[i % 2]])
                    S.op("act", lambda e: e.activation(out=Lp[i % 3][:, c0:T], in_=E[i % 2][:, c0:T], func=AF.Ln, bias=1.0),
                         reads=[BE[i % 2]], writes=[BLp[i % 3]])
                    if u["diag"]:
                        S.op("dve", lambda e: e.tensor_tensor(out=Lp[i % 3][:, c0:c0 + 128], in0=Lp[i % 3][:, c0:c0 + 128],
                                                              in1=tri_bf, op=ALU.mult),
                             reads=[BLp[i % 3], Bcst], writes=[BLp[i % 3]])

                def stage2(i, u):
                    zb = i % 4
                    c0 = u["c0"]
                    S.op("pe", lambda e: e.matmul(ps[zb][:, c0:T], lhsT=negU_bf, rhs=Lp[i % 3][:, c0:T], start=False, stop=u["first"],
                                                  skip_group_check=True),
                         reads=[BLp[i % 3], Bcst], writes=[P[zb]])
                    if u["first"]:
                        S.op("pool", lambda e: e.memset(Ls[:], 0.0), writes=[BLs])
                    else:
                        S.op("pe", lambda e: e.matmul(ps[zb][:, c0:T], lhsT=negones_bf, rhs=Ls[:, c0:T], start=False, stop=True,
                                                      skip_group_check=True),
                             reads=[BLs, Bcst], writes=[P[zb]])
                    if not u["last"]:
                        S.op("pool", lambda e: e.tensor_tensor(out=Ls[:, c0:T], in0=Ls[:, c0:T], in1=Lp[i % 3][:, c0:T], op=ALU.add),
                             reads=[BLs, BLp[i % 3]], writes=[BLs])

                def stage3(i, u):
                    hk = u["h"] % 2
                    zb = i % 4
                    c0 = u["c0"]
                    ob = 4 + (u["h"] * NOWN + u["j"]) % 2
                    S.op("act", lambda e: e.activation(out=Aa[i % 2][:, c0:T], in_=ps[zb][:, c0:T], func=AF.Exp),
                         reads=[P[zb]], writes=[BAa[i % 2]])
                    if u["diag"]:
                        S.op("dve", lambda e: e.tensor_tensor(out=Aa[i % 2][:, c0:c0 + 128], in0=Aa[i % 2][:, c0:c0 + 128],
                                                              in1=tri_bf, op=ALU.mult),
                             reads=[BAa[i % 2], Bcst], writes=[BAa[i % 2]])
                    S.op("pe", lambda e: e.matmul(ps[ob][:, c0:T], lhsT=Vh[hk][:, u["kb"], :], rhs=Aa[i % 2][:, c0:T],
                                                  start=u["first"], stop=u["last"], skip_group_check=True),
                         reads=[BVh[hk], BAa[i % 2]], writes=[P[ob]])
                    if u["last"]:
                        h, j = u["h"], u["j"]
                        k = (h * NOWN + j) % 2
                        S.op("act", lambda e: e.activation(out=sqa[:], in_=ps[ob][:], func=AF.Square), reads=[P[ob]], writes=[Bsqa])
                        S.op("pe", lambda e: e.matmul(ps[6][:], lhsT=ones_bf, rhs=sqa[:], start=True, stop=True),
                             reads=[Bsqa, Bcst], writes=[P[6]])
                        S.op("act", lambda e: e.activation(out=rsa[:], in_=ps[6][:], func=AF.Sqrt, scale=1.0 / 128, bias=EPS),
                             reads=[P[6]], writes=[Brsa])
                        S.op("dve", lambda e: e.reciprocal(out=rsa[:], in_=rsa[:]), reads=[Brsa], writes=[Brsa])
                        S.op("dve", lambda e: e.scalar_tensor_tensor(out=ost[k][:], in0=ps[ob][:], scalar=vec[:, OFF_GH + h:OFF_GH + h + 1],
                                                                     op0=ALU.mult, in1=rsa[:], op1=ALU.mult),
                             reads=[P[ob], Brsa, Bvec], writes=[Bost[k]])
                        S.dma("sp", oT_s[j, :, h, :], ost[k][:], Bost[k], reads=[Bost[k]], writes=[BoT_s])

                load_head(0)
                n = len(units)
                for i in range(n + 3):
                    if i < n:
                        u = units[i]
                        if u["first"] and u["j"] == 0 and u["h"] + 1 < NH:
                            load_head(u["h"] + 1)
                        stage0(i, u)
                    if 0 <= i - 1 < n:
                        stage1(i - 1, units[i - 1])
                    if 0 <= i - 2 < n:
                        stage2(i - 2, units[i - 2])
                    if 0 <= i - 3 < n:
                        stage3(i - 3, units[i - 3])
                S.barrier()

        if stop_after >= 4:
            with ExitStack() as ph:
                X = sb(ph, "X2", [128, NDC, T], F32)
                BX = [Buf(f"X2_{i}") for i in range(4)]
                h2T = sb(ph, "h2T", [128, NDC, T], BF16)
                Bh2T = Buf("h2T")
                S12 = sb(ph, "S12", [128, 4, 8, 256], F32)
                BS12 = Buf("S12")
                TD = sb(ph, "TD", [128, 4, 8, 2], F32)
                BTD = Buf("TD")
                sq = [sb(ph, f"sqb{i}", [128, T], BF16) for i in range(2)]
                Bsq = [Buf("sqb0"), Buf("sqb1")]
                rstd = sb(ph, "rstd2", [128, T], F32)
                Brstd = Buf("rstd2")
                skb = sb(ph, "skb", [128, 2, 128], BF16)
                Bskb = Buf("skb")
                S.dma("pool", skb[:], skT, Bskb, writes=[Bskb])
                for j in range(NOWN):
                    with ExitStack() as p4:
                        oTt = sb(p4, "oTt", [128, NDC, T], BF16)
                        BoTt = Buf("oTt")
                        Wo = [sb(p4, f"Wo{i}", [128, NDC, 256], BF16) for i in range(2)]
                        BWo = [Buf("Wo0"), Buf("Wo1")]
                        for qd in range(4):
                            S.dma("sp", X[:, qd * 8:(qd + 1) * 8, :], xT[2 * j + 1, :, qd * 8:(qd + 1) * 8, :], BX[qd], writes=[BX[qd]])
                        S.dma("sp", oTt[:], oT_s[j], BoTt, reads=[BoT_s], writes=[BoTt])
                        for cg in range(16):
                            k = cg % 2
                            S.dma("pool", Wo[k][:], w_out[:, cg * 256:(cg + 1) * 256].rearrange("(ec p) n -> p ec n", p=128),
                                  BWo[k], writes=[BWo[k]])
                            for dd in range(2):
                                dch = cg * 2 + dd
                                r = dch % 4
                                for ec in range(NDC):
                                    S.op("pe", lambda e: e.matmul(ps[r][:], lhsT=Wo[k][:, ec, dd * 128:(dd + 1) * 128], rhs=oTt[:, ec, :],
                                                                  start=(ec == 0), stop=(ec == NDC - 1)),
                                         reads=[BWo[k], BoTt], writes=[P[r]])
                                S.op("dve", lambda e: e.scalar_tensor_tensor(out=X[:, dch, :], in0=ps[r][:], scalar=gate1[:, dch:dch + 1],
                                                                             op0=ALU.mult, in1=X[:, dch, :], op1=ALU.add),
                                     reads=[P[r], BX[dch // 8]] + Bmv, writes=[BX[dch // 8]])
                        if debug:
                            S.dma("sp", dbg_x1[j], X[:], BX[0], reads=BX, writes=[Bdbg])
                        S.barrier()
                    if stop_after < 5:
                        continue
                    with ExitStack() as p5:
                        tmp = [sb(p5, f"tmpb{i}", [128, T], F32) for i in range(2)]
                        Btmp = [Buf("tmpb0"), Buf("tmpb1")]
                        Wq = [sb(p5, f"Wq{i}", [128, NDC, 256], BF16) for i in range(2)]
                        BWq = [Buf("Wq0"), Buf("Wq1")]
                        qT = [sb(p5, f"qT{i}", [128, 2, T], BF16) for i in range(2)]
                        BqT = [Buf("qT0"), Buf("qT1")]
                        v16 = sb(p5, "v16", [128, 2, 16], F32)
                        Bv16 = Buf("v16")
                        tmpk = sb(p5, "tmpk", [128, 128], F32)
                        Btmpk = Buf("tmpk")
                        cand = sb(p5, "cand", [128, 256], F32)
                        cand2 = sb(p5, "cand2", [128, 256], F32)
                        Bcand, Bcand2 = Buf("cand"), Buf("cand2")
                        c16 = sb(p5, "c16", [128, 16], F32)
                        Bc16 = Buf("c16")
                        e16 = sb(p5, "e16", [128, 16], F32)
                        Be16 = Buf("e16")
                        sm = sb(p5, "sm", [128, 4], F32)
                        Bsm = Buf("sm")
                        emit_rstd((sq, Bsq), X, BX, rstd, Brstd, 4, 1.0 / D)
                        emit_norm_mod(X, BX, rstd, Brstd, A2, B2, tmp, Btmp, h2T, Bh2T)
                        if debug:
                            S.dma("sp", dbg_h2[j], h2T[:], Bh2T, reads=[Bh2T], writes=[Bdbg])
                        for hq in range(8):
                            k = hq % 2
                            S.dma("pool", Wq[k][:], w_query[:, hq * 256:(hq + 1) * 256].rearrange("(dc p) n -> p dc n", p=128),
                                  BWq[k], writes=[BWq[k]])
                            for cc in range(2):
                                r = cc
                                for dc in range(NDC):
                                    S.op("pe", lambda e: e.matmul(ps[r][:], lhsT=Wq[k][:, dc, cc * 128:(cc + 1) * 128], rhs=h2T[:, dc, :],
                                                                  start=(dc == 0), stop=(dc == NDC - 1)),
                                         reads=[BWq[k], Bh2T], writes=[P[r]])
                                S.op("act", lambda e: e.activation(out=qT[k][:, cc, :], in_=ps[r][:], func=AF.Copy),
                                     reads=[P[r]], writes=[BqT[k]])
                            for tb in range(4):
                                r = 2 + tb % 2
                                for w_ in range(2):
                                    S.op("pe", lambda e: e.matmul(ps[r][:, w_ * 128:(w_ + 1) * 128], lhsT=qT[k][:, w_, tb * 128:(tb + 1) * 128],
                                                                  rhs=skb[:, w_, :], start=True, stop=True, skip_group_check=True),
                                         reads=[BqT[k], Bskb], writes=[P[r]])
                                S.op("act", lambda e: e.activation(out=S12[:, tb, hq, :], in_=ps[r][:, 0:256], func=AF.Copy),
                                     reads=[P[r]], writes=[BS12])
                                for w_ in range(2):
                                    src = S12[:, tb, hq, w_ * 128:(w_ + 1) * 128]
                                    S.op("dve", lambda e: e.max(out=v16[:, w_, 0:8], in_=src), reads=[BS12], writes=[Bv16])
                                    S.op("dve", lambda e: e.match_replace(out=tmpk[:], in_to_replace=v16[:, w_, 0:8], in_values=src,
                                                                          imm_value=-1e30), reads=[BS12, Bv16], writes=[Btmpk])
                                    S.op("dve", lambda e: e.max(out=v16[:, w_, 8:16], in_=tmpk[:]), reads=[Btmpk], writes=[Bv16])
                                S.op("pool", lambda e: e.tensor_tensor(out=cand[:].rearrange("p (a b) -> p a b", a=16),
                                                                       in0=v16[:, 0, :].unsqueeze(2).broadcast_to([128, 16, 16]),
                                                                       in1=v16[:, 1, :].unsqueeze(1).broadcast_to([128, 16, 16]), op=ALU.add),
                                     reads=[Bv16], writes=[Bcand])
                                S.op("dve", lambda e: e.max(out=c16[:, 0:8], in_=cand[:]), reads=[Bcand], writes=[Bc16])
                                S.op("dve", lambda e: e.match_replace(out=cand2[:], in_to_replace=c16[:, 0:8], in_values=cand[:],
                                                                      imm_value=-1e30), reads=[Bcand, Bc16], writes=[Bcand2])
                                S.op("dve", lambda e: e.max(out=c16[:, 8:16], in_=cand2[:]), reads=[Bcand2], writes=[Bc16])
                                S.op("dve", lambda e: e.tensor_scalar(out=sm[:, 0:1], in0=c16[:, 0:1], scalar1=-1.0, scalar2=None, op0=ALU.mult),
                                     reads=[Bc16], writes=[Bsm])
                                S.op("act", lambda e: e.activation(out=e16[:], in_=c16[:], func=AF.Exp, bias=sm[:, 0:1], accum_out=sm[:, 1:2]),
                                     reads=[Bc16, Bsm], writes=[Be16, Bsm])
                                S.op("act", lambda e: e.activation(out=sm[:, 2:3], in_=sm[:, 1:2], func=AF.Ln), reads=[Bsm], writes=[Bsm])
                                S.op("dve", lambda e: e.tensor_tensor(out=TD[:, tb, hq, 1:2], in0=sm[:, 0:1], in1=sm[:, 2:3], op=ALU.subtract),
                                     reads=[Bsm], writes=[BTD])
                                S.op("dve", lambda e: e.tensor_copy(out=TD[:, tb, hq, 0:1], in_=c16[:, 15:16]), reads=[Bc16], writes=[BTD])
                        if debug:
                            S.dma("sp", dbg_s12[j], S12[:], BS12, reads=[BS12], writes=[Bdbg])
                            S.dma("sp", dbg_td[j], TD[:], BTD, reads=[BTD], writes=[Bdbg])
                        S.barrier()
                    if stop_after < 6:
                        continue
                    with ExitStack() as p6:
                        ub = [sb(p6, f"ub{i}", [128, NDC, 128], BF16) for i in range(2)]
                        Bub = [Buf("ub0"), Buf("ub1")]
                        vb = [sb(p6, f"vb{i}", [128, GRP, 2048], BF16) for i in range(2)]
                        Bvb = [Buf("vb0"), Buf("vb1")]
                        actT = [sb(p6, f"actT{i}", [128, T], BF16) for i in range(GRP)]
                        BactT = [Buf(f"actT{i}") for i in range(GRP)]
                        NCB, NGM, LAG = 3, 6, 4
                        Cb = [sb(p6, f"Cb{i}", [128, GRP * 128], F32) for i in range(NCB)]
                        BCb = [Buf(f"Cb{i}") for i in range(NCB)]
                        Gx = [sb(p6, f"Gx{i}", [128, GRP * 128], BF16) for i in range(NCB)]
                        BGx = [Buf(f"Gx{i}") for i in range(NCB)]
                        Gm = [sb(p6, f"Gm{i}", [128, GRP * 128], BF16) for i in range(NGM)]
                        BGm = [Buf(f"Gm{i}") for i in range(NGM)]
                        gl = [sb(p6, f"gl{i}", [128, T], BF16) for i in range(GRP)]
                        Bgl = [Buf(f"gl{i}") for i in range(GRP)]
                        WTB = [0, 1, 2, 3]
                        SB_ = [4, 5]
                        YB = [6, 7]
                        cnt = dict(w=0, s=0, y=0)
                        if j == 0 and debug:
                            print("sbuf bytes remaining in PEER scope:", nc.sbuf_bytes_remaining)
                        pending = []

                        def wbuild_elem(g, pi):
                            tb, hq = pi // 8, pi % 8
                            k = cnt["w"] % NCB
                            km = cnt["w"] % NGM
                            cnt["w"] += 1
                            i0 = g * GRP
                            S.op("pool", lambda e: e.tensor_tensor(
                                out=Cb[k][:].rearrange("p (a b) -> p a b", a=GRP),
                                in0=S12[:, tb, hq, i0:i0 + GRP].unsqueeze(2).broadcast_to([128, GRP, 128]),
                                in1=S12[:, tb, hq, 128:256].unsqueeze(1).broadcast_to([128, GRP, 128]), op=ALU.add),
                                reads=[BS12], writes=[BCb[k]])
                            S.op("act", lambda e: e.activation(out=Gx[k][:], in_=Cb[k][:], func=AF.Exp, bias=TD[:, tb, hq, 1:2]),
                                 reads=[BCb[k], BTD], writes=[BGx[k]])
                            S.op("dve", lambda e: e.scalar_tensor_tensor(out=Gm[km][:], in0=Cb[k][:], scalar=TD[:, tb, hq, 0:1], op0=ALU.is_ge,
                                                                         in1=Gx[k][:], op1=ALU.mult),
                                 reads=[BCb[k], BGx[k], BTD], writes=[BGm[km]])
                            pending.append((km, tb, hq))

                        def emit_tr():
                            km, tb, hq = pending.pop(0)
                            for a in range(GRP):
                                S.op("pe", lambda e: e.matmul(ps[WTB[a]][:, tb * 128:(tb + 1) * 128], lhsT=Gm[km][:, a * 128:(a + 1) * 128],
                                                              rhs=ident_bf, start=(hq == 0), stop=(hq == 7), skip_group_check=True),
                                     reads=[BGm[km], Bcst], writes=[P[WTB[a]]])

                        def load_u(eb):
                            k = eb % 2
                            S.dma("pool", ub[k][:].rearrange("p a b -> p (a b)"), uT[eb], Bub[k], writes=[Bub[k]], max_dma_last_dim=8192)

                        def load_v(g, hf):
                            S.dma("pool", vb[hf][:], vE[g * GRP * 128:(g + 1) * GRP * 128, hf * 2048:(hf + 1) * 2048].rearrange("(a p) d -> p a d", p=128),
                                  Bvb[hf], writes=[Bvb[hf]], max_dma_last_dim=8192)

                        def s_block(eb, dcs):
                            ku = eb % 2
                            sbk = SB_[eb % 2]
                            for dc in dcs:
                                S.op("pe", lambda e: e.matmul(ps[sbk][:], lhsT=ub[ku][:, dc, :], rhs=h2T[:, dc, :],
                                                              start=(dc == 0), stop=(dc == NDC - 1)),
                                     reads=[Bub[ku], Bh2T], writes=[P[sbk]])

                        def s_finish(eb):
                            sbk = SB_[eb % 2]
                            a_ = eb % GRP
                            S.op("act", lambda e: e.activation(out=gl[a_][:], in_=ps[sbk][:], func=AF.Gelu),
                                 reads=[P[sbk]], writes=[Bgl[a_]])
                            if eb + 2 < NEB:
                                load_u(eb + 2)

                        def boundary():
                            while pending:
                                emit_tr()
                            for a_ in range(GRP):
                                S.op("dve", lambda e: e.tensor_tensor(out=actT[a_][:], in0=gl[a_][:], in1=ps[WTB[a_]][:], op=ALU.mult),
                                     reads=[Bgl[a_], P[WTB[a_]]], writes=[BactT[a_]])

                        load_u(0)
                        load_u(1)
                        load_v(0, 0)
                        load_v(0, 1)
                        for pi in range(32):
                            wbuild_elem(0, pi)
                            emit_tr()
                        for a in range(GRP):
                            s_block(a, range(NDC))
                            s_finish(a)
                        boundary()
                        for g in range(NGRP):
                            for dch in range(NDC):
                                hf = dch // 16
                                yb = YB[cnt["y"] % 2]
                                cnt["y"] += 1
                                if dch == 0 and g > 0:
                                    load_v(g, 1)
                                for a in range(GRP):
                                    S.op("pe", lambda e: e.matmul(ps[yb][:], lhsT=vb[hf][:, a, (dch % 16) * 128:(dch % 16 + 1) * 128], rhs=actT[a][:],
                                                                  start=(a == 0), stop=(a == GRP - 1)),
                                         reads=[Bvb[hf], BactT[a]], writes=[P[yb]])
                                S.op("dve", lambda e: e.scalar_tensor_tensor(out=X[:, dch, :], in0=ps[yb][:], scalar=gate2[:, dch:dch + 1],
                                                                             op0=ALU.mult, in1=X[:, dch, :], op1=ALU.add),
                                     reads=[P[yb], BX[dch // 8]] + Bmv, writes=[BX[dch // 8]])
                                if g + 1 < NGRP:
                                    wbuild_elem(g + 1, dch)
                                    if len(pending) > LAG:
                                        emit_tr()
                                    eb = (g + 1) * GRP + dch // 8
                                    s_block(eb, range((dch % 8) * 4, (dch % 8) * 4 + 4))
                                    if dch % 8 == 7:
                                        s_finish(eb)
                                    if dch == 16:
                                        load_v(g + 1, 0)
                            if g + 1 < NGRP:
                                boundary()
                        emit_rstd((sq, Bsq), X, BX, rstd, Brstd, 4, 1.0 / D)
                        ostf = [Cb[0], Cb[1]]
                        Bostf = [BCb[0], BCb[1]]
                        for dc in range(NDC):
                            k = dc % 2
                            S.op("dve", lambda e: e.scalar_tensor_tensor(out=ostf[k][:], in0=X[:, dc, :], scalar=vec[:, OFF_GF + dc:OFF_GF + dc + 1],
                                                                         op0=ALU.mult, in1=rstd[:], op1=ALU.mult),
                                 reads=[BX[dc // 8], Brstd, Bvec], writes=[Bostf[k]])
                            S.dma("sp", outT[j, :, dc, :], ostf[k][:], Bostf[k], reads=[Bostf[k]], writes=[Bout])
                        S.barrier()
        S.finish()
    return nc


def _consts():
    j = np.arange(128)
    ones = np.ones((128, 128), np.float32)
    ident = np.eye(128, dtype=np.float32)
    negU = -(j[:, None] >= j[None, :]).astype(np.float32)
    tri = (j[:, None] < j[None, :]).astype(np.float32)
    return np.ascontiguousarray(np.stack([ones, -ones, ident, negU, tri], axis=1))


def _pcol(v):
    return np.ascontiguousarray(np.asarray(v, np.float32).reshape(-1, 128).T)


def prepare_inputs(x, c, w_ada, b_ada, g_norm1, w_in, g_attn_head, w_pool, s_pool, w_out, g_norm2, w_query,
                   sub_keys_1, sub_keys_2, u_experts, v_experts, g_final, cores=range(8)):
    x = np.asarray(x, np.float32)
    shared = dict(
        w_ada=np.ascontiguousarray(np.asarray(w_ada, np.float32)[0]),
        consts=_consts(),
        w_in=np.ascontiguousarray(np.asarray(w_in, np.float32)[0]),
        w_pool=np.ascontiguousarray(np.asarray(w_pool, np.float32)[0]),
        w_out=np.ascontiguousarray(np.asarray(w_out, np.float32)[0]),
        w_query=np.ascontiguousarray(np.asarray(w_query, np.float32)[0]),
        skT=np.ascontiguousarray(np.stack([np.asarray(sub_keys_1, np.float32)[0].T, np.asarray(sub_keys_2, np.float32)[0].T], axis=1)),
        vE=np.ascontiguousarray(np.asarray(v_experts, np.float32)[0]),
    )
    u = np.asarray(u_experts, np.float32)[0]
    shared["uT"] = np.ascontiguousarray(u.reshape(NEB, 128, NDC, 128).transpose(0, 3, 2, 1)).reshape(NEB, 128, NDC * 128)
    in_maps = []
    for core in cores:
        b, par = core // 2, core % 2
        xb = x[b]
        xt = xb.reshape(NSLOT, T, NDC, 128).transpose(0, 3, 2, 1)
        loc = np.zeros((NSLOT, 128, NDC, T), np.float32)
        if par == 1:
            loc[:] = xt
        else:
            loc[1:] = xt[:NSLOT - 1]
        vecs = np.zeros((128, NVEC), np.float32)
        vecs[:, OFF_BADA:OFF_BADA + 192] = _pcol(np.asarray(b_ada)[0])
        vecs[:, OFF_G1:OFF_G1 + 32] = _pcol(np.asarray(g_norm1)[0])
        vecs[:, OFF_G2:OFF_G2 + 32] = _pcol(np.asarray(g_norm2)[0])
        vecs[:, OFF_GF:OFF_GF + 32] = _pcol(np.asarray(g_final))
        vecs[:, OFF_GH:OFF_GH + 16] = np.asarray(g_attn_head, np.float32)[0].T
        vecs[:, OFF_SP:OFF_SP + 16] = _pcol(np.asarray(s_pool)[0])
        vecs[:, OFF_FLAG] = float(par)
        for g, w in enumerate(POOL_W):
            cntv = np.minimum(np.arange(16) + 1, w) if par == 0 else np.full(16, w)
            vecs[:, OFF_INVC + g * 16:OFF_INVC + (g + 1) * 16] = (1.0 / cntv.astype(np.float32))[None, :]
        m = dict(shared)
        m["xT"] = loc
        m["cT"] = _pcol(np.asarray(c, np.float32)[b])
        m["vecs"] = vecs
        in_maps.append(m)
    return in_maps


def assemble_output(results, cores=range(8)):
    out = np.zeros((4, 4096, D), np.float32)
    for core, r in zip(cores, results):
        b, par = core // 2, core % 2
        o = np.asarray(r["outT"])
        for j in range(NOWN):
            tile = 2 * j + par
            out[b, tile * T:(tile + 1) * T, :] = o[j].transpose(2, 1, 0).reshape(T, D)
    return out


_NC_CACHE = {}


def kernel(**inputs):
    if "nc" not in _NC_CACHE:
        _NC_CACHE["nc"] = build_program()
    nc = _NC_CACHE["nc"]
    in_maps = prepare_inputs(**inputs)
    res = run_bass_kernel_spmd(nc, in_maps, core_ids=list(range(8)))
    return assemble_output(res.results)
```

```python
import numpy as np
from contextlib import ExitStack
import concourse.bass as bass
import concourse.mybir as mybir
from concourse.bass_utils import run_bass_kernel_spmd

F32 = mybir.dt.float32
BF16 = mybir.dt.bfloat16
AF = mybir.ActivationFunctionType
ALU = mybir.AluOpType

D = 4096
NDC = 32
T = 512
NSLOT = 8
NOWN = 4
NH = 16
EPS = 1e-6
NEB = 128
GRP = 4
NGRP = NEB // GRP
POOL_W = (2, 4, 8, 16)
NVEC = 192 + 32 * 3 + 16 + 16 + 1 + 64
OFF_BADA, OFF_G1, OFF_G2, OFF_GF, OFF_GH, OFF_SP, OFF_FLAG, OFF_INVC = 0, 192, 224, 256, 288, 304, 320, 321


class _Buf:
    def __init__(self, name):
        self.name = name
        self.last_write = None
        self.reads = {}
        self.dsem = None


_BUFS = {}


def Buf(name):
    if name not in _BUFS:
        _BUFS[name] = _Buf(name)
    return _BUFS[name]


class Sched:
    def __init__(self, nc, stack):
        self.nc = nc
        self.stack = stack
        self.eng = {"pe": nc.tensor, "act": nc.scalar, "dve": nc.vector, "pool": nc.gpsimd, "sp": nc.sync}
        self.sem = {}
        self.cnt = {}
        for e in ("pe", "act", "dve", "pool"):
            self.sem[e] = stack.enter_context(nc.semaphore("s_" + e))
            self.cnt[e] = 0
        self.waited = {e: {} for e in self.eng}
        self.dcnt = {}
        self.dsems = []

    def _wait(self, engine, ev):
        if ev[0] == "c":
            _, e, v = ev
            if e == "pe" and engine == "pe":
                return
            key = ("c", e)
            sem = self.sem[e]
        else:
            b = ev[1]
            key = ("d", b.name)
            sem = b.dsem
            v = self.dcnt[b.name]
        if self.waited[engine].get(key, 0) >= v:
            return
        self.waited[engine][key] = v
        self.eng[engine].wait_ge(sem, v)

    def _deps(self, engine, reads, writes):
        for b in reads:
            if b.last_write is not None:
                self._wait(engine, b.last_write)
        for b in writes:
            if b.last_write is not None:
                self._wait(engine, b.last_write)
            for ev in list(b.reads.values()):
                self._wait(engine, ev)

    def _commit(self, ev, reads, writes):
        key = (ev[0], ev[1]) if ev[0] == "c" else ("d", ev[1].name)
        for b in reads:
            if b not in writes:
                b.reads[key] = ev
        for b in writes:
            b.last_write = ev
            b.reads = {}

    def op(self, engine, fn, reads=(), writes=()):
        reads = list(reads)
        writes = list(writes)
        self._deps(engine, reads, writes)
        ins = fn(self.eng[engine])
        ins.then_inc(self.sem[engine], 1)
        self.cnt[engine] += 1
        self._commit(("c", engine, self.cnt[engine]), reads, writes)
        return ins

    def dma(self, queue, out, in_, owner, reads=(), writes=(), **kw):
        reads = list(reads)
        writes = list(writes)
        if owner.dsem is None:
            owner.dsem = self.stack.enter_context(self.nc.semaphore("d_" + owner.name))
            self.dcnt[owner.name] = 0
            self.dsems.append(owner)
        self._deps(queue, reads, writes)
        ins = self.eng[queue].dma_start(out=out, in_=in_, **kw)
        ins.then_inc(owner.dsem, 16)
        self.dcnt[owner.name] += 16
        self._commit(("d", owner), reads, writes)
        return ins

    def barrier(self):
        for q in ("pe", "act", "dve", "pool", "sp"):
            for e in ("pe", "act", "dve", "pool"):
                if self.cnt[e] > 0 and e != q:
                    self._wait(q, ("c", e, self.cnt[e]))
            for b in self.dsems:
                self._wait(q, ("d", b))

    def finish(self):
        for e in ("pe", "act", "dve", "pool"):
            if self.cnt[e] > 0:
                self._wait("sp", ("c", e, self.cnt[e]))
        for b in self.dsems:
            self._wait("sp", ("d", b))


def build_program(stop_after=99, debug=False):
    nc = bass.Bass("TRN2", target_bir_lowering=False)
    _BUFS.clear()
    uniq = [0]

    def din(name, shape):
        return nc.dram_tensor(name, shape, F32, kind="ExternalInput").ap()

    def dscr(name, shape, dt=BF16):
        kind = "ExternalOutput" if debug else "Internal"
        return nc.dram_tensor(name, shape, dt, kind=kind).ap()

    xT = din("xT", [NSLOT, 128, NDC, T])
    cT = din("cT", [128, NDC])
    w_ada = din("w_ada", [D, 6 * D])
    vecs = din("vecs", [128, NVEC])
    consts = din("consts", [128, 5, 128])
    w_in = din("w_in", [D, 2 * D])
    w_pool = din("w_pool", [4, 512, 512])
    w_out = din("w_out", [D, D])
    w_query = din("w_query", [D, 2048])
    skT = din("skT", [128, 2, 128])
    uT = din("uT", [NEB, 128, NDC * 128])
    vE = din("vE", [NEB * 128, D])
    outT = nc.dram_tensor("outT", [NOWN, 128, NDC, T], F32, kind="ExternalOutput").ap()

    hT_s = dscr("hT_s", [NSLOT, 128, NDC, T])
    QT_s = dscr("QT_s", [NH, 128, NOWN * T])
    KT_s = dscr("KT_s", [NH, 128, NSLOT * T])
    V_s = dscr("V_s", [NSLOT * 4, 128, 2048])
    oT_s = dscr("oT_s", [NOWN, 128, NDC, T])
    if debug:
        dbg_mod = nc.dram_tensor("dbg_mod", [128, 192], F32, kind="ExternalOutput").ap()
        dbg_x1 = nc.dram_tensor("dbg_x1", [NOWN, 128, NDC, T], F32, kind="ExternalOutput").ap()
        dbg_h2 = nc.dram_tensor("dbg_h2", [NOWN, 128, NDC, T], BF16, kind="ExternalOutput").ap()
        dbg_s12 = nc.dram_tensor("dbg_s12", [NOWN, 128, 4, 8, 256], F32, kind="ExternalOutput").ap()
        dbg_td = nc.dram_tensor("dbg_td", [NOWN, 128, 4, 8, 2], F32, kind="ExternalOutput").ap()
    BhT_s, BQT_s, BKT_s, BV_s, BoT_s, Bout = [Buf(n) for n in ("hT_s", "QT_s", "KT_s", "V_s", "oT_s", "outd")]
    Bdbg = Buf("dbg")

    with ExitStack() as gs:
        S = Sched(nc, gs)

        def sb(st, name, shape, dt):
            uniq[0] += 1
            return st.enter_context(nc.sbuf_tensor(f"{name}_{uniq[0]}", shape, dt))

        ps = [gs.enter_context(nc.psum_tensor(f"ps{i}", [128, 512], F32)) for i in range(8)]
        P = [Buf(f"ps{i}") for i in range(8)]

        cst = sb(gs, "cst", [128, 5, 128], BF16)
        Bcst = Buf("cst")
        S.dma("pool", cst[:], consts, Bcst, writes=[Bcst])
        ones_bf, negones_bf, ident_bf, negU_bf, tri_bf = [cst[:, i, :] for i in range(5)]
        vec = sb(gs, "vec", [128, NVEC], F32)
        Bvec = Buf("vec")
        S.dma("sp", vec[:], vecs, Bvec, writes=[Bvec])
        modv = sb(gs, "modv", [128, 192], F32)
        A12 = sb(gs, "A12", [128, 64], F32)
        Bmod = Buf("modv")
        BA12 = Buf("A12")
        flag = vec[:, OFF_FLAG:OFF_FLAG + 1]

        cT_sb = sb(gs, "cT_sb", [128, NDC], F32)
        sc = sb(gs, "sc", [128, NDC], BF16)
        Bc, Bsc = Buf("cT_sb"), Buf("sc")
        Bwa = [Buf("wa0"), Buf("wa1")]
        S.dma("sp", cT_sb[:], cT, Bc, writes=[Bc])
        S.op("act", lambda e: e.activation(out=sc[:], in_=cT_sb[:], func=AF.Silu), reads=[Bc], writes=[Bsc])

        def mod_load(wa, ct):
            k = ct % 2
            S.dma("pool", wa[k][:], w_ada[:, ct * 512:(ct + 1) * 512].rearrange("(dc p) n -> p dc n", p=128),
                  Bwa[k], writes=[Bwa[k]], max_dma_last_dim=8192)

        def mod_mm(wa, ct, idx, bank, col0):
            k = ct % 2
            mm, dc = idx // NDC, idx % NDC
            jc = ct * 4 + mm - col0
            S.op("pe", lambda e: e.matmul(ps[bank][:, jc:jc + 1], lhsT=wa[k][:, dc, mm * 128:(mm + 1) * 128],
                                          rhs=sc[:, dc:dc + 1], start=(dc == 0), stop=(dc == NDC - 1), skip_group_check=True),
                 reads=[Bwa[k], Bsc], writes=[P[bank]])

        with ExitStack() as ph:
            wa = [sb(ph, f"wa{i}", [128, NDC, 512], BF16) for i in range(2)]
            mod_load(wa, 0)
            for ct in range(16):
                if ct + 1 < 16:
                    mod_load(wa, ct + 1)
                for idx in range(128):
                    mod_mm(wa, ct, idx, 0, 0)
            S.op("dve", lambda e: e.tensor_tensor(out=modv[:, 0:64], in0=ps[0][:, 0:64], in1=vec[:, OFF_BADA:OFF_BADA + 64],
                                                  op=ALU.add), reads=[P[0], Bvec], writes=[Bmod])
            S.op("dve", lambda e: e.scalar_tensor_tensor(out=A12[:, 0:32], in0=modv[:, 32:64], scalar=1.0, op0=ALU.add,
                                                         in1=vec[:, OFF_G1:OFF_G1 + 32], op1=ALU.mult),
                 reads=[Bmod, Bvec], writes=[BA12])
            S.barrier()
        A1 = A12[:, 0:32]
        A2 = A12[:, 32:64]
        B1 = modv[:, 0:32]
        gate1 = modv[:, 64:96]
        B2 = modv[:, 96:128]
        gate2 = modv[:, 160:192]
        Bmv = [Bmod, BA12, Bvec]

        def emit_rstd(st_bufs, Xt, BXs, rstd, Brstd, bank, n_part_inv):
            sq, Bsq = st_bufs
            for dc in range(NDC):
                k = dc % 2
                S.op("act", lambda e: e.activation(out=sq[k][:], in_=Xt[:, dc, :], func=AF.Square),
                     reads=[BXs[dc // 8]], writes=[Bsq[k]])
                S.op("pe", lambda e: e.matmul(ps[bank][:], lhsT=ones_bf, rhs=sq[k][:], start=(dc == 0), stop=(dc == NDC - 1)),
                     reads=[Bsq[k], Bcst], writes=[P[bank]])
            S.op("act", lambda e: e.activation(out=rstd[:], in_=ps[bank][:], func=AF.Sqrt, scale=n_part_inv, bias=EPS),
                 reads=[P[bank]], writes=[Brstd])
            S.op("dve", lambda e: e.reciprocal(out=rstd[:], in_=rstd[:]), reads=[Brstd], writes=[Brstd])

        def emit_norm_mod(Xt, BXs, rstd, Brstd, Acol, Bcol, tmp, Btmp, hT, BhT):
            for dc in range(NDC):
                k = dc % 2
                S.op("dve", lambda e: e.scalar_tensor_tensor(out=tmp[k][:], in0=Xt[:, dc, :], scalar=Acol[:, dc:dc + 1],
                                                             op0=ALU.mult, in1=rstd[:], op1=ALU.mult),
                     reads=[BXs[dc // 8], Brstd] + Bmv, writes=[Btmp[k]])
                S.op("act", lambda e: e.activation(out=hT[:, dc, :], in_=tmp[k][:], func=AF.Identity,
                                                   bias=Bcol[:, dc:dc + 1]),
                     reads=[Btmp[k]] + Bmv, writes=[BhT])

        if stop_after >= 1:
            with ExitStack() as ph:
                X = sb(ph, "X1", [128, NDC, T], F32)
                BX = [Buf(f"X1_{i}") for i in range(4)]
                sq = [sb(ph, f"sq{i}", [128, T], BF16) for i in range(2)]
                Bsq = [Buf("sq0"), Buf("sq1")]
                rstd = sb(ph, "rstd", [128, T], F32)
                Brstd = Buf("rstd")
                tmp = [sb(ph, f"tmp{i}", [128, T], F32) for i in range(2)]
                Btmp = [Buf("tmp0"), Buf("tmp1")]
                hT = [sb(ph, f"hT{i}", [128, NDC, T], BF16) for i in range(2)]
                BhT = [Buf("hT0"), Buf("hT1")]
                for s in range(NSLOT):
                    for qd in range(4):
                        S.dma("sp", X[:, qd * 8:(qd + 1) * 8, :], xT[s, :, qd * 8:(qd + 1) * 8, :], BX[qd], writes=[BX[qd]])
                    emit_rstd((sq, Bsq), X, BX, rstd, Brstd, s % 2, 1.0 / D)
                    emit_norm_mod(X, BX, rstd, Brstd, A1, B1, tmp, Btmp, hT[s % 2], BhT[s % 2])
                    S.dma("act", hT_s[s], hT[s % 2][:], BhT[s % 2], reads=[BhT[s % 2]], writes=[BhT_s])
                S.barrier()

        if stop_after >= 2:
            with ExitStack() as ph:
                Wg = [sb(ph, f"Wg{i}", [128, NDC, 512], BF16) for i in range(2)]
                BWg = [Buf("Wg0"), Buf("Wg1")]
                hTt = [sb(ph, f"hTt{i}", [128, NDC, T], BF16) for i in range(2)]
                BhTt = [Buf("hTt0"), Buf("hTt1")]
                stg = [sb(ph, f"stg{i}", [128, 512], BF16) for i in range(4)]
                Bstg = [Buf(f"stg{i}") for i in range(4)]
                Pb = [sb(ph, f"Pb{i}", [128, 528], F32) for i in range(4)]
                BPb = [Buf(f"Pb{i}") for i in range(4)]
                T1 = sb(ph, "T1", [128, 528], F32)
                T2 = sb(ph, "T2", [128, 528], F32)
                BT1, BT2 = Buf("T1"), Buf("T2")
                dT = [sb(ph, f"dT{i}", [128, 512], BF16) for i in range(4)]
                BdT = [Buf(f"dT{i}") for i in range(4)]
                d16 = sb(ph, "d16", [128, 16], F32)
                Bd16 = Buf("d16")
                hh = sb(ph, "hh", [128, NDC, 16], BF16)
                Bhh = Buf("hh")
                wp = sb(ph, "wp", [128, 4, 512], BF16)
                Bwp = Buf("wp")
                nload = [0]
                nps = [0]
                nst = [0]

                def load_h(slot):
                    k = nload[0] % 2
                    nload[0] += 1
                    S.dma("sp", hTt[k][:], hT_s[slot], BhTt[k], reads=[BhT_s], writes=[BhTt[k]])
                    return k

                for cg in range(16):
                    kw = cg % 2
                    S.dma("pool", Wg[kw][:], w_in[:, cg * 512:(cg + 1) * 512].rearrange("(dc p) n -> p dc n", p=128),
                          BWg[kw], writes=[BWg[kw]], max_dma_last_dim=8192)
                    kind = cg // 4
                    sub = cg % 4
                    if kind in (0, 1):
                        slots = [1, 3, 5, 7] if kind == 0 else list(range(NSLOT))
                        for si, slot in enumerate(slots):
                            kh = load_h(slot)
                            for hx in range(4):
                                head = sub * 4 + hx
                                r = nps[0] % 4
                                nps[0] += 1
                                for dc in range(NDC):
                                    S.op("pe", lambda e: e.matmul(ps[r][:], lhsT=Wg[kw][:, dc, hx * 128:(hx + 1) * 128],
                                                                  rhs=hTt[kh][:, dc, :], start=(dc == 0), stop=(dc == NDC - 1)),
                                         reads=[BWg[kw], BhTt[kh]], writes=[P[r]])
                                q = nst[0] % 4
                                nst[0] += 1
                                sc_ = (128.0 ** -0.5) if kind == 0 else 1.0
                                S.op("act", lambda e: e.activation(out=stg[q][:], in_=ps[r][:], func=AF.Copy, scale=sc_),
                                     reads=[P[r]], writes=[Bstg[q]])
                                if kind == 0:
                                    S.dma("act", QT_s[head, :, si * T:(si + 1) * T], stg[q][:], Bstg[q], reads=[Bstg[q]], writes=[BQT_s])
                                else:
                                    S.dma("act", KT_s[head, :, slot * T:(slot + 1) * T], stg[q][:], Bstg[q], reads=[Bstg[q]], writes=[BKT_s])
                    elif kind == 2:
                        for slot in range(NSLOT):
                            kh = load_h(slot)
                            for tb in range(4):
                                r = nps[0] % 4
                                nps[0] += 1
                                for dc in range(NDC):
                                    S.op("pe", lambda e: e.matmul(ps[r][:], lhsT=hTt[kh][:, dc, tb * 128:(tb + 1) * 128],
                                                                  rhs=Wg[kw][:, dc, :], start=(dc == 0), stop=(dc == NDC - 1)),
                                         reads=[BWg[kw], BhTt[kh]], writes=[P[r]])
                                q = nst[0] % 4
                                nst[0] += 1
                                if slot == 0:
                                    S.op("act", lambda e: e.activation(out=stg[q][:], in_=ps[r][:], func=AF.Identity, scale=flag),
                                         reads=[P[r], Bvec], writes=[Bstg[q]])
                                else:
                                    S.op("act", lambda e: e.activation(out=stg[q][:], in_=ps[r][:], func=AF.Copy),
                                         reads=[P[r]], writes=[Bstg[q]])
                                S.dma("act", V_s[slot * 4 + tb, :, sub * 512:(sub + 1) * 512], stg[q][:], Bstg[q],
                                      reads=[Bstg[q]], writes=[BV_s])
                    else:
                        g = sub
                        wdw = POOL_W[g]
                        S.dma("pool", wp[:], w_pool[g].rearrange("(cc p) n -> p cc n", p=128), Bwp, writes=[Bwp])
                        for j in range(NOWN):
                            S.dma("sp", hh[:], hT_s[2 * j, :, :, T - 16:T], Bhh, reads=[BhT_s], writes=[Bhh])
                            kh = load_h(2 * j + 1)
                            for cc in range(4):
                                r = nps[0] % 4
                                nps[0] += 1
                                for dc in range(NDC):
                                    S.op("pe", lambda e: e.matmul(ps[r][:, 0:16], lhsT=Wg[kw][:, dc, cc * 128:(cc + 1) * 128],
                                                                  rhs=hh[:, dc, :], start=(dc == 0), stop=(dc == NDC - 1)),
                                         reads=[BWg[kw], Bhh], writes=[P[r]])
                                if j == 0:
                                    S.op("act", lambda e: e.activation(out=Pb[cc][:, 0:16], in_=ps[r][:, 0:16], func=AF.Identity, scale=flag),
                                         reads=[P[r], Bvec], writes=[BPb[cc]])
                                else:
                                    S.op("act", lambda e: e.activation(out=Pb[cc][:, 0:16], in_=ps[r][:, 0:16], func=AF.Copy),
                                         reads=[P[r]], writes=[BPb[cc]])
                                r = nps[0] % 4
                                nps[0] += 1
                                for dc in range(NDC):
                                    S.op("pe", lambda e: e.matmul(ps[r][:], lhsT=Wg[kw][:, dc, cc * 128:(cc + 1) * 128],
                                                                  rhs=hTt[kh][:, dc, :], start=(dc == 0), stop=(dc == NDC - 1)),
                                         reads=[BWg[kw], BhTt[kh]], writes=[P[r]])
                                S.op("act", lambda e: e.activation(out=Pb[cc][:, 16:528], in_=ps[r][:], func=AF.Copy),
                                     reads=[P[r]], writes=[BPb[cc]])
                                S.op("dve", lambda e: e.tensor_tensor(out=T1[:, 1:528], in0=Pb[cc][:, 1:528], in1=Pb[cc][:, 0:527], op=ALU.add),
                                     reads=[BPb[cc]], writes=[BT1])
                                ws, Bws = T1, BT1
                                if wdw >= 4:
                                    S.op("dve", lambda e: e.tensor_tensor(out=T2[:, 3:528], in0=T1[:, 3:528], in1=T1[:, 1:526], op=ALU.add),
                                         reads=[BT1], writes=[BT2])
                                    ws, Bws = T2, BT2
                                if wdw >= 8:
                                    S.op("dve", lambda e: e.tensor_tensor(out=T1[:, 7:528], in0=T2[:, 7:528], in1=T2[:, 3:524], op=ALU.add),
                                         reads=[BT2], writes=[BT1])
                                    ws, Bws = T1, BT1
                                if wdw >= 16:
                                    S.op("dve", lambda e: e.tensor_tensor(out=T2[:, 15:528], in0=T1[:, 15:528], in1=T1[:, 7:520], op=ALU.add),
                                         reads=[BT1], writes=[BT2])
                                    ws, Bws = T2, BT2
                                S.op("dve", lambda e: e.scalar_tensor_tensor(out=dT[cc][:], in0=ws[:, 16:528], scalar=1.0 / wdw, op0=ALU.mult,
                                                                             in1=Pb[cc][:, 16:528], op1=ALU.subtract),
                                     reads=[Bws, BPb[cc]], writes=[BdT[cc]])
                                if j == 0:
                                    S.op("dve", lambda e: e.tensor_tensor(out=d16[:], in0=ws[:, 16:32],
                                                                          in1=vec[:, OFF_INVC + g * 16:OFF_INVC + (g + 1) * 16], op=ALU.mult),
                                         reads=[Bws, Bvec], writes=[Bd16])
                                    S.op("dve", lambda e: e.tensor_tensor(out=dT[cc][:, 0:16], in0=d16[:], in1=Pb[cc][:, 16:32], op=ALU.subtract),
                                         reads=[Bd16, BPb[cc]], writes=[BdT[cc]])
                            for dd in range(4):
                                r = nps[0] % 4
                                nps[0] += 1
                                for cc in range(4):
                                    S.op("pe", lambda e: e.matmul(ps[r][:], lhsT=wp[:, cc, dd * 128:(dd + 1) * 128], rhs=dT[cc][:],
                                                                  start=(cc == 0), stop=(cc == 3)),
                                         reads=[Bwp, BdT[cc]], writes=[P[r]])
                                q = nst[0] % 4
                                nst[0] += 1
                                col = OFF_SP + g * 4 + dd
                                S.op("act", lambda e: e.activation(out=stg[q][:], in_=ps[r][:], func=AF.Identity, scale=vec[:, col:col + 1]),
                                     reads=[P[r], Bvec], writes=[Bstg[q]])
                                S.dma("act", oT_s[j, :, 16 + g * 4 + dd, :], stg[q][:], Bstg[q], reads=[Bstg[q]], writes=[BoT_s])
                S.barrier()

        if stop_after >= 3:
            with ExitStack() as ph:
                KTh = [sb(ph, f"KTh{i}", [128, NSLOT * T], BF16) for i in range(2)]
                Vh = [sb(ph, f"Vh{i}", [128, NSLOT * 4, 128], BF16) for i in range(2)]
                QTh = [sb(ph, f"QTh{i}", [128, NOWN * T], BF16) for i in range(2)]
                BKTh = [Buf("KTh0"), Buf("KTh1")]
                BVh = [Buf("Vh0"), Buf("Vh1")]
                BQTh = [Buf("QTh0"), Buf("QTh1")]
                E = [sb(ph, f"E{i}", [128, T], F32) for i in range(2)]
                BE = [Buf("E0"), Buf("E1")]
                Lp = [sb(ph, f"Lp{i}", [128, T], BF16) for i in range(3)]
                BLp = [Buf(f"Lp{i}") for i in range(3)]
                Ls = sb(ph, "Ls", [128, T], BF16)
                BLs = Buf("Ls")
                Aa = [sb(ph, f"Aa{i}", [128, T], BF16) for i in range(2)]
                BAa = [Buf("Aa0"), Buf("Aa1")]
                sqa = sb(ph, "sqa", [128, T], BF16)
                Bsqa = Buf("sqa")
                rsa = sb(ph, "rsa", [128, T], F32)
                Brsa = Buf("rsa")
                ost = [sb(ph, f"ost{i}", [128, T], BF16) for i in range(2)]
                Bost = [Buf("ost0"), Buf("ost1")]

                units = []
                for h in range(NH):
                    for j in range(NOWN):
                        qs = 2 * j + 1
                        kbs = list(range(qs * 4 + 3, -1, -1))
                        for ui, kb in enumerate(kbs):
                            c0 = (kb - qs * 4) * 128 if kb >= qs * 4 else 0
                            units.append(dict(h=h, j=j, kb=kb, c0=c0, first=(ui == 0), last=(ui == len(kbs) - 1),
                                              diag=(kb >= qs * 4)))

                def load_head(h):
                    k = h % 2
                    S.dma("sp", KTh[k][:], KT_s[h], BKTh[k], reads=[BKT_s], writes=[BKTh[k]])
                    S.dma("sp", QTh[k][:], QT_s[h], BQTh[k], reads=[BQT_s], writes=[BQTh[k]])
                    S.dma("sp", Vh[k][:], V_s[:, :, h * 128:(h + 1) * 128].rearrange("tb p d -> p tb d"), BVh[k],
                          reads=[BV_s], writes=[BVh[k]])

                def stage0(i, u):
                    hk = u["h"] % 2
                    zb = i % 4
                    c0 = u["c0"]
                    qcol = u["j"] * T
                    S.op("pe", lambda e: e.matmul(ps[zb][:, c0:T], lhsT=KTh[hk][:, u["kb"] * 128:(u["kb"] + 1) * 128],
                                                  rhs=QTh[hk][:, qcol + c0:qcol + T], start=True, stop=True),
                         reads=[BKTh[hk], BQTh[hk]], writes=[P[zb]])

                def stage1(i, u):
                    zb = i % 4
                    c0 = u["c0"]
                    S.op("act", lambda e: e.activation(out=E[i % 2][:, c0:T], in_=ps[zb][:, c0:T], func=AF.Exp),
                         reads=[P[zb]], writes=[BE[i % 2]])
                    S.op("act", lambda e: e.activation(out=Lp[i % 3][:, c0:T], in_=E[i % 2][:, c0:T], func=AF.Ln, bias=1.0),
                         reads=[BE[i % 2]], writes=[BLp[i % 3]])
                    if u["diag"]:
                        S.op("dve", lambda e: e.tensor_tensor(out=Lp[i % 3][:, c0:c0 + 128], in0=Lp[i % 3][:, c0:c0 + 128],
                                                              in1=tri_bf, op=ALU.mult),
                             reads=[BLp[i % 3], Bcst], writes=[BLp[i % 3]])

                def stage2(i, u):
                    zb = i % 4
                    c0 = u["c0"]
                    S.op("pe", lambda e: e.matmul(ps[zb][:, c0:T], lhsT=negU_bf, rhs=Lp[i % 3][:, c0:T], start=False, stop=u["first"],
                                                  skip_group_check=True),
                         reads=[BLp[i % 3], Bcst], writes=[P[zb]])
                    if u["first"]:
                        S.op("pool", lambda e: e.memset(Ls[:], 0.0), writes=[BLs])
                    else:
                        S.op("pe", lambda e: e.matmul(ps[zb][:, c0:T], lhsT=negones_bf, rhs=Ls[:, c0:T], start=False, stop=True,
                                                      skip_group_check=True),
                             reads=[BLs, Bcst], writes=[P[zb]])
                    if not u["last"]:
                        S.op("pool", lambda e: e.tensor_tensor(out=Ls[:, c0:T], in0=Ls[:, c0:T], in1=Lp[i % 3][:, c0:T], op=ALU.add),
                             reads=[BLs, BLp[i % 3]], writes=[BLs])

                def stage3(i, u):
                    hk = u["h"] % 2
                    zb = i % 4
                    c0 = u["c0"]
                    ob = 4 + (u["h"] * NOWN + u["j"]) % 2
                    S.op("act", lambda e: e.activation(out=Aa[i % 2][:, c0:T], in_=ps[zb][:, c0:T], func=AF.Exp),
                         reads=[P[zb]], writes=[BAa[i % 2]])
                    if u["diag"]:
                        S.op("dve", lambda e: e.tensor_tensor(out=Aa[i % 2][:, c0:c0 + 128], in0=Aa[i % 2][:, c0:c0 + 128],
                                                              in1=tri_bf, op=ALU.mult),
                             reads=[BAa[i % 2], Bcst], writes=[BAa[i % 2]])
                    S.op("pe", lambda e: e.matmul(ps[ob][:, c0:T], lhsT=Vh[hk][:, u["kb"], :], rhs=Aa[i % 2][:, c0:T],
                                                  start=u["first"], stop=u["last"], skip_group_check=True),
                         reads=[BVh[hk], BAa[i % 2]], writes=[P[ob]])
                    if u["last"]:
                        S.op("act", lambda e: e.activation(out=sqa[:], in_=ps[ob][:], func=AF.Square), reads=[P[ob]], writes=[Bsqa])
                        S.op("pe", lambda e: e.matmul(ps[6][:], lhsT=ones_bf, rhs=sqa[:], start=True, stop=True),
                             reads=[Bsqa, Bcst], writes=[P[6]])
                        epi.append((i + 2, u["h"], u["j"], ob))

                def epilogue_b(h, j, ob):
                    k = (h * NOWN + j) % 2
                    S.op("act", lambda e: e.activation(out=rsa[:], in_=ps[6][:], func=AF.Ln, scale=1.0 / 128, bias=EPS),
                         reads=[P[6]], writes=[Brsa])
                    S.op("act", lambda e: e.activation(out=rsa[:], in_=rsa[:], func=AF.Exp, scale=-0.5), reads=[Brsa], writes=[Brsa])
                    S.op("dve", lambda e: e.scalar_tensor_tensor(out=ost[k][:], in0=ps[ob][:], scalar=vec[:, OFF_GH + h:OFF_GH + h + 1],
                                                                 op0=ALU.mult, in1=rsa[:], op1=ALU.mult),
                         reads=[P[ob], Brsa, Bvec], writes=[Bost[k]])
                    S.dma("sp", oT_s[j, :, h, :], ost[k][:], Bost[k], reads=[Bost[k]], writes=[BoT_s])

                load_head(0)
                n = len(units)
                epi = []
                wa3 = [sb(ph, f"wa3_{i}", [128, NDC, 512], BF16) for i in range(2)]
                mod_load(wa3, 16)
                for i in range(n + 3):
                    if i < n:
                        u = units[i]
                        if u["first"] and u["j"] == 0 and u["h"] + 1 < NH:
                            load_head(u["h"] + 1)
                        stage0(i, u)
                        mq = i * 4
                        if mq < 32 * 128:
                            ct = 16 + mq // 128
                            if mq % 128 == 0 and ct + 1 < 48:
                                mod_load(wa3, ct + 1)
                            for idx in range(mq % 128, mq % 128 + 4):
                                mod_mm(wa3, ct, idx, 7, 64)
                    if 0 <= i - 1 < n:
                        stage1(i - 1, units[i - 1])
                    if 0 <= i - 2 < n:
                        stage2(i - 2, units[i - 2])
                    if 0 <= i - 3 < n:
                        stage3(i - 3, units[i - 3])
                    while epi and epi[0][0] <= i - 3:
                        epilogue_b(*epi.pop(0)[1:])
                while epi:
                    epilogue_b(*epi.pop(0)[1:])
                S.op("dve", lambda e: e.tensor_tensor(out=modv[:, 64:192], in0=ps[7][:, 0:128], in1=vec[:, OFF_BADA + 64:OFF_BADA + 192],
                                                      op=ALU.add), reads=[P[7], Bvec], writes=[Bmod])
                S.op("dve", lambda e: e.scalar_tensor_tensor(out=A12[:, 32:64], in0=modv[:, 128:160], scalar=1.0, op0=ALU.add,
                                                             in1=vec[:, OFF_G2:OFF_G2 + 32], op1=ALU.mult),
                     reads=[Bmod, Bvec], writes=[BA12])
                if debug:
                    S.dma("sp", dbg_mod, modv[:], Bmod, reads=[Bmod], writes=[Bdbg])
                S.barrier()

        if stop_after >= 4:
            with ExitStack() as ph:
                X = sb(ph, "X2", [128, NDC, T], F32)
                BX = [Buf(f"X2_{i}") for i in range(4)]
                h2T = sb(ph, "h2T", [128, NDC, T], BF16)
                Bh2T = Buf("h2T")
                S12 = sb(ph, "S12", [128, 4, 8, 256], F32)
                BS12 = Buf("S12")
                TD = sb(ph, "TD", [128, 4, 8, 2], F32)
                BTD = Buf("TD")
                sq = [sb(ph, f"sqb{i}", [128, T], BF16) for i in range(2)]
                Bsq = [Buf("sqb0"), Buf("sqb1")]
                rstd = sb(ph, "rstd2", [128, T], F32)
                Brstd = Buf("rstd2")
                skb = sb(ph, "skb", [128, 2, 128], BF16)
                Bskb = Buf("skb")
                S.dma("pool", skb[:], skT, Bskb, writes=[Bskb])
                for j in range(NOWN):
                    with ExitStack() as p4:
                        oTt = sb(p4, "oTt", [128, NDC, T], BF16)
                        BoTt = Buf("oTt")
                        Wo = [sb(p4, f"Wo{i}", [128, NDC, 256], BF16) for i in range(2)]
                        BWo = [Buf("Wo0"), Buf("Wo1")]
                        for qd in range(4):
                            S.dma("sp", X[:, qd * 8:(qd + 1) * 8, :], xT[2 * j + 1, :, qd * 8:(qd + 1) * 8, :], BX[qd], writes=[BX[qd]])
                        S.dma("sp", oTt[:], oT_s[j], BoTt, reads=[BoT_s], writes=[BoTt])
                        for cg in range(16):
                            k = cg % 2
                            S.dma("pool", Wo[k][:], w_out[:, cg * 256:(cg + 1) * 256].rearrange("(ec p) n -> p ec n", p=128),
                                  BWo[k], writes=[BWo[k]])
                            for dd in range(2):
                                dch = cg * 2 + dd
                                r = dch % 4
                                for ec in range(NDC):
                                    S.op("pe", lambda e: e.matmul(ps[r][:], lhsT=Wo[k][:, ec, dd * 128:(dd + 1) * 128], rhs=oTt[:, ec, :],
                                                                  start=(ec == 0), stop=(ec == NDC - 1)),
                                         reads=[BWo[k], BoTt], writes=[P[r]])
                                S.op("dve", lambda e: e.scalar_tensor_tensor(out=X[:, dch, :], in0=ps[r][:], scalar=gate1[:, dch:dch + 1],
                                                                             op0=ALU.mult, in1=X[:, dch, :], op1=ALU.add),
                                     reads=[P[r], BX[dch // 8]] + Bmv, writes=[BX[dch // 8]])
                        if debug:
                            S.dma("sp", dbg_x1[j], X[:], BX[0], reads=BX, writes=[Bdbg])
                        S.barrier()
                    if stop_after < 5:
                        continue
                    with ExitStack() as p5:
                        tmp = [sb(p5, f"tmpb{i}", [128, T], F32) for i in range(2)]
                        Btmp = [Buf("tmpb0"), Buf("tmpb1")]
                        Wq = [sb(p5, f"Wq{i}", [128, NDC, 256], BF16) for i in range(2)]
                        BWq = [Buf("Wq0"), Buf("Wq1")]
                        qT = [sb(p5, f"qT{i}", [128, 2, T], BF16) for i in range(2)]
                        BqT = [Buf("qT0"), Buf("qT1")]
                        v16 = [sb(p5, f"v16_{i}", [128, 2, 16], F32) for i in range(2)]
                        Bv16 = [Buf("v16_0"), Buf("v16_1")]
                        tmpk = [sb(p5, f"tmpk{i}", [128, 128], F32) for i in range(2)]
                        Btmpk = [Buf("tmpk0"), Buf("tmpk1")]
                        cand = [sb(p5, f"cand{i}", [128, 256], F32) for i in range(2)]
                        cand2 = [sb(p5, f"cand2_{i}", [128, 256], F32) for i in range(2)]
                        Bcand = [Buf("cand_0"), Buf("cand_1")]
                        Bcand2 = [Buf("cand2_0"), Buf("cand2_1")]
                        c16a = sb(p5, "c16a", [128, 32, 16], F32)
                        Bc16 = Buf("c16a")
                        e16a = sb(p5, "e16a", [128, 32, 16], F32)
                        Be16 = Buf("e16a")
                        sm = sb(p5, "sm", [128, 64], F32)
                        Bsm = Buf("sm")
                        todo = []

                        def cand_topk(kk, idx):
                            S.op("dve", lambda e: e.max(out=c16a[:, idx, 0:8], in_=cand[kk][:]), reads=[Bcand[kk]], writes=[Bc16])
                            S.op("dve", lambda e: e.match_replace(out=cand2[kk][:], in_to_replace=c16a[:, idx, 0:8], in_values=cand[kk][:],
                                                                  imm_value=-1e30), reads=[Bcand[kk], Bc16], writes=[Bcand2[kk]])
                            S.op("dve", lambda e: e.max(out=c16a[:, idx, 8:16], in_=cand2[kk][:]), reads=[Bcand2[kk]], writes=[Bc16])

                        emit_rstd((sq, Bsq), X, BX, rstd, Brstd, 4, 1.0 / D)
                        emit_norm_mod(X, BX, rstd, Brstd, A2, B2, tmp, Btmp, h2T, Bh2T)
                        if debug:
                            S.dma("sp", dbg_h2[j], h2T[:], Bh2T, reads=[Bh2T], writes=[Bdbg])
                        def load_wq(hq_):
                            S.dma("pool", Wq[hq_ % 2][:], w_query[:, hq_ * 256:(hq_ + 1) * 256].rearrange("(dc p) n -> p dc n", p=128),
                                  BWq[hq_ % 2], writes=[BWq[hq_ % 2]])

                        load_wq(0)
                        load_wq(1)
                        for hq in range(8):
                            k = hq % 2
                            for cc in range(2):
                                r = cc
                                for dc in range(NDC):
                                    S.op("pe", lambda e: e.matmul(ps[r][:], lhsT=Wq[k][:, dc, cc * 128:(cc + 1) * 128], rhs=h2T[:, dc, :],
                                                                  start=(dc == 0), stop=(dc == NDC - 1)),
                                         reads=[BWq[k], Bh2T], writes=[P[r]])
                                S.op("act", lambda e: e.activation(out=qT[k][:, cc, :], in_=ps[r][:], func=AF.Copy),
                                     reads=[P[r]], writes=[BqT[k]])
                            if hq + 2 < 8:
                                load_wq(hq + 2)
                            for tb in range(4):
                                r = 2 + tb % 2
                                for w_ in range(2):
                                    S.op("pe", lambda e: e.matmul(ps[r][:, w_ * 128:(w_ + 1) * 128], lhsT=qT[k][:, w_, tb * 128:(tb + 1) * 128],
                                                                  rhs=skb[:, w_, :], start=True, stop=True, skip_group_check=True),
                                         reads=[BqT[k], Bskb], writes=[P[r]])
                                S.op("act", lambda e: e.activation(out=S12[:, tb, hq, :], in_=ps[r][:, 0:256], func=AF.Copy),
                                     reads=[P[r]], writes=[BS12])
                                kk = (hq * 4 + tb) % 2
                                idx = tb * 8 + hq
                                for w_ in range(2):
                                    src = S12[:, tb, hq, w_ * 128:(w_ + 1) * 128]
                                    S.op("dve", lambda e: e.max(out=v16[kk][:, w_, 0:8], in_=src), reads=[BS12], writes=[Bv16[kk]])
                                    S.op("dve", lambda e: e.match_replace(out=tmpk[kk][:], in_to_replace=v16[kk][:, w_, 0:8], in_values=src,
                                                                          imm_value=-1e30), reads=[BS12, Bv16[kk]], writes=[Btmpk[kk]])
                                    S.op("dve", lambda e: e.max(out=v16[kk][:, w_, 8:16], in_=tmpk[kk][:]), reads=[Btmpk[kk]], writes=[Bv16[kk]])
                                S.op("pool", lambda e: e.tensor_tensor(out=cand[kk][:].rearrange("p (a b) -> p a b", a=16),
                                                                       in0=v16[kk][:, 0, :].unsqueeze(2).broadcast_to([128, 16, 16]),
                                                                       in1=v16[kk][:, 1, :].unsqueeze(1).broadcast_to([128, 16, 16]), op=ALU.add),
                                     reads=[Bv16[kk]], writes=[Bcand[kk]])
                                todo.append((kk, idx))
                                if len(todo) > 1:
                                    cand_topk(*todo.pop(0))
                        while todo:
                            cand_topk(*todo.pop(0))
                        TDv = TD[:].rearrange("p a b c -> p (a b) c")
                        S.op("dve", lambda e: e.tensor_copy(out=TDv[:, :, 0:1], in_=c16a[:, :, 15:16]), reads=[Bc16], writes=[BTD])
                        S.op("dve", lambda e: e.tensor_tensor(out=e16a[:], in0=c16a[:], in1=c16a[:, :, 0:1].broadcast_to([128, 32, 16]),
                                                              op=ALU.subtract), reads=[Bc16], writes=[Be16])
                        S.op("act", lambda e: e.activation(out=e16a[:], in_=e16a[:], func=AF.Exp), reads=[Be16], writes=[Be16])
                        S.op("dve", lambda e: e.reduce_sum(out=sm[:, 0:32], in_=e16a[:], axis=mybir.AxisListType.X), reads=[Be16], writes=[Bsm])
                        S.op("act", lambda e: e.activation(out=sm[:, 32:64], in_=sm[:, 0:32], func=AF.Ln), reads=[Bsm], writes=[Bsm])
                        S.op("dve", lambda e: e.scalar_tensor_tensor(out=TDv[:, :, 1:2], in0=c16a[:, :, 0:1], scalar=-1.0, op0=ALU.mult,
                                                                     in1=sm[:, 32:64].unsqueeze(2), op1=ALU.subtract),
                             reads=[Bc16, Bsm], writes=[BTD])
                        if debug:
                            S.dma("sp", dbg_s12[j], S12[:], BS12, reads=[BS12], writes=[Bdbg])
                            S.dma("sp", dbg_td[j], TD[:], BTD, reads=[BTD], writes=[Bdbg])
                        S.barrier()
                    if stop_after < 6:
                        continue
                    with ExitStack() as p6:
                        ub = [sb(p6, f"ub{i}", [128, NDC, 128], BF16) for i in range(2)]
                        Bub = [Buf("ub0"), Buf("ub1")]
                        vb = [sb(p6, f"vb{i}", [128, GRP, 2048], BF16) for i in range(2)]
                        Bvb = [Buf("vb0"), Buf("vb1")]
                        actT = [sb(p6, f"actT{i}", [128, T], BF16) for i in range(GRP)]
                        BactT = [Buf(f"actT{i}") for i in range(GRP)]
                        NCB, NGM, LAG = 3, 6, 4
                        Cb = [sb(p6, f"Cb{i}", [128, GRP * 128], F32) for i in range(NCB)]
                        BCb = [Buf(f"Cb{i}") for i in range(NCB)]
                        Gx = [sb(p6, f"Gx{i}", [128, GRP * 128], BF16) for i in range(NCB)]
                        BGx = [Buf(f"Gx{i}") for i in range(NCB)]
                        Gm = [sb(p6, f"Gm{i}", [128, GRP * 128], BF16) for i in range(NGM)]
                        BGm = [Buf(f"Gm{i}") for i in range(NGM)]
                        gl = [sb(p6, f"gl{i}", [128, T], BF16) for i in range(GRP)]
                        Bgl = [Buf(f"gl{i}") for i in range(GRP)]
                        WTB = [0, 1, 2, 3]
                        SB_ = [4, 5]
                        YB = [6, 7]
                        cnt = dict(w=0, s=0, y=0)
                        if j == 0 and debug:
                            print("sbuf bytes remaining in PEER scope:", nc.sbuf_bytes_remaining)
                        pending = []

                        def wbuild_elem(g, pi):
                            tb, hq = pi // 8, pi % 8
                            k = cnt["w"] % NCB
                            km = cnt["w"] % NGM
                            cnt["w"] += 1
                            i0 = g * GRP
                            S.op("pool", lambda e: e.tensor_tensor(
                                out=Cb[k][:].rearrange("p (a b) -> p a b", a=GRP),
                                in0=S12[:, tb, hq, i0:i0 + GRP].unsqueeze(2).broadcast_to([128, GRP, 128]),
                                in1=S12[:, tb, hq, 128:256].unsqueeze(1).broadcast_to([128, GRP, 128]), op=ALU.add),
                                reads=[BS12], writes=[BCb[k]])
                            S.op("act", lambda e: e.activation(out=Gx[k][:], in_=Cb[k][:], func=AF.Exp, bias=TD[:, tb, hq, 1:2]),
                                 reads=[BCb[k], BTD], writes=[BGx[k]])
                            S.op("dve", lambda e: e.scalar_tensor_tensor(out=Gm[km][:], in0=Cb[k][:], scalar=TD[:, tb, hq, 0:1], op0=ALU.is_ge,
                                                                         in1=Gx[k][:], op1=ALU.mult),
                                 reads=[BCb[k], BGx[k], BTD], writes=[BGm[km]])
                            pending.append((km, tb, hq))

                        def emit_tr():
                            km, tb, hq = pending.pop(0)
                            for a in range(GRP):
                                S.op("pe", lambda e: e.matmul(ps[WTB[a]][:, tb * 128:(tb + 1) * 128], lhsT=Gm[km][:, a * 128:(a + 1) * 128],
                                                              rhs=ident_bf, start=(hq == 0), stop=(hq == 7), skip_group_check=True),
                                     reads=[BGm[km], Bcst], writes=[P[WTB[a]]])

                        def load_u(eb):
                            k = eb % 2
                            S.dma("pool", ub[k][:].rearrange("p a b -> p (a b)"), uT[eb], Bub[k], writes=[Bub[k]], max_dma_last_dim=8192)

                        def load_v(g, hf):
                            S.dma("pool", vb[hf][:], vE[g * GRP * 128:(g + 1) * GRP * 128, hf * 2048:(hf + 1) * 2048].rearrange("(a p) d -> p a d", p=128),
                                  Bvb[hf], writes=[Bvb[hf]], max_dma_last_dim=8192)

                        def s_block(eb, dcs):
                            ku = eb % 2
                            sbk = SB_[eb % 2]
                            for dc in dcs:
                                S.op("pe", lambda e: e.matmul(ps[sbk][:], lhsT=ub[ku][:, dc, :], rhs=h2T[:, dc, :],
                                                              start=(dc == 0), stop=(dc == NDC - 1)),
                                     reads=[Bub[ku], Bh2T], writes=[P[sbk]])

                        def s_finish(eb):
                            sbk = SB_[eb % 2]
                            a_ = eb % GRP
                            S.op("act", lambda e: e.activation(out=gl[a_][:], in_=ps[sbk][:], func=AF.Gelu),
                                 reads=[P[sbk]], writes=[Bgl[a_]])
                            if eb + 2 < NEB:
                                load_u(eb + 2)

                        def boundary():
                            while pending:
                                emit_tr()
                            for a_ in range(GRP):
                                S.op("dve", lambda e: e.tensor_tensor(out=actT[a_][:], in0=gl[a_][:], in1=ps[WTB[a_]][:], op=ALU.mult),
                                     reads=[Bgl[a_], P[WTB[a_]]], writes=[BactT[a_]])

                        load_u(0)
                        load_u(1)
                        load_v(0, 0)
                        load_v(0, 1)
                        for pi in range(32):
                            wbuild_elem(0, pi)
                            emit_tr()
                        for a in range(GRP):
                            s_block(a, range(NDC))
                            s_finish(a)
                        boundary()
                        for g in range(NGRP):
                            for dch in range(NDC):
                                hf = dch // 16
                                yb = YB[cnt["y"] % 2]
                                cnt["y"] += 1
                                if dch == 0 and g > 0:
                                    load_v(g, 1)
                                for a in range(GRP):
                                    S.op("pe", lambda e: e.matmul(ps[yb][:], lhsT=vb[hf][:, a, (dch % 16) * 128:(dch % 16 + 1) * 128], rhs=actT[a][:],
                                                                  start=(a == 0), stop=(a == GRP - 1)),
                                         reads=[Bvb[hf], BactT[a]], writes=[P[yb]])
                                S.op("dve", lambda e: e.scalar_tensor_tensor(out=X[:, dch, :], in0=ps[yb][:], scalar=gate2[:, dch:dch + 1],
                                                                             op0=ALU.mult, in1=X[:, dch, :], op1=ALU.add),
                                     reads=[P[yb], BX[dch // 8]] + Bmv, writes=[BX[dch // 8]])
                                if g + 1 < NGRP:
                                    wbuild_elem(g + 1, dch)
                                    if len(pending) > LAG:
                                        emit_tr()
                                    eb = (g + 1) * GRP + dch // 8
                                    s_block(eb, range((dch % 8) * 4, (dch % 8) * 4 + 4))
                                    if dch % 8 == 7:
                                        s_finish(eb)
                                    if dch == 16:
                                        load_v(g + 1, 0)
                            if g + 1 < NGRP:
                                boundary()
                        emit_rstd((sq, Bsq), X, BX, rstd, Brstd, 4, 1.0 / D)
                        ostf = [Cb[0], Cb[1]]
                        Bostf = [BCb[0], BCb[1]]
                        for dc in range(NDC):
                            k = dc % 2
                            S.op("dve", lambda e: e.scalar_tensor_tensor(out=ostf[k][:], in0=X[:, dc, :], scalar=vec[:, OFF_GF + dc:OFF_GF + dc + 1],
                                                                         op0=ALU.mult, in1=rstd[:], op1=ALU.mult),
                                 reads=[BX[dc // 8], Brstd, Bvec], writes=[Bostf[k]])
                            S.dma("sp", outT[j, :, dc, :], ostf[k][:], Bostf[k], reads=[Bostf[k]], writes=[Bout])
                        S.barrier()
        S.finish()
    return nc


def _consts():
    j = np.arange(128)
    ones = np.ones((128, 128), np.float32)
    ident = np.eye(128, dtype=np.float32)
    negU = -(j[:, None] >= j[None, :]).astype(np.float32)
    tri = (j[:, None] < j[None, :]).astype(np.float32)
    return np.ascontiguousarray(np.stack([ones, -ones, ident, negU, tri], axis=1))


def _pcol(v):
    return np.ascontiguousarray(np.asarray(v, np.float32).reshape(-1, 128).T)


def prepare_inputs(x, c, w_ada, b_ada, g_norm1, w_in, g_attn_head, w_pool, s_pool, w_out, g_norm2, w_query,
                   sub_keys_1, sub_keys_2, u_experts, v_experts, g_final, cores=range(8)):
    x = np.asarray(x, np.float32)
    shared = dict(
        w_ada=np.ascontiguousarray(np.asarray(w_ada, np.float32)[0]),
        consts=_consts(),
        w_in=np.ascontiguousarray(np.asarray(w_in, np.float32)[0]),
        w_pool=np.ascontiguousarray(np.asarray(w_pool, np.float32)[0]),
        w_out=np.ascontiguousarray(np.asarray(w_out, np.float32)[0]),
        w_query=np.ascontiguousarray(np.asarray(w_query, np.float32)[0]),
        skT=np.ascontiguousarray(np.stack([np.asarray(sub_keys_1, np.float32)[0].T, np.asarray(sub_keys_2, np.float32)[0].T], axis=1)),
        vE=np.ascontiguousarray(np.asarray(v_experts, np.float32)[0]),
    )
    u = np.asarray(u_experts, np.float32)[0]
    shared["uT"] = np.ascontiguousarray(u.reshape(NEB, 128, NDC, 128).transpose(0, 3, 2, 1)).reshape(NEB, 128, NDC * 128)
    in_maps = []
    for core in cores:
        b, par = core // 2, core % 2
        xb = x[b]
        xt = xb.reshape(NSLOT, T, NDC, 128).transpose(0, 3, 2, 1)
        loc = np.zeros((NSLOT, 128, NDC, T), np.float32)
        if par == 1:
            loc[:] = xt
        else:
            loc[1:] = xt[:NSLOT - 1]
        vecs = np.zeros((128, NVEC), np.float32)
        vecs[:, OFF_BADA:OFF_BADA + 192] = _pcol(np.asarray(b_ada)[0])
        vecs[:, OFF_G1:OFF_G1 + 32] = _pcol(np.asarray(g_norm1)[0])
        vecs[:, OFF_G2:OFF_G2 + 32] = _pcol(np.asarray(g_norm2)[0])
        vecs[:, OFF_GF:OFF_GF + 32] = _pcol(np.asarray(g_final))
        vecs[:, OFF_GH:OFF_GH + 16] = np.asarray(g_attn_head, np.float32)[0].T
        vecs[:, OFF_SP:OFF_SP + 16] = _pcol(np.asarray(s_pool)[0])
        vecs[:, OFF_FLAG] = float(par)
        for g, w in enumerate(POOL_W):
            cntv = np.minimum(np.arange(16) + 1, w) if par == 0 else np.full(16, w)
            vecs[:, OFF_INVC + g * 16:OFF_INVC + (g + 1) * 16] = (1.0 / cntv.astype(np.float32))[None, :]
        m = dict(shared)
        m["xT"] = loc
        m["cT"] = _pcol(np.asarray(c, np.float32)[b])
        m["vecs"] = vecs
        in_maps.append(m)
    return in_maps


def assemble_output(results, cores=range(8)):
    out = np.zeros((4, 4096, D), np.float32)
    for core, r in zip(cores, results):
        b, par = core // 2, core % 2
        o = np.asarray(r["outT"])
        for j in range(NOWN):
            tile = 2 * j + par
            out[b, tile * T:(tile + 1) * T, :] = o[j].transpose(2, 1, 0).reshape(T, D)
    return out


_NC_CACHE = {}


def kernel(**inputs):
    if "nc" not in _NC_CACHE:
        _NC_CACHE["nc"] = build_program()
    nc = _NC_CACHE["nc"]
    in_maps = prepare_inputs(**inputs)
    res = run_bass_kernel_spmd(nc, in_maps, core_ids=list(range(8)))
    return assemble_output(res.results)
```

```python
import numpy as np
from contextlib import ExitStack
import concourse.bass as bass
import concourse.mybir as mybir
from concourse.bass_utils import run_bass_kernel_spmd

F32 = mybir.dt.float32
BF16 = mybir.dt.bfloat16
AF = mybir.ActivationFunctionType
ALU = mybir.AluOpType

D = 4096
NDC = 32
T = 512
NSLOT = 8
NOWN = 4
NH = 16
EPS = 1e-6
NEB = 128
GRP = 4
NGRP = NEB // GRP
POOL_W = (2, 4, 8, 16)
NVEC = 192 + 32 * 3 + 16 + 16 + 1 + 64
OFF_BADA, OFF_G1, OFF_G2, OFF_GF, OFF_GH, OFF_SP, OFF_FLAG, OFF_INVC = 0, 192, 224, 256, 288, 304, 320, 321


class _Buf:
    def __init__(self, name):
        self.name = name
        self.last_write = None
        self.reads = {}
        self.dsem = None


_BUFS = {}


def Buf(name):
    if name not in _BUFS:
        _BUFS[name] = _Buf(name)
    return _BUFS[name]


class Sched:
    def __init__(self, nc, stack):
        self.nc = nc
        self.stack = stack
        self.eng = {"pe": nc.tensor, "act": nc.scalar, "dve": nc.vector, "pool": nc.gpsimd, "sp": nc.sync}
        self.sem = {}
        self.cnt = {}
        for e in ("pe", "act", "dve", "pool"):
            self.sem[e] = stack.enter_context(nc.semaphore("s_" + e))
            self.cnt[e] = 0
        self.waited = {e: {} for e in self.eng}
        self.dcnt = {}
        self.dsems = []

    def _wait(self, engine, ev):
        if ev[0] == "c":
            _, e, v = ev
            if e == "pe" and engine == "pe":
                return
            key = ("c", e)
            sem = self.sem[e]
        else:
            b = ev[1]
            key = ("d", b.name)
            sem = b.dsem
            v = self.dcnt[b.name]
        if self.waited[engine].get(key, 0) >= v:
            return
        self.waited[engine][key] = v
        self.eng[engine].wait_ge(sem, v)

    def _deps(self, engine, reads, writes):
        for b in reads:
            if b.last_write is not None:
                self._wait(engine, b.last_write)
        for b in writes:
            if b.last_write is not None:
                self._wait(engine, b.last_write)
            for ev in list(b.reads.values()):
                self._wait(engine, ev)

    def _commit(self, ev, reads, writes):
        key = (ev[0], ev[1]) if ev[0] == "c" else ("d", ev[1].name)
        for b in reads:
            if b not in writes:
                b.reads[key] = ev
        for b in writes:
            b.last_write = ev
            b.reads = {}

    def op(self, engine, fn, reads=(), writes=()):
        reads = list(reads)
        writes = list(writes)
        self._deps(engine, reads, writes)
        ins = fn(self.eng[engine])
        ins.then_inc(self.sem[engine], 1)
        self.cnt[engine] += 1
        self._commit(("c", engine, self.cnt[engine]), reads, writes)
        return ins

    def dma(self, queue, out, in_, owner, reads=(), writes=(), **kw):
        reads = list(reads)
        writes = list(writes)
        if owner.dsem is None:
            owner.dsem = self.stack.enter_context(self.nc.semaphore("d_" + owner.name))
            self.dcnt[owner.name] = 0
            self.dsems.append(owner)
        self._deps(queue, reads, writes)
        ins = self.eng[queue].dma_start(out=out, in_=in_, **kw)
        ins.then_inc(owner.dsem, 16)
        self.dcnt[owner.name] += 16
        self._commit(("d", owner), reads, writes)
        return ins

    def barrier(self):
        for q in ("pe", "act", "dve", "pool", "sp"):
            for e in ("pe", "act", "dve", "pool"):
                if self.cnt[e] > 0 and e != q:
                    self._wait(q, ("c", e, self.cnt[e]))
            for b in self.dsems:
                self._wait(q, ("d", b))

    def finish(self):
        for e in ("pe", "act", "dve", "pool"):
            if self.cnt[e] > 0:
                self._wait("sp", ("c", e, self.cnt[e]))
        for b in self.dsems:
            self._wait("sp", ("d", b))


def build_program(stop_after=99, debug=False):
    nc = bass.Bass("TRN2", target_bir_lowering=False)
    _BUFS.clear()
    uniq = [0]

    def din(name, shape):
        return nc.dram_tensor(name, shape, F32, kind="ExternalInput").ap()

    def dscr(name, shape, dt=BF16):
        kind = "ExternalOutput" if debug else "Internal"
        return nc.dram_tensor(name, shape, dt, kind=kind).ap()

    xT = din("xT", [NSLOT, 128, NDC, T])
    cT = din("cT", [128, NDC])
    w_ada = din("w_ada", [D, 6 * D])
    vecs = din("vecs", [128, NVEC])
    consts = din("consts", [128, 5, 128])
    w_in = din("w_in", [D, 2 * D])
    w_pool = din("w_pool", [4, 512, 512])
    w_out = din("w_out", [D, D])
    w_query = din("w_query", [D, 2048])
    skT = din("skT", [128, 2, 128])
    uT = din("uT", [NEB, 128, NDC * 128])
    vE = din("vE", [NEB * 128, D])
    outT = nc.dram_tensor("outT", [NOWN, 128, NDC, T], F32, kind="ExternalOutput").ap()

    hT_s = dscr("hT_s", [NSLOT, 128, NDC, T])
    QT_s = dscr("QT_s", [NH, 128, NOWN * T])
    KT_s = dscr("KT_s", [NH, 128, NSLOT * T])
    V_s = dscr("V_s", [NSLOT * 4, 128, 2048])
    oT_s = dscr("oT_s", [NOWN, 128, NDC, T])
    if debug:
        dbg_mod = nc.dram_tensor("dbg_mod", [128, 192], F32, kind="ExternalOutput").ap()
        dbg_x1 = nc.dram_tensor("dbg_x1", [NOWN, 128, NDC, T], F32, kind="ExternalOutput").ap()
        dbg_h2 = nc.dram_tensor("dbg_h2", [NOWN, 128, NDC, T], BF16, kind="ExternalOutput").ap()
        dbg_s12 = nc.dram_tensor("dbg_s12", [NOWN, 128, 4, 8, 256], F32, kind="ExternalOutput").ap()
        dbg_td = nc.dram_tensor("dbg_td", [NOWN, 128, 4, 8, 2], F32, kind="ExternalOutput").ap()
    BhT_s, BQT_s, BKT_s, BV_s, BoT_s, Bout = [Buf(n) for n in ("hT_s", "QT_s", "KT_s", "V_s", "oT_s", "outd")]
    Bdbg = Buf("dbg")

    with ExitStack() as gs:
        S = Sched(nc, gs)

        def sb(st, name, shape, dt):
            uniq[0] += 1
            return st.enter_context(nc.sbuf_tensor(f"{name}_{uniq[0]}", shape, dt))

        ps = [gs.enter_context(nc.psum_tensor(f"ps{i}", [128, 512], F32)) for i in range(8)]
        P = [Buf(f"ps{i}") for i in range(8)]

        cst = sb(gs, "cst", [128, 5, 128], BF16)
        Bcst = Buf("cst")
        S.dma("pool", cst[:], consts, Bcst, writes=[Bcst])
        ones_bf, negones_bf, ident_bf, negU_bf, tri_bf = [cst[:, i, :] for i in range(5)]
        vec = sb(gs, "vec", [128, NVEC], F32)
        Bvec = Buf("vec")
        S.dma("sp", vec[:], vecs, Bvec, writes=[Bvec])
        modv = sb(gs, "modv", [128, 192], F32)
        A12 = sb(gs, "A12", [128, 64], F32)
        Bmod = Buf("modv")
        BA12 = Buf("A12")
        flag = vec[:, OFF_FLAG:OFF_FLAG + 1]

        cT_sb = sb(gs, "cT_sb", [128, NDC], F32)
        sc = sb(gs, "sc", [128, NDC], BF16)
        Bc, Bsc = Buf("cT_sb"), Buf("sc")
        Bwa = [Buf("wa0"), Buf("wa1")]
        S.dma("sp", cT_sb[:], cT, Bc, writes=[Bc])
        S.op("act", lambda e: e.activation(out=sc[:], in_=cT_sb[:], func=AF.Silu), reads=[Bc], writes=[Bsc])

        def mod_load(wa, ct):
            k = ct % 2
            S.dma("pool", wa[k][:], w_ada[:, ct * 512:(ct + 1) * 512].rearrange("(dc p) n -> p dc n", p=128),
                  Bwa[k], writes=[Bwa[k]], max_dma_last_dim=8192)

        def mod_mm(wa, ct, idx, bank, col0):
            k = ct % 2
            mm, dc = idx // NDC, idx % NDC
            jc = ct * 4 + mm - col0
            S.op("pe", lambda e: e.matmul(ps[bank][:, jc:jc + 1], lhsT=wa[k][:, dc, mm * 128:(mm + 1) * 128],
                                          rhs=sc[:, dc:dc + 1], start=(dc == 0), stop=(dc == NDC - 1), skip_group_check=True),
                 reads=[Bwa[k], Bsc], writes=[P[bank]])

        with ExitStack() as ph:
            wa = [sb(ph, f"wa{i}", [128, NDC, 512], BF16) for i in range(2)]
            mod_load(wa, 0)
            for ct in range(16):
                if ct + 1 < 16:
                    mod_load(wa, ct + 1)
                for idx in range(128):
                    mod_mm(wa, ct, idx, 0, 0)
            S.op("dve", lambda e: e.tensor_tensor(out=modv[:, 0:64], in0=ps[0][:, 0:64], in1=vec[:, OFF_BADA:OFF_BADA + 64],
                                                  op=ALU.add), reads=[P[0], Bvec], writes=[Bmod])
            S.op("dve", lambda e: e.scalar_tensor_tensor(out=A12[:, 0:32], in0=modv[:, 32:64], scalar=1.0, op0=ALU.add,
                                                         in1=vec[:, OFF_G1:OFF_G1 + 32], op1=ALU.mult),
                 reads=[Bmod, Bvec], writes=[BA12])
            S.barrier()
        A1 = A12[:, 0:32]
        A2 = A12[:, 32:64]
        B1 = modv[:, 0:32]
        gate1 = modv[:, 64:96]
        B2 = modv[:, 96:128]
        gate2 = modv[:, 160:192]
        Bmv = [Bmod, BA12, Bvec]

        def emit_rstd(st_bufs, Xt, BXs, rstd, Brstd, bank, n_part_inv):
            sq, Bsq = st_bufs
            for dc in range(NDC):
                k = dc % 2
                S.op("act", lambda e: e.activation(out=sq[k][:], in_=Xt[:, dc, :], func=AF.Square),
                     reads=[BXs[dc // 8]], writes=[Bsq[k]])
                S.op("pe", lambda e: e.matmul(ps[bank][:], lhsT=ones_bf, rhs=sq[k][:], start=(dc == 0), stop=(dc == NDC - 1)),
                     reads=[Bsq[k], Bcst], writes=[P[bank]])
            S.op("act", lambda e: e.activation(out=rstd[:], in_=ps[bank][:], func=AF.Sqrt, scale=n_part_inv, bias=EPS),
                 reads=[P[bank]], writes=[Brstd])
            S.op("dve", lambda e: e.reciprocal(out=rstd[:], in_=rstd[:]), reads=[Brstd], writes=[Brstd])

        def emit_norm_mod(Xt, BXs, rstd, Brstd, Acol, Bcol, tmp, Btmp, hT, BhT):
            for dc in range(NDC):
                k = dc % 2
                S.op("dve", lambda e: e.scalar_tensor_tensor(out=tmp[k][:], in0=Xt[:, dc, :], scalar=Acol[:, dc:dc + 1],
                                                             op0=ALU.mult, in1=rstd[:], op1=ALU.mult),
                     reads=[BXs[dc // 8], Brstd] + Bmv, writes=[Btmp[k]])
                S.op("act", lambda e: e.activation(out=hT[:, dc, :], in_=tmp[k][:], func=AF.Identity,
                                                   bias=Bcol[:, dc:dc + 1]),
                     reads=[Btmp[k]] + Bmv, writes=[BhT])

        if stop_after >= 1:
            with ExitStack() as ph:
                Xs = [sb(ph, f"X1_{b_}", [128, NDC, T], F32) for b_ in range(2)]
                BXs = [[Buf(f"X1_{b_}_{i}") for i in range(4)] for b_ in range(2)]
                sq = [sb(ph, f"sq{i}", [128, T], BF16) for i in range(2)]
                Bsq = [Buf(f"sq{i}") for i in range(2)]
                rstds = [sb(ph, f"rstd{i}", [128, T], F32) for i in range(2)]
                Brstds = [Buf("rstd_a"), Buf("rstd_b")]
                tmp = [sb(ph, f"tmp{i}", [128, T], F32) for i in range(2)]
                Btmp = [Buf("tmp0"), Buf("tmp1")]
                hT = [sb(ph, f"hT{i}", [128, NDC, T], BF16) for i in range(2)]
                BhT = [Buf("hT0"), Buf("hT1")]

                def load_x(s_):
                    for qd in range(4):
                        S.dma("sp", Xs[s_ % 2][:, qd * 8:(qd + 1) * 8, :], xT[s_, :, qd * 8:(qd + 1) * 8, :], BXs[s_ % 2][qd],
                              writes=[BXs[s_ % 2][qd]])

                def sumsq(s_):
                    X_, BX_ = Xs[s_ % 2], BXs[s_ % 2]
                    for dc in range(NDC):
                        k = dc % 2
                        S.op("pool", lambda e: e.tensor_tensor(out=sq[k][:], in0=X_[:, dc, :], in1=X_[:, dc, :], op=ALU.mult),
                             reads=[BX_[dc // 8]], writes=[Bsq[k]])
                        S.op("pe", lambda e: e.matmul(ps[s_ % 2][:], lhsT=ones_bf, rhs=sq[k][:], start=(dc == 0), stop=(dc == NDC - 1)),
                             reads=[Bsq[k], Bcst], writes=[P[s_ % 2]])

                def finish_rstd(s_):
                    r_, Br_ = rstds[s_ % 2], Brstds[s_ % 2]
                    S.op("act", lambda e: e.activation(out=r_[:], in_=ps[s_ % 2][:], func=AF.Sqrt, scale=1.0 / D, bias=EPS),
                         reads=[P[s_ % 2]], writes=[Br_])
                    S.op("dve", lambda e: e.reciprocal(out=r_[:], in_=r_[:]), reads=[Br_], writes=[Br_])

                load_x(0)
                sumsq(0)
                finish_rstd(0)
                for s in range(NSLOT):
                    if s + 1 < NSLOT:
                        load_x(s + 1)
                        sumsq(s + 1)
                    emit_norm_mod(Xs[s % 2], BXs[s % 2], rstds[s % 2], Brstds[s % 2], A1, B1, tmp, Btmp, hT[s % 2], BhT[s % 2])
                    S.dma("act", hT_s[s], hT[s % 2][:], BhT[s % 2], reads=[BhT[s % 2]], writes=[BhT_s])
                    if s + 1 < NSLOT:
                        finish_rstd(s + 1)
                S.barrier()

        if stop_after >= 2:
            with ExitStack() as ph:
                Wg = [sb(ph, f"Wg{i}", [128, NDC, 512], BF16) for i in range(2)]
                BWg = [Buf("Wg0"), Buf("Wg1")]
                hTt = [sb(ph, f"hTt{i}", [128, NDC, T], BF16) for i in range(2)]
                BhTt = [Buf("hTt0"), Buf("hTt1")]
                stg = [sb(ph, f"stg{i}", [128, 512], BF16) for i in range(4)]
                Bstg = [Buf(f"stg{i}") for i in range(4)]
                Pb = [sb(ph, f"Pb{i}", [128, 528], F32) for i in range(4)]
                BPb = [Buf(f"Pb{i}") for i in range(4)]
                T1 = sb(ph, "T1", [128, 528], F32)
                T2 = sb(ph, "T2", [128, 528], F32)
                BT1, BT2 = Buf("T1"), Buf("T2")
                dT = [sb(ph, f"dT{i}", [128, 512], BF16) for i in range(4)]
                BdT = [Buf(f"dT{i}") for i in range(4)]
                d16 = sb(ph, "d16", [128, 16], F32)
                Bd16 = Buf("d16")
                hh = sb(ph, "hh", [128, NDC, 16], BF16)
                Bhh = Buf("hh")
                wp = sb(ph, "wp", [128, 4, 512], BF16)
                Bwp = Buf("wp")
                nload = [0]
                nps = [0]
                nst = [0]

                def load_h(slot):
                    k = nload[0] % 2
                    nload[0] += 1
                    S.dma("sp", hTt[k][:], hT_s[slot], BhTt[k], reads=[BhT_s], writes=[BhTt[k]])
                    return k

                for cg in range(16):
                    kw = cg % 2
                    S.dma("pool", Wg[kw][:], w_in[:, cg * 512:(cg + 1) * 512].rearrange("(dc p) n -> p dc n", p=128),
                          BWg[kw], writes=[BWg[kw]], max_dma_last_dim=8192)
                    kind = cg // 4
                    sub = cg % 4
                    if kind in (0, 1):
                        slots = [1, 3, 5, 7] if kind == 0 else list(range(NSLOT))
                        for si, slot in enumerate(slots):
                            kh = load_h(slot)
                            for hx in range(4):
                                head = sub * 4 + hx
                                r = nps[0] % 4
                                nps[0] += 1
                                for dc in range(NDC):
                                    S.op("pe", lambda e: e.matmul(ps[r][:], lhsT=Wg[kw][:, dc, hx * 128:(hx + 1) * 128],
                                                                  rhs=hTt[kh][:, dc, :], start=(dc == 0), stop=(dc == NDC - 1)),
                                         reads=[BWg[kw], BhTt[kh]], writes=[P[r]])
                                q = nst[0] % 4
                                nst[0] += 1
                                sc_ = (128.0 ** -0.5) if kind == 0 else 1.0
                                S.op("act", lambda e: e.activation(out=stg[q][:], in_=ps[r][:], func=AF.Copy, scale=sc_),
                                     reads=[P[r]], writes=[Bstg[q]])
                                if kind == 0:
                                    S.dma("act", QT_s[head, :, si * T:(si + 1) * T], stg[q][:], Bstg[q], reads=[Bstg[q]], writes=[BQT_s])
                                else:
                                    S.dma("act", KT_s[head, :, slot * T:(slot + 1) * T], stg[q][:], Bstg[q], reads=[Bstg[q]], writes=[BKT_s])
                    elif kind == 2:
                        for slot in range(NSLOT):
                            kh = load_h(slot)
                            for tb in range(4):
                                r = nps[0] % 4
                                nps[0] += 1
                                for dc in range(NDC):
                                    S.op("pe", lambda e: e.matmul(ps[r][:], lhsT=hTt[kh][:, dc, tb * 128:(tb + 1) * 128],
                                                                  rhs=Wg[kw][:, dc, :], start=(dc == 0), stop=(dc == NDC - 1)),
                                         reads=[BWg[kw], BhTt[kh]], writes=[P[r]])
                                q = nst[0] % 4
                                nst[0] += 1
                                if slot == 0:
                                    S.op("act", lambda e: e.activation(out=stg[q][:], in_=ps[r][:], func=AF.Identity, scale=flag),
                                         reads=[P[r], Bvec], writes=[Bstg[q]])
                                else:
                                    S.op("act", lambda e: e.activation(out=stg[q][:], in_=ps[r][:], func=AF.Copy),
                                         reads=[P[r]], writes=[Bstg[q]])
                                S.dma("act", V_s[slot * 4 + tb, :, sub * 512:(sub + 1) * 512], stg[q][:], Bstg[q],
                                      reads=[Bstg[q]], writes=[BV_s])
                    else:
                        g = sub
                        wdw = POOL_W[g]
                        S.dma("pool", wp[:], w_pool[g].rearrange("(cc p) n -> p cc n", p=128), Bwp, writes=[Bwp])
                        for j in range(NOWN):
                            S.dma("sp", hh[:], hT_s[2 * j, :, :, T - 16:T], Bhh, reads=[BhT_s], writes=[Bhh])
                            kh = load_h(2 * j + 1)
                            for cc in range(4):
                                r = nps[0] % 4
                                nps[0] += 1
                                for dc in range(NDC):
                                    S.op("pe", lambda e: e.matmul(ps[r][:, 0:16], lhsT=Wg[kw][:, dc, cc * 128:(cc + 1) * 128],
                                                                  rhs=hh[:, dc, :], start=(dc == 0), stop=(dc == NDC - 1)),
                                         reads=[BWg[kw], Bhh], writes=[P[r]])
                                if j == 0:
                                    S.op("act", lambda e: e.activation(out=Pb[cc][:, 0:16], in_=ps[r][:, 0:16], func=AF.Identity, scale=flag),
                                         reads=[P[r], Bvec], writes=[BPb[cc]])
                                else:
                                    S.op("act", lambda e: e.activation(out=Pb[cc][:, 0:16], in_=ps[r][:, 0:16], func=AF.Copy),
                                         reads=[P[r]], writes=[BPb[cc]])
                                r = nps[0] % 4
                                nps[0] += 1
                                for dc in range(NDC):
                                    S.op("pe", lambda e: e.matmul(ps[r][:], lhsT=Wg[kw][:, dc, cc * 128:(cc + 1) * 128],
                                                                  rhs=hTt[kh][:, dc, :], start=(dc == 0), stop=(dc == NDC - 1)),
                                         reads=[BWg[kw], BhTt[kh]], writes=[P[r]])
                                S.op("act", lambda e: e.activation(out=Pb[cc][:, 16:528], in_=ps[r][:], func=AF.Copy),
                                     reads=[P[r]], writes=[BPb[cc]])
                                S.op("dve", lambda e: e.tensor_tensor(out=T1[:, 1:528], in0=Pb[cc][:, 1:528], in1=Pb[cc][:, 0:527], op=ALU.add),
                                     reads=[BPb[cc]], writes=[BT1])
                                ws, Bws = T1, BT1
                                if wdw >= 4:
                                    S.op("dve", lambda e: e.tensor_tensor(out=T2[:, 3:528], in0=T1[:, 3:528], in1=T1[:, 1:526], op=ALU.add),
                                         reads=[BT1], writes=[BT2])
                                    ws, Bws = T2, BT2
                                if wdw >= 8:
                                    S.op("dve", lambda e: e.tensor_tensor(out=T1[:, 7:528], in0=T2[:, 7:528], in1=T2[:, 3:524], op=ALU.add),
                                         reads=[BT2], writes=[BT1])
                                    ws, Bws = T1, BT1
                                if wdw >= 16:
                                    S.op("dve", lambda e: e.tensor_tensor(out=T2[:, 15:528], in0=T1[:, 15:528], in1=T1[:, 7:520], op=ALU.add),
                                         reads=[BT1], writes=[BT2])
                                    ws, Bws = T2, BT2
                                S.op("dve", lambda e: e.scalar_tensor_tensor(out=dT[cc][:], in0=ws[:, 16:528], scalar=1.0 / wdw, op0=ALU.mult,
                                                                             in1=Pb[cc][:, 16:528], op1=ALU.subtract),
                                     reads=[Bws, BPb[cc]], writes=[BdT[cc]])
                                if j == 0:
                                    S.op("dve", lambda e: e.tensor_tensor(out=d16[:], in0=ws[:, 16:32],
                                                                          in1=vec[:, OFF_INVC + g * 16:OFF_INVC + (g + 1) * 16], op=ALU.mult),
                                         reads=[Bws, Bvec], writes=[Bd16])
                                    S.op("dve", lambda e: e.tensor_tensor(out=dT[cc][:, 0:16], in0=d16[:], in1=Pb[cc][:, 16:32], op=ALU.subtract),
                                         reads=[Bd16, BPb[cc]], writes=[BdT[cc]])
                            for dd in range(4):
                                r = nps[0] % 4
                                nps[0] += 1
                                for cc in range(4):
                                    S.op("pe", lambda e: e.matmul(ps[r][:], lhsT=wp[:, cc, dd * 128:(dd + 1) * 128], rhs=dT[cc][:],
                                                                  start=(cc == 0), stop=(cc == 3)),
                                         reads=[Bwp, BdT[cc]], writes=[P[r]])
                                q = nst[0] % 4
                                nst[0] += 1
                                col = OFF_SP + g * 4 + dd
                                S.op("act", lambda e: e.activation(out=stg[q][:], in_=ps[r][:], func=AF.Identity, scale=vec[:, col:col + 1]),
                                     reads=[P[r], Bvec], writes=[Bstg[q]])
                                S.dma("act", oT_s[j, :, 16 + g * 4 + dd, :], stg[q][:], Bstg[q], reads=[Bstg[q]], writes=[BoT_s])
                S.barrier()

        if stop_after >= 3:
            with ExitStack() as ph:
                KTh = [sb(ph, f"KTh{i}", [128, NSLOT * T], BF16) for i in range(2)]
                Vh = [sb(ph, f"Vh{i}", [128, NSLOT * 4, 128], BF16) for i in range(2)]
                QTh = [sb(ph, f"QTh{i}", [128, NOWN * T], BF16) for i in range(2)]
                BKTh = [Buf("KTh0"), Buf("KTh1")]
                BVh = [Buf("Vh0"), Buf("Vh1")]
                BQTh = [Buf("QTh0"), Buf("QTh1")]
                E = [sb(ph, f"E{i}", [128, T], F32) for i in range(2)]
                BE = [Buf("E0"), Buf("E1")]
                Lp = [sb(ph, f"Lp{i}", [128, T], BF16) for i in range(3)]
                BLp = [Buf(f"Lp{i}") for i in range(3)]
                Ls = sb(ph, "Ls", [128, T], BF16)
                BLs = Buf("Ls")
                Aa = [sb(ph, f"Aa{i}", [128, T], BF16) for i in range(2)]
                BAa = [Buf("Aa0"), Buf("Aa1")]
                sqa = sb(ph, "sqa", [128, T], BF16)
                Bsqa = Buf("sqa")
                rsa = sb(ph, "rsa", [128, T], F32)
                Brsa = Buf("rsa")
                ost = [sb(ph, f"ost{i}", [128, T], BF16) for i in range(2)]
                Bost = [Buf("ost0"), Buf("ost1")]

                units = []
                for h in range(NH):
                    for j in range(NOWN):
                        qs = 2 * j + 1
                        kbs = list(range(qs * 4 + 3, -1, -1))
                        for ui, kb in enumerate(kbs):
                            c0 = (kb - qs * 4) * 128 if kb >= qs * 4 else 0
                            units.append(dict(h=h, j=j, kb=kb, c0=c0, first=(ui == 0), last=(ui == len(kbs) - 1),
                                              diag=(kb >= qs * 4)))

                def load_head(h):
                    k = h % 2
                    S.dma("sp", KTh[k][:], KT_s[h], BKTh[k], reads=[BKT_s], writes=[BKTh[k]])
                    S.dma("sp", QTh[k][:], QT_s[h], BQTh[k], reads=[BQT_s], writes=[BQTh[k]])
                    S.dma("sp", Vh[k][:], V_s[:, :, h * 128:(h + 1) * 128].rearrange("tb p d -> p tb d"), BVh[k],
                          reads=[BV_s], writes=[BVh[k]])

                def stage0(i, u):
                    hk = u["h"] % 2
                    zb = i % 4
                    c0 = u["c0"]
                    qcol = u["j"] * T
                    S.op("pe", lambda e: e.matmul(ps[zb][:, c0:T], lhsT=KTh[hk][:, u["kb"] * 128:(u["kb"] + 1) * 128],
                                                  rhs=QTh[hk][:, qcol + c0:qcol + T], start=True, stop=True),
                         reads=[BKTh[hk], BQTh[hk]], writes=[P[zb]])

                def stage1(i, u):
                    zb = i % 4
                    c0 = u["c0"]
                    S.op("act", lambda e: e.activation(out=E[i % 2][:, c0:T], in_=ps[zb][:, c0:T], func=AF.Exp),
                         reads=[P[zb]], writes=[BE[i % 2]])
                    S.op("act", lambda e: e.activation(out=Lp[i % 3][:, c0:T], in_=E[i % 2][:, c0:T], func=AF.Ln, bias=1.0),
                         reads=[BE[i % 2]], writes=[BLp[i % 3]])
                    if u["diag"]:
                        S.op("dve", lambda e: e.tensor_tensor(out=Lp[i % 3][:, c0:c0 + 128], in0=Lp[i % 3][:, c0:c0 + 128],
                                                              in1=tri_bf, op=ALU.mult),
                             reads=[BLp[i % 3], Bcst], writes=[BLp[i % 3]])

                def stage2(i, u):
                    zb = i % 4
                    c0 = u["c0"]
                    S.op("pe", lambda e: e.matmul(ps[zb][:, c0:T], lhsT=negU_bf, rhs=Lp[i % 3][:, c0:T], start=False, stop=u["first"],
                                                  skip_group_check=True),
                         reads=[BLp[i % 3], Bcst], writes=[P[zb]])
                    if u["first"]:
                        S.op("pool", lambda e: e.memset(Ls[:], 0.0), writes=[BLs])
                    else:
                        S.op("pe", lambda e: e.matmul(ps[zb][:, c0:T], lhsT=negones_bf, rhs=Ls[:, c0:T], start=False, stop=True,
                                                      skip_group_check=True),
                             reads=[BLs, Bcst], writes=[P[zb]])
                    if not u["last"]:
                        S.op("pool", lambda e: e.tensor_tensor(out=Ls[:, c0:T], in0=Ls[:, c0:T], in1=Lp[i % 3][:, c0:T], op=ALU.add),
                             reads=[BLs, BLp[i % 3]], writes=[BLs])

                def stage3(i, u):
                    hk = u["h"] % 2
                    zb = i % 4
                    c0 = u["c0"]
                    ob = 4 + (u["h"] * NOWN + u["j"]) % 2
                    S.op("act", lambda e: e.activation(out=Aa[i % 2][:, c0:T], in_=ps[zb][:, c0:T], func=AF.Exp),
                         reads=[P[zb]], writes=[BAa[i % 2]])
                    if u["diag"]:
                        S.op("dve", lambda e: e.tensor_tensor(out=Aa[i % 2][:, c0:c0 + 128], in0=Aa[i % 2][:, c0:c0 + 128],
                                                              in1=tri_bf, op=ALU.mult),
                             reads=[BAa[i % 2], Bcst], writes=[BAa[i % 2]])
                    S.op("pe", lambda e: e.matmul(ps[ob][:, c0:T], lhsT=Vh[hk][:, u["kb"], :], rhs=Aa[i % 2][:, c0:T],
                                                  start=u["first"], stop=u["last"], skip_group_check=True),
                         reads=[BVh[hk], BAa[i % 2]], writes=[P[ob]])
                    if u["last"]:
                        S.op("act", lambda e: e.activation(out=sqa[:], in_=ps[ob][:], func=AF.Square), reads=[P[ob]], writes=[Bsqa])
                        S.op("pe", lambda e: e.matmul(ps[6][:], lhsT=ones_bf, rhs=sqa[:], start=True, stop=True),
                             reads=[Bsqa, Bcst], writes=[P[6]])
                        epi.append((i + 2, u["h"], u["j"], ob))

                def epilogue_b(h, j, ob):
                    k = (h * NOWN + j) % 2
                    S.op("act", lambda e: e.activation(out=rsa[:], in_=ps[6][:], func=AF.Ln, scale=1.0 / 128, bias=EPS),
                         reads=[P[6]], writes=[Brsa])
                    S.op("act", lambda e: e.activation(out=rsa[:], in_=rsa[:], func=AF.Exp, scale=-0.5), reads=[Brsa], writes=[Brsa])
                    S.op("dve", lambda e: e.scalar_tensor_tensor(out=ost[k][:], in0=ps[ob][:], scalar=vec[:, OFF_GH + h:OFF_GH + h + 1],
                                                                 op0=ALU.mult, in1=rsa[:], op1=ALU.mult),
                         reads=[P[ob], Brsa, Bvec], writes=[Bost[k]])
                    S.dma("sp", oT_s[j, :, h, :], ost[k][:], Bost[k], reads=[Bost[k]], writes=[BoT_s])

                load_head(0)
                n = len(units)
                epi = []
                wa3 = [sb(ph, f"wa3_{i}", [128, NDC, 512], BF16) for i in range(2)]
                mod_load(wa3, 16)
                for i in range(n + 3):
                    if i < n:
                        u = units[i]
                        if u["first"] and u["j"] == 0 and u["h"] + 1 < NH:
                            load_head(u["h"] + 1)
                        stage0(i, u)
                        mq = i * 4
                        if mq < 32 * 128:
                            ct = 16 + mq // 128
                            if mq % 128 == 0 and ct + 1 < 48:
                                mod_load(wa3, ct + 1)
                            for idx in range(mq % 128, mq % 128 + 4):
                                mod_mm(wa3, ct, idx, 7, 64)
                    if 0 <= i - 1 < n:
                        stage1(i - 1, units[i - 1])
                    if 0 <= i - 2 < n:
                        stage2(i - 2, units[i - 2])
                    if 0 <= i - 3 < n:
                        stage3(i - 3, units[i - 3])
                    while epi and epi[0][0] <= i - 3:
                        epilogue_b(*epi.pop(0)[1:])
                while epi:
                    epilogue_b(*epi.pop(0)[1:])
                S.op("dve", lambda e: e.tensor_tensor(out=modv[:, 64:192], in0=ps[7][:, 0:128], in1=vec[:, OFF_BADA + 64:OFF_BADA + 192],
                                                      op=ALU.add), reads=[P[7], Bvec], writes=[Bmod])
                S.op("dve", lambda e: e.scalar_tensor_tensor(out=A12[:, 32:64], in0=modv[:, 128:160], scalar=1.0, op0=ALU.add,
                                                             in1=vec[:, OFF_G2:OFF_G2 + 32], op1=ALU.mult),
                     reads=[Bmod, Bvec], writes=[BA12])
                if debug:
                    S.dma("sp", dbg_mod, modv[:], Bmod, reads=[Bmod], writes=[Bdbg])
                S.barrier()

        if stop_after >= 4:
            with ExitStack() as ph:
                X = sb(ph, "X2", [128, NDC, T], F32)
                BX = [Buf(f"X2_{i}") for i in range(4)]
                h2T = sb(ph, "h2T", [128, NDC, T], BF16)
                Bh2T = Buf("h2T")
                S12 = sb(ph, "S12", [128, 4, 8, 256], F32)
                BS12 = Buf("S12")
                TD = sb(ph, "TD", [128, 4, 8, 2], F32)
                BTD = Buf("TD")
                sq = [sb(ph, f"sqb{i}", [128, T], BF16) for i in range(2)]
                Bsq = [Buf("sqb0"), Buf("sqb1")]
                rstd = sb(ph, "rstd2", [128, T], F32)
                Brstd = Buf("rstd2")
                skb = sb(ph, "skb", [128, 2, 128], BF16)
                Bskb = Buf("skb")
                S.dma("pool", skb[:], skT, Bskb, writes=[Bskb])
                for j in range(NOWN):
                    with ExitStack() as p4:
                        oTt = sb(p4, "oTt", [128, NDC, T], BF16)
                        BoTt = Buf("oTt")
                        Wo = [sb(p4, f"Wo{i}", [128, NDC, 256], BF16) for i in range(2)]
                        BWo = [Buf("Wo0"), Buf("Wo1")]
                        for qd in range(4):
                            S.dma("sp", X[:, qd * 8:(qd + 1) * 8, :], xT[2 * j + 1, :, qd * 8:(qd + 1) * 8, :], BX[qd], writes=[BX[qd]])
                        S.dma("sp", oTt[:], oT_s[j], BoTt, reads=[BoT_s], writes=[BoTt])
                        for cg in range(16):
                            k = cg % 2
                            S.dma("pool", Wo[k][:], w_out[:, cg * 256:(cg + 1) * 256].rearrange("(ec p) n -> p ec n", p=128),
                                  BWo[k], writes=[BWo[k]])
                            for dd in range(2):
                                dch = cg * 2 + dd
                                r = dch % 4
                                for ec in range(NDC):
                                    S.op("pe", lambda e: e.matmul(ps[r][:], lhsT=Wo[k][:, ec, dd * 128:(dd + 1) * 128], rhs=oTt[:, ec, :],
                                                                  start=(ec == 0), stop=(ec == NDC - 1)),
                                         reads=[BWo[k], BoTt], writes=[P[r]])
                                S.op("dve", lambda e: e.scalar_tensor_tensor(out=X[:, dch, :], in0=ps[r][:], scalar=gate1[:, dch:dch + 1],
                                                                             op0=ALU.mult, in1=X[:, dch, :], op1=ALU.add),
                                     reads=[P[r], BX[dch // 8]] + Bmv, writes=[BX[dch // 8]])
                        if debug:
                            S.dma("sp", dbg_x1[j], X[:], BX[0], reads=BX, writes=[Bdbg])
                        S.barrier()
                    if stop_after < 5:
                        continue
                    with ExitStack() as p5:
                        tmp = [sb(p5, f"tmpb{i}", [128, T], F32) for i in range(2)]
                        Btmp = [Buf("tmpb0"), Buf("tmpb1")]
                        Wq = [sb(p5, f"Wq{i}", [128, NDC, 256], BF16) for i in range(2)]
                        BWq = [Buf("Wq0"), Buf("Wq1")]
                        qT = [sb(p5, f"qT{i}", [128, 2, T], BF16) for i in range(2)]
                        BqT = [Buf("qT0"), Buf("qT1")]
                        v16 = [sb(p5, f"v16_{i}", [128, 2, 16], F32) for i in range(2)]
                        Bv16 = [Buf("v16_0"), Buf("v16_1")]
                        tmpk = [sb(p5, f"tmpk{i}", [128, 128], F32) for i in range(2)]
                        Btmpk = [Buf("tmpk0"), Buf("tmpk1")]
                        cand = [sb(p5, f"cand{i}", [128, 256], F32) for i in range(2)]
                        cand2 = [sb(p5, f"cand2_{i}", [128, 256], F32) for i in range(2)]
                        Bcand = [Buf("cand_0"), Buf("cand_1")]
                        Bcand2 = [Buf("cand2_0"), Buf("cand2_1")]
                        c16a = sb(p5, "c16a", [128, 32, 16], F32)
                        Bc16 = Buf("c16a")
                        e16a = sb(p5, "e16a", [128, 32, 16], F32)
                        Be16 = Buf("e16a")
                        sm = sb(p5, "sm", [128, 64], F32)
                        Bsm = Buf("sm")
                        todo = []

                        def cand_topk(kk, idx):
                            S.op("dve", lambda e: e.max(out=c16a[:, idx, 0:8], in_=cand[kk][:]), reads=[Bcand[kk]], writes=[Bc16])
                            S.op("dve", lambda e: e.match_replace(out=cand2[kk][:], in_to_replace=c16a[:, idx, 0:8], in_values=cand[kk][:],
                                                                  imm_value=-1e30), reads=[Bcand[kk], Bc16], writes=[Bcand2[kk]])
                            S.op("dve", lambda e: e.max(out=c16a[:, idx, 8:16], in_=cand2[kk][:]), reads=[Bcand2[kk]], writes=[Bc16])

                        emit_rstd((sq, Bsq), X, BX, rstd, Brstd, 4, 1.0 / D)
                        emit_norm_mod(X, BX, rstd, Brstd, A2, B2, tmp, Btmp, h2T, Bh2T)
                        if debug:
                            S.dma("sp", dbg_h2[j], h2T[:], Bh2T, reads=[Bh2T], writes=[Bdbg])
                        def load_wq(hq_):
                            S.dma("pool", Wq[hq_ % 2][:], w_query[:, hq_ * 256:(hq_ + 1) * 256].rearrange("(dc p) n -> p dc n", p=128),
                                  BWq[hq_ % 2], writes=[BWq[hq_ % 2]])

                        load_wq(0)
                        load_wq(1)
                        for hq in range(8):
                            k = hq % 2
                            for cc in range(2):
                                r = cc
                                for dc in range(NDC):
                                    S.op("pe", lambda e: e.matmul(ps[r][:], lhsT=Wq[k][:, dc, cc * 128:(cc + 1) * 128], rhs=h2T[:, dc, :],
                                                                  start=(dc == 0), stop=(dc == NDC - 1)),
                                         reads=[BWq[k], Bh2T], writes=[P[r]])
                                S.op("act", lambda e: e.activation(out=qT[k][:, cc, :], in_=ps[r][:], func=AF.Copy),
                                     reads=[P[r]], writes=[BqT[k]])
                            if hq + 2 < 8:
                                load_wq(hq + 2)
                            for tb in range(4):
                                r = 2 + tb % 2
                                for w_ in range(2):
                                    S.op("pe", lambda e: e.matmul(ps[r][:, w_ * 128:(w_ + 1) * 128], lhsT=qT[k][:, w_, tb * 128:(tb + 1) * 128],
                                                                  rhs=skb[:, w_, :], start=True, stop=True, skip_group_check=True),
                                         reads=[BqT[k], Bskb], writes=[P[r]])
                                S.op("act", lambda e: e.activation(out=S12[:, tb, hq, :], in_=ps[r][:, 0:256], func=AF.Copy),
                                     reads=[P[r]], writes=[BS12])
                                kk = (hq * 4 + tb) % 2
                                idx = tb * 8 + hq
                                for w_ in range(2):
                                    src = S12[:, tb, hq, w_ * 128:(w_ + 1) * 128]
                                    S.op("dve", lambda e: e.max(out=v16[kk][:, w_, 0:8], in_=src), reads=[BS12], writes=[Bv16[kk]])
                                    S.op("dve", lambda e: e.match_replace(out=tmpk[kk][:], in_to_replace=v16[kk][:, w_, 0:8], in_values=src,
                                                                          imm_value=-1e30), reads=[BS12, Bv16[kk]], writes=[Btmpk[kk]])
                                    S.op("dve", lambda e: e.max(out=v16[kk][:, w_, 8:16], in_=tmpk[kk][:]), reads=[Btmpk[kk]], writes=[Bv16[kk]])
                                S.op("pool", lambda e: e.tensor_tensor(out=cand[kk][:].rearrange("p (a b) -> p a b", a=16),
                                                                       in0=v16[kk][:, 0, :].unsqueeze(2).broadcast_to([128, 16, 16]),
                                                                       in1=v16[kk][:, 1, :].unsqueeze(1).broadcast_to([128, 16, 16]), op=ALU.add),
                                     reads=[Bv16[kk]], writes=[Bcand[kk]])
                                todo.append((kk, idx))
                                if len(todo) > 1:
                                    cand_topk(*todo.pop(0))
                        while todo:
                            cand_topk(*todo.pop(0))
                        TDv = TD[:].rearrange("p a b c -> p (a b) c")
                        S.op("dve", lambda e: e.tensor_copy(out=TDv[:, :, 0:1], in_=c16a[:, :, 15:16]), reads=[Bc16], writes=[BTD])
                        S.op("dve", lambda e: e.tensor_tensor(out=e16a[:], in0=c16a[:], in1=c16a[:, :, 0:1].broadcast_to([128, 32, 16]),
                                                              op=ALU.subtract), reads=[Bc16], writes=[Be16])
                        S.op("act", lambda e: e.activation(out=e16a[:], in_=e16a[:], func=AF.Exp), reads=[Be16], writes=[Be16])
                        S.op("dve", lambda e: e.reduce_sum(out=sm[:, 0:32], in_=e16a[:], axis=mybir.AxisListType.X), reads=[Be16], writes=[Bsm])
                        S.op("act", lambda e: e.activation(out=sm[:, 32:64], in_=sm[:, 0:32], func=AF.Ln), reads=[Bsm], writes=[Bsm])
                        S.op("dve", lambda e: e.scalar_tensor_tensor(out=TDv[:, :, 1:2], in0=c16a[:, :, 0:1], scalar=-1.0, op0=ALU.mult,
                                                                     in1=sm[:, 32:64].unsqueeze(2), op1=ALU.subtract),
                             reads=[Bc16, Bsm], writes=[BTD])
                        if debug:
                            S.dma("sp", dbg_s12[j], S12[:], BS12, reads=[BS12], writes=[Bdbg])
                            S.dma("sp", dbg_td[j], TD[:], BTD, reads=[BTD], writes=[Bdbg])
                        S.barrier()
                    if stop_after < 6:
                        continue
                    with ExitStack() as p6:
                        ub = [sb(p6, f"ub{i}", [128, NDC, 128], BF16) for i in range(2)]
                        Bub = [Buf("ub0"), Buf("ub1")]
                        vb = [sb(p6, f"vb{i}", [128, GRP, 2048], BF16) for i in range(2)]
                        Bvb = [Buf("vb0"), Buf("vb1")]
                        actT = [sb(p6, f"actT{i}", [128, T], BF16) for i in range(GRP)]
                        BactT = [Buf(f"actT{i}") for i in range(GRP)]
                        NCB, NGM, LAG = 3, 6, 4
                        Cb = [sb(p6, f"Cb{i}", [128, GRP * 128], F32) for i in range(NCB)]
                        BCb = [Buf(f"Cb{i}") for i in range(NCB)]
                        Gx = [sb(p6, f"Gx{i}", [128, GRP * 128], BF16) for i in range(NCB)]
                        BGx = [Buf(f"Gx{i}") for i in range(NCB)]
                        Gm = [sb(p6, f"Gm{i}", [128, GRP * 128], BF16) for i in range(NGM)]
                        BGm = [Buf(f"Gm{i}") for i in range(NGM)]
                        gl = [sb(p6, f"gl{i}", [128, T], BF16) for i in range(GRP)]
                        Bgl = [Buf(f"gl{i}") for i in range(GRP)]
                        WTB = [0, 1, 2, 3]
                        SB_ = [4, 5]
                        YB = [6, 7]
                        cnt = dict(w=0, s=0, y=0)
                        if j == 0 and debug:
                            print("sbuf bytes remaining in PEER scope:", nc.sbuf_bytes_remaining)
                        pending = []

                        def wbuild_elem(g, pi):
                            tb, hq = pi // 8, pi % 8
                            k = cnt["w"] % NCB
                            km = cnt["w"] % NGM
                            cnt["w"] += 1
                            i0 = g * GRP
                            S.op("pool", lambda e: e.tensor_tensor(
                                out=Cb[k][:].rearrange("p (a b) -> p a b", a=GRP),
                                in0=S12[:, tb, hq, i0:i0 + GRP].unsqueeze(2).broadcast_to([128, GRP, 128]),
                                in1=S12[:, tb, hq, 128:256].unsqueeze(1).broadcast_to([128, GRP, 128]), op=ALU.add),
                                reads=[BS12], writes=[BCb[k]])
                            S.op("act", lambda e: e.activation(out=Gx[k][:], in_=Cb[k][:], func=AF.Exp, bias=TD[:, tb, hq, 1:2]),
                                 reads=[BCb[k], BTD], writes=[BGx[k]])
                            S.op("dve", lambda e: e.scalar_tensor_tensor(out=Gm[km][:], in0=Cb[k][:], scalar=TD[:, tb, hq, 0:1], op0=ALU.is_ge,
                                                                         in1=Gx[k][:], op1=ALU.mult),
                                 reads=[BCb[k], BGx[k], BTD], writes=[BGm[km]])
                            pending.append((km, tb, hq))

                        def emit_tr():
                            km, tb, hq = pending.pop(0)
                            for a in range(GRP):
                                S.op("pe", lambda e: e.matmul(ps[WTB[a]][:, tb * 128:(tb + 1) * 128], lhsT=Gm[km][:, a * 128:(a + 1) * 128],
                                                              rhs=ident_bf, start=(hq == 0), stop=(hq == 7), skip_group_check=True),
                                     reads=[BGm[km], Bcst], writes=[P[WTB[a]]])

                        def load_u(eb):
                            k = eb % 2
                            S.dma("pool", ub[k][:].rearrange("p a b -> p (a b)"), uT[eb], Bub[k], writes=[Bub[k]], max_dma_last_dim=8192)

                        def load_v(g, hf):
                            S.dma("pool", vb[hf][:], vE[g * GRP * 128:(g + 1) * GRP * 128, hf * 2048:(hf + 1) * 2048].rearrange("(a p) d -> p a d", p=128),
                                  Bvb[hf], writes=[Bvb[hf]], max_dma_last_dim=8192)

                        def s_block(eb, dcs):
                            ku = eb % 2
                            sbk = SB_[eb % 2]
                            for dc in dcs:
                                S.op("pe", lambda e: e.matmul(ps[sbk][:], lhsT=ub[ku][:, dc, :], rhs=h2T[:, dc, :],
                                                              start=(dc == 0), stop=(dc == NDC - 1)),
                                     reads=[Bub[ku], Bh2T], writes=[P[sbk]])

                        def s_finish(eb):
                            sbk = SB_[eb % 2]
                            a_ = eb % GRP
                            S.op("act", lambda e: e.activation(out=gl[a_][:], in_=ps[sbk][:], func=AF.Gelu),
                                 reads=[P[sbk]], writes=[Bgl[a_]])
                            if eb + 2 < NEB:
                                load_u(eb + 2)

                        def boundary():
                            rest = list(pending)
                            del pending[:]
                            for a_ in range(GRP):
                                for (km, tb, hq) in rest:
                                    S.op("pe", lambda e: e.matmul(ps[WTB[a_]][:, tb * 128:(tb + 1) * 128], lhsT=Gm[km][:, a_ * 128:(a_ + 1) * 128],
                                                                  rhs=ident_bf, start=(hq == 0), stop=(hq == 7), skip_group_check=True),
                                         reads=[BGm[km], Bcst], writes=[P[WTB[a_]]])
                                S.op("dve", lambda e: e.tensor_tensor(out=actT[a_][:], in0=gl[a_][:], in1=ps[WTB[a_]][:], op=ALU.mult),
                                     reads=[Bgl[a_], P[WTB[a_]]], writes=[BactT[a_]])

                        load_u(0)
                        load_u(1)
                        load_v(0, 0)
                        load_v(0, 1)
                        for pi in range(32):
                            wbuild_elem(0, pi)
                            emit_tr()
                            if pi % 8 == 7:
                                s_block(pi // 8, range(NDC))
                                s_finish(pi // 8)
                        boundary()
                        for g in range(NGRP):
                            for dch in range(NDC):
                                hf = dch // 16
                                yb = YB[cnt["y"] % 2]
                                cnt["y"] += 1
                                if dch == 0 and g > 0:
                                    load_v(g, 1)
                                for a in range(GRP):
                                    S.op("pe", lambda e: e.matmul(ps[yb][:], lhsT=vb[hf][:, a, (dch % 16) * 128:(dch % 16 + 1) * 128], rhs=actT[a][:],
                                                                  start=(a == 0), stop=(a == GRP - 1)),
                                         reads=[Bvb[hf], BactT[a]], writes=[P[yb]])
                                S.op("dve", lambda e: e.scalar_tensor_tensor(out=X[:, dch, :], in0=ps[yb][:], scalar=gate2[:, dch:dch + 1],
                                                                             op0=ALU.mult, in1=X[:, dch, :], op1=ALU.add),
                                     reads=[P[yb], BX[dch // 8]] + Bmv, writes=[BX[dch // 8]])
                                if g + 1 < NGRP:
                                    wbuild_elem(g + 1, dch)
                                    if len(pending) > LAG:
                                        emit_tr()
                                    eb = (g + 1) * GRP + dch // 8
                                    s_block(eb, range((dch % 8) * 4, (dch % 8) * 4 + 4))
                                    if dch % 8 == 7:
                                        s_finish(eb)
                                    if dch == 16:
                                        load_v(g + 1, 0)
                            if g + 1 < NGRP:
                                boundary()
                        emit_rstd((sq, Bsq), X, BX, rstd, Brstd, 4, 1.0 / D)
                        ostf = [Cb[0], Cb[1]]
                        Bostf = [BCb[0], BCb[1]]
                        for dc in range(NDC):
                            k = dc % 2
                            S.op("dve", lambda e: e.scalar_tensor_tensor(out=ostf[k][:], in0=X[:, dc, :], scalar=vec[:, OFF_GF + dc:OFF_GF + dc + 1],
                                                                         op0=ALU.mult, in1=rstd[:], op1=ALU.mult),
                                 reads=[BX[dc // 8], Brstd, Bvec], writes=[Bostf[k]])
                            S.dma("sp", outT[j, :, dc, :], ostf[k][:], Bostf[k], reads=[Bostf[k]], writes=[Bout])
                        S.barrier()
        S.finish()
    return nc


def _consts():
    j = np.arange(128)
    ones = np.ones((128, 128), np.float32)
    ident = np.eye(128, dtype=np.float32)
    negU = -(j[:, None] >= j[None, :]).astype(np.float32)
    tri = (j[:, None] < j[None, :]).astype(np.float32)
    return np.ascontiguousarray(np.stack([ones, -ones, ident, negU, tri], axis=1))


def _pcol(v):
    return np.ascontiguousarray(np.asarray(v, np.float32).reshape(-1, 128).T)


def prepare_inputs(x, c, w_ada, b_ada, g_norm1, w_in, g_attn_head, w_pool, s_pool, w_out, g_norm2, w_query,
                   sub_keys_1, sub_keys_2, u_experts, v_experts, g_final, cores=range(8)):
    x = np.asarray(x, np.float32)
    shared = dict(
        w_ada=np.ascontiguousarray(np.asarray(w_ada, np.float32)[0]),
        consts=_consts(),
        w_in=np.ascontiguousarray(np.asarray(w_in, np.float32)[0]),
        w_pool=np.ascontiguousarray(np.asarray(w_pool, np.float32)[0]),
        w_out=np.ascontiguousarray(np.asarray(w_out, np.float32)[0]),
        w_query=np.ascontiguousarray(np.asarray(w_query, np.float32)[0]),
        skT=np.ascontiguousarray(np.stack([np.asarray(sub_keys_1, np.float32)[0].T, np.asarray(sub_keys_2, np.float32)[0].T], axis=1)),
        vE=np.ascontiguousarray(np.asarray(v_experts, np.float32)[0]),
    )
    u = np.asarray(u_experts, np.float32)[0]
    shared["uT"] = np.ascontiguousarray(u.reshape(NEB, 128, NDC, 128).transpose(0, 3, 2, 1)).reshape(NEB, 128, NDC * 128)
    in_maps = []
    for core in cores:
        b, par = core // 2, core % 2
        xb = x[b]
        xt = xb.reshape(NSLOT, T, NDC, 128).transpose(0, 3, 2, 1)
        loc = np.zeros((NSLOT, 128, NDC, T), np.float32)
        if par == 1:
            loc[:] = xt
        else:
            loc[1:] = xt[:NSLOT - 1]
        vecs = np.zeros((128, NVEC), np.float32)
        vecs[:, OFF_BADA:OFF_BADA + 192] = _pcol(np.asarray(b_ada)[0])
        vecs[:, OFF_G1:OFF_G1 + 32] = _pcol(np.asarray(g_norm1)[0])
        vecs[:, OFF_G2:OFF_G2 + 32] = _pcol(np.asarray(g_norm2)[0])
        vecs[:, OFF_GF:OFF_GF + 32] = _pcol(np.asarray(g_final))
        vecs[:, OFF_GH:OFF_GH + 16] = np.asarray(g_attn_head, np.float32)[0].T
        vecs[:, OFF_SP:OFF_SP + 16] = _pcol(np.asarray(s_pool)[0])
        vecs[:, OFF_FLAG] = float(par)
        for g, w in enumerate(POOL_W):
            cntv = np.minimum(np.arange(16) + 1, w) if par == 0 else np.full(16, w)
            vecs[:, OFF_INVC + g * 16:OFF_INVC + (g + 1) * 16] = (1.0 / cntv.astype(np.float32))[None, :]
        m = dict(shared)
        m["xT"] = loc
        m["cT"] = _pcol(np.asarray(c, np.float32)[b])
        m["vecs"] = vecs
        in_maps.append(m)
    return in_maps


def assemble_output(results, cores=range(8)):
    out = np.zeros((4, 4096, D), np.float32)
    for core, r in zip(cores, results):
        b, par = core // 2, core % 2
        o = np.asarray(r["outT"])
        for j in range(NOWN):
            tile = 2 * j + par
            out[b, tile * T:(tile + 1) * T, :] = o[j].transpose(2, 1, 0).reshape(T, D)
    return out


_NC_CACHE = {}


def kernel(**inputs):
    if "nc" not in _NC_CACHE:
        _NC_CACHE["nc"] = build_program()
    nc = _NC_CACHE["nc"]
    in_maps = prepare_inputs(**inputs)
    res = run_bass_kernel_spmd(nc, in_maps, core_ids=list(range(8)))
    return assemble_output(res.results)
```
